# Optimizing a Trainium2 kernel written in Bass

```python
import math
import jax, jax.numpy as jnp
from jax import lax
import numpy as np

D_MODEL = 1024
BATCH = 4
SEQ = 8192
DEPTH = 1

GRID_W = 64
CTX_LEN = 256
N_ADA = 6
DIFF_HEADS = 8
DIFF_DH = 64
Q_W = 2 * DIFF_HEADS * DIFF_DH
K_W = 2 * DIFF_HEADS * DIFF_DH
V_W = DIFF_HEADS * 2 * DIFF_DH
ATTN_OUT = V_W
Q_BLK = 128
ROPE_BASE = 10000.0
SGU_CHUNK = 128
SGU_GROUPS = 8
SGU_W = 1024
SGU_GROUP_W = SGU_W // SGU_GROUPS
Z_W = 2 * SGU_W
GATE_W = 2 * D_MODEL
IN_W = Q_W + K_W + V_W + Z_W + GATE_W
PEER_HEADS = 8
PEER_N_KEYS = 128
PEER_N_EXPERTS = PEER_N_KEYS * PEER_N_KEYS
PEER_TOPK = 16
PEER_PK_DIM = 128
PEER_Q_DIM = 2 * PEER_PK_DIM
PEER_BLK = 128
RMS_EPS = 1e-6

kernel_name = 'hybrid_diffattn_sgu_peer_dit_layer'


def rmsnorm(x, g=None, eps=RMS_EPS):
    xf = x.astype(jnp.float32)
    y = xf * lax.rsqrt(jnp.mean(xf * xf, axis=-1, keepdims=True) + eps)
    if g is not None:
        y = y * g.astype(jnp.float32)
    return y.astype(x.dtype)


def layernorm(x, g, b, eps=1e-5):
    xf = x.astype(jnp.float32)
    mu = jnp.mean(xf, axis=-1, keepdims=True)
    xc = xf - mu
    y = xc * lax.rsqrt(jnp.mean(xc * xc, axis=-1, keepdims=True) + eps)
    return (y * g.astype(jnp.float32) + b.astype(jnp.float32)).astype(x.dtype)


def modulate(h, shift, scale):
    return h * (1.0 + scale) + shift


def _rotate(xp, pos):
    nfreq = xp.shape[-1] // 2
    inv = ROPE_BASE ** (-jnp.arange(nfreq, dtype=jnp.float32) / nfreq)
    ang = pos.astype(jnp.float32)[:, None] * inv[None, :]
    cos = jnp.cos(ang)[None, :, None, :]
    sin = jnp.sin(ang)[None, :, None, :]
    x1 = xp[..., :nfreq].astype(jnp.float32)
    x2 = xp[..., nfreq:].astype(jnp.float32)
    return jnp.concatenate([x1 * cos - x2 * sin, x1 * sin + x2 * cos], axis=-1)


def axial_rope(x, rows, cols):
    half = x.shape[-1] // 2
    return jnp.concatenate([_rotate(x[..., :half], rows), _rotate(x[..., half:], cols)], axis=-1).astype(x.dtype)


def diff_attention(q, k, v, lam):
    B, L = q.shape[0], q.shape[1]
    Lk = k.shape[1]
    nb = L // Q_BLK
    qb = q.reshape(B, nb, Q_BLK, 2 * DIFF_HEADS, DIFF_DH).transpose(1, 0, 2, 3, 4)

    def one(qblk):
        s = jnp.einsum('bqhd,bkhd->bhqk', qblk, k).astype(jnp.float32) * (DIFF_DH ** -0.5)
        a = jax.nn.softmax(s, axis=-1).reshape(B, DIFF_HEADS, 2, Q_BLK, Lk)
        a = a[:, :, 0] - lam * a[:, :, 1]
        return jnp.einsum('bhqk,bkhe->bqhe', a.astype(v.dtype), v)

    o = lax.map(one, qb)
    return o.transpose(1, 0, 2, 3, 4).reshape(B, L, DIFF_HEADS, 2 * DIFF_DH)


def spatial_gating(z, ln_g, ln_b, w_s, b_s):
    u, v = jnp.split(z, 2, axis=-1)
    v = layernorm(v, ln_g, ln_b)
    B, L = v.shape[0], v.shape[1]
    vb = v.reshape(B, L // SGU_CHUNK, SGU_CHUNK, SGU_GROUPS, SGU_GROUP_W)
    mixed = jnp.einsum('gpq,bnqgc->bnpgc', w_s, vb) + jnp.transpose(b_s)[None, None, :, :, None]
    return u * mixed.reshape(B, L, SGU_W)


def split_proj(p):
    B, L = p.shape[0], p.shape[1]
    q, k, v, z, gl = jnp.split(p, [Q_W, Q_W + K_W, Q_W + K_W + V_W, Q_W + K_W + V_W + Z_W], axis=-1)
    q = q.reshape(B, L, 2 * DIFF_HEADS, DIFF_DH)
    k = k.reshape(B, L, 2 * DIFF_HEADS, DIFF_DH)
    v = v.reshape(B, L, DIFF_HEADS, 2 * DIFF_DH)
    return q, k, v, z, gl


def mix_branches(q, k_all, v_all, z, gate_logits, lam, lam_init, subln_g,
                 sgu_ln_g, sgu_ln_b, sgu_w, sgu_b, w_branch_attn, w_branch_sgu, w_out):
    B, L = q.shape[0], q.shape[1]
    o = diff_attention(q, k_all, v_all, lam)
    o = rmsnorm(o, subln_g) * (1.0 - lam_init)
    y_attn = o.reshape(B, L, ATTN_OUT) @ w_branch_attn
    y_sgu = spatial_gating(jax.nn.gelu(z, approximate=False), sgu_ln_g, sgu_ln_b, sgu_w, sgu_b) @ w_branch_sgu
    g_attn, g_sgu = jnp.split(jax.nn.sigmoid(gate_logits), 2, axis=-1)
    return (g_attn * y_attn + g_sgu * y_sgu) @ w_out


def peer(h, w_query, sub_keys, u_tab, v_tab):
    B, L, D = h.shape
    T = B * L
    xt = h.reshape(T, D)
    q = rmsnorm((xt @ w_query).reshape(T, PEER_HEADS, 2, PEER_PK_DIM))
    s = jnp.einsum('thpd,pnd->thpn', q, sub_keys).astype(jnp.float32)
    s1, i1 = lax.top_k(s[:, :, 0], PEER_TOPK)
    s2, i2 = lax.top_k(s[:, :, 1], PEER_TOPK)
    n_cand = PEER_TOPK * PEER_TOPK
    cand = (s1[..., :, None] + s2[..., None, :]).reshape(T, PEER_HEADS, n_cand)
    cidx = (i1[..., :, None] * PEER_N_KEYS + i2[..., None, :]).reshape(T, PEER_HEADS, n_cand)
    best, pos = lax.top_k(cand, PEER_TOPK)
    eidx = jnp.take_along_axis(cidx, pos, axis=-1)
    gate = jax.nn.softmax(best, axis=-1)
    nb = T // PEER_BLK

    def one(args):
        xb, ib, gb = args
        act = jax.nn.gelu(jnp.einsum('td,thkd->thk', xb, u_tab[ib]), approximate=False) * gb.astype(xb.dtype)
        return jnp.einsum('thk,thkd->td', act, v_tab[ib])

    out = lax.map(one, (xt.reshape(nb, PEER_BLK, D),
                        eidx.reshape(nb, PEER_BLK, PEER_HEADS, PEER_TOPK),
                        gate.reshape(nb, PEER_BLK, PEER_HEADS, PEER_TOPK)))
    return out.reshape(B, L, D)


def setup_inputs(seed: int = 0) -> dict:
    key = jax.random.key(seed)
    ks = jax.random.split(key, 28)
    D = D_MODEL

    def nrm(k, shape, scale):
        return jax.random.normal(k, shape, jnp.float32) * scale

    return {
        'x': nrm(ks[0], (BATCH, SEQ, D), 1.0),
        'c': nrm(ks[1], (BATCH, D), 1.0),
        'ctx': nrm(ks[2], (BATCH, CTX_LEN, D), 1.0),
        'c_ctx': nrm(ks[3], (D,), 1.0),
        'w_ada': nrm(ks[4], (DEPTH, D, N_ADA * D), 0.2 * D ** -0.5),
        'b_ada': nrm(ks[5], (DEPTH, N_ADA * D), 0.02),
        'g_pre_mix': 1.0 + nrm(ks[6], (DEPTH, D), 0.02),
        'g_post_mix': 1.0 + nrm(ks[7], (DEPTH, D), 0.02),
        'g_pre_ffn': 1.0 + nrm(ks[8], (DEPTH, D), 0.02),
        'g_post_ffn': 1.0 + nrm(ks[9], (DEPTH, D), 0.02),
        'w_in': nrm(ks[10], (DEPTH, D, IN_W), D ** -0.5),
        'b_gate': nrm(ks[11], (DEPTH, GATE_W), 0.02),
        'lambda_q1': nrm(ks[12], (DEPTH, DIFF_DH), 0.1),
        'lambda_k1': nrm(ks[13], (DEPTH, DIFF_DH), 0.1),
        'lambda_q2': nrm(ks[14], (DEPTH, DIFF_DH), 0.1),
        'lambda_k2': nrm(ks[15], (DEPTH, DIFF_DH), 0.1),
        'subln_g': 1.0 + nrm(ks[16], (DEPTH, 2 * DIFF_DH), 0.02),
        'sgu_ln_g': 1.0 + nrm(ks[17], (DEPTH, SGU_W), 0.02),
        'sgu_ln_b': nrm(ks[18], (DEPTH, SGU_W), 0.02),
        'sgu_w': nrm(ks[19], (DEPTH, SGU_GROUPS, SGU_CHUNK, SGU_CHUNK), SGU_CHUNK ** -0.5),
        'sgu_b': 1.0 + nrm(ks[20], (DEPTH, SGU_GROUPS, SGU_CHUNK), 0.02),
        'w_branch_attn': nrm(ks[21], (DEPTH, ATTN_OUT, D), ATTN_OUT ** -0.5),
        'w_branch_sgu': nrm(ks[22], (DEPTH, SGU_W, D), SGU_W ** -0.5),
        'w_out': nrm(ks[23], (DEPTH, D, D), D ** -0.5),
        'peer_w_query': nrm(ks[24], (DEPTH, D, PEER_HEADS * PEER_Q_DIM), D ** -0.5),
        'peer_sub_keys': nrm(ks[25], (DEPTH, 2, PEER_N_KEYS, PEER_PK_DIM), PEER_PK_DIM ** -0.5),
        'peer_u': nrm(ks[26], (DEPTH, PEER_N_EXPERTS, D), D ** -0.5),
        'peer_v': nrm(ks[27], (DEPTH, PEER_N_EXPERTS, D), D ** -0.5),
    }


def reference(x, c, ctx, c_ctx, w_ada, b_ada, g_pre_mix, g_post_mix, g_pre_ffn, g_post_ffn,
              w_in, b_gate, lambda_q1, lambda_k1, lambda_q2, lambda_k2, subln_g,
              sgu_ln_g, sgu_ln_b, sgu_w, sgu_b, w_branch_attn, w_branch_sgu, w_out,
              peer_w_query, peer_sub_keys, peer_u, peer_v):
    B, L, _ = x.shape
    Lc = ctx.shape[1]
    n_rows = L // GRID_W
    rows = jnp.repeat(jnp.arange(n_rows, dtype=jnp.int32), GRID_W)
    cols = jnp.tile(jnp.arange(GRID_W, dtype=jnp.int32), n_rows)

    for l in range(DEPTH):
        last = l == DEPTH - 1
        lam_init = 0.8 - 0.6 * math.exp(-0.3 * l)
        lam = (jnp.exp(jnp.dot(lambda_q1[l].astype(jnp.float32), lambda_k1[l].astype(jnp.float32)))
               - jnp.exp(jnp.dot(lambda_q2[l].astype(jnp.float32), lambda_k2[l].astype(jnp.float32)))
               + lam_init)
        mod = jax.nn.silu(c) @ w_ada[l] + b_ada[l]
        sh1, sc1, gt1, sh2, sc2, gt2 = [m[:, None, :] for m in jnp.split(mod, N_ADA, axis=-1)]
        mod_c = jax.nn.silu(c_ctx) @ w_ada[l] + b_ada[l]
        sh1c, sc1c, gt1c, sh2c, sc2c, gt2c = jnp.split(mod_c, N_ADA, axis=-1)

        h = modulate(rmsnorm(x, g_pre_mix[l]), sh1, sc1)
        hc = modulate(rmsnorm(ctx, g_pre_mix[l]), sh1c, sc1c)
        q, k, v, z, gl = split_proj(h @ w_in[l])
        q = axial_rope(q, rows, cols)
        k = axial_rope(k, rows, cols)
        if last:
            kv_c = hc @ w_in[l][:, Q_W:Q_W + K_W + V_W]
            kc = kv_c[..., :K_W].reshape(B, Lc, 2 * DIFF_HEADS, DIFF_DH)
            vc = kv_c[..., K_W:].reshape(B, Lc, DIFF_HEADS, 2 * DIFF_DH)
        else:
            qc, kc, vc, zc, glc = split_proj(hc @ w_in[l])
        k_all = jnp.concatenate([kc, k], axis=1)
        v_all = jnp.concatenate([vc, v], axis=1)
        y = mix_branches(q, k_all, v_all, z, gl + b_gate[l], lam, lam_init, subln_g[l],
                         sgu_ln_g[l], sgu_ln_b[l], sgu_w[l], sgu_b[l],
                         w_branch_attn[l], w_branch_sgu[l], w_out[l])
        x_new = x + gt1 * rmsnorm(y, g_post_mix[l])

        if not last:
            yc = mix_branches(qc, kc, vc, zc, glc + b_gate[l], lam, lam_init, subln_g[l],
                              sgu_ln_g[l], sgu_ln_b[l], sgu_w[l], sgu_b[l],
                              w_branch_attn[l], w_branch_sgu[l], w_out[l])
            ctx_new = ctx + gt1c * rmsnorm(yc, g_post_mix[l])
            h2c = modulate(rmsnorm(ctx_new, g_pre_ffn[l]), sh2c, sc2c)
            ctx = ctx_new + gt2c * rmsnorm(peer(h2c, peer_w_query[l], peer_sub_keys[l], peer_u[l], peer_v[l]), g_post_ffn[l])

        h2 = modulate(rmsnorm(x_new, g_pre_ffn[l]), sh2, sc2)
        x = x_new + gt2 * rmsnorm(peer(h2, peer_w_query[l], peer_sub_keys[l], peer_u[l], peer_v[l]), g_post_ffn[l])
    return x
```

```python
import contextlib
import math
import numpy as np
import concourse.bass as bass
import concourse.mybir as mybir
from concourse.bass_utils import run_bass_kernel_spmd

F32 = mybir.dt.float32
BF16 = mybir.dt.bfloat16
ALU = mybir.AluOpType
AF = mybir.ActivationFunctionType
AX = mybir.AxisListType

D = 1024
L = 8192
LC = 256
LK = L + LC
NQ = 4096
NE = 16384
RMS_EPS = 1e-6
LAM_INIT = 0.8 - 0.6 * math.exp(-0.3 * 0)
NEG = -1.0e30

ENGS = ("pe", "act", "dve", "pool", "sp")
DMA_POOL = {"sp": 28, "act": 8, "pool": 20}
SAME_ENGINE_SYNC = {"pe": False, "act": True, "dve": True, "pool": True, "sp": False}


class Buf:
    __slots__ = ("name", "last_write", "readers")

    def __init__(self, name):
        self.name = name
        self.last_write = None
        self.readers = []


class T:
    __slots__ = ("ap", "buf")

    def __init__(self, ap, buf):
        self.ap = ap
        self.buf = buf if isinstance(buf, Buf) else Buf(buf)

    def __getitem__(self, key):
        return self.ap[key]


def _bufs(xs):
    out = []
    for x in xs:
        if x is None:
            continue
        out.append(x.buf if isinstance(x, T) else x)
    return out


class Prog:
    def __init__(self, nc):
        self.nc = nc
        self.streams = {e: [] for e in ENGS}
        self.cnt = {e: 0 for e in ENGS}
        self.seen = {e: {} for e in ENGS}
        self.dma_i = {q: 0 for q in DMA_POOL}
        self.signal = {e: set() for e in ENGS}

    def _need(self, eng, deps):
        need = {}
        for d in deps:
            if d is None:
                continue
            key, val, src = d
            if src == eng and not SAME_ENGINE_SYNC[eng]:
                continue
            if self.seen[eng].get(key, 0) >= val:
                continue
            if need.get(key, 0) < val:
                need[key] = val
        for key, val in need.items():
            self.seen[eng][key] = val
            self.streams[eng].append(("wait", key, val))
            if key[0] == "eng":
                self.signal[key[1]].add(val)

    @staticmethod
    def _deps(reads, writes):
        deps = []
        for r in reads:
            deps.append(r.last_write)
        for w in writes:
            deps.append(w.last_write)
            deps.extend(w.readers)
        return deps

    @staticmethod
    def _commit(tok, reads, writes):
        for r in reads:
            r.readers.append(tok)
            if len(r.readers) > 64:
                best = {}
                for t in r.readers:
                    if best.get(t[0], (0,))[0] < t[1]:
                        best[t[0]] = (t[1], t[2])
                r.readers = [(k, v[0], v[1]) for k, v in best.items()]
        for w in writes:
            w.last_write = tok
            w.readers = []

    def op(self, eng, fn, reads=(), writes=()):
        reads = _bufs(reads)
        writes = _bufs(writes)
        self._need(eng, self._deps(reads, writes))
        self.cnt[eng] += 1
        tok = (("eng", eng), self.cnt[eng], eng)
        self.streams[eng].append(("op", fn, ("eng", eng), self.cnt[eng]))
        self._commit(tok, reads, writes)
        return tok

    def dma(self, q, out, in_, reads=(), writes=(), **kw):
        reads = _bufs(reads)
        writes = _bufs(writes)
        n = DMA_POOL[q]
        i = self.dma_i[q]
        self.dma_i[q] += 1
        slot, gen = i % n, i // n
        key = ("dma", q, slot)
        deps = self._deps(reads, writes)
        if gen > 0:
            deps.append((key, 16 * gen, None))
        self._need(q, deps)
        tok = (key, 16 * (gen + 1), None)
        self.streams[q].append(("dma", lambda e: e.dma_start(out=out, in_=in_, **kw), key, 16))
        self._commit(tok, reads, writes)
        return tok

    def barrier(self):
        toks = [(("eng", e), self.cnt[e], None) for e in ENGS if self.cnt[e] > 0]
        for q, n in DMA_POOL.items():
            i = self.dma_i[q]
            for slot in range(n):
                g = (i - slot + n - 1) // n if i > slot else 0
                if g > 0:
                    toks.append((("dma", q, slot), 16 * g, None))
        for e in ENGS:
            self._need(e, [t for t in toks if t[0] != ("eng", e)])

    def emit(self):
        nc = self.nc
        with contextlib.ExitStack() as es:
            sems = {}
            for e in ENGS:
                sems[("eng", e)] = es.enter_context(nc.semaphore("s_" + e))
            for q, n in DMA_POOL.items():
                for s in range(n):
                    sems[("dma", q, s)] = es.enter_context(nc.semaphore(f"d_{q}{s}"))
            block = es.enter_context(nc.Block())

            semval = {}
            for en in ENGS:
                c = 0
                m = {}
                for item in self.streams[en]:
                    if item[0] == "op" and item[3] in self.signal[en]:
                        c += 1
                        m[item[3]] = c
                semval[en] = m

            def run(engname):
                def body(e):
                    for item in self.streams[engname]:
                        if item[0] == "wait":
                            key, val = item[1], item[2]
                            if key[0] == "eng":
                                val = semval[key[1]][val]
                            e.wait_ge(sems[key], val)
                        elif item[0] == "dma":
                            _, fn, key, inc = item
                            fn(e).then_inc(sems[key], inc)
                        else:
                            _, fn, key, idx = item
                            ins = fn(e)
                            if idx in self.signal[engname]:
                                ins.then_inc(sems[key], 1)
                return body

            block.sync(run("sp"))
            block.scalar(run("act"))
            block.vector(run("dve"))
            block.gpsimd(run("pool"))
            block.tensor(run("pe"))


class Arena:
    def __init__(self, tensor, size, name):
        self.t = tensor
        self.size = size
        self.off = 0
        self.name = name
        self.n = 0

    def alloc(self, n, name=None):
        assert self.off + n <= self.size, f"arena {self.name} overflow: {self.off}+{n}>{self.size} ({name})"
        ap = self.t[:, self.off:self.off + n]
        self.off += n
        self.n += 1
        return T(ap, Buf(f"{self.name}_{name or self.n}"))

    def mark(self):
        return self.off

    def reset(self, m):
        self.off = m


def build_program(dbg=None, phases="ABCEF"):
    nc = bass.Bass("TRN2", target_bir_lowering=False)
    P = Prog(nc)

    def din(name, shape, dt=F32):
        return nc.dram_tensor(name, list(shape), dt, kind="ExternalInput").ap()

    def dscr(name, shape, dt):
        return T(nc.dram_tensor(name, list(shape), dt, kind="Internal").ap(), name)

    xb = din("xb", [L, D])
    xown = din("xown", [NQ, D])
    ctxb = din("ctxb", [LC, D])
    cvec = din("cvec", [2, D])
    w_ada = din("w_ada", [D, 6 * D])
    b_ada = din("b_ada", [1, 6 * D])
    g_pre_mix = din("g_pre_mix", [1, D])
    g_post_mix = din("g_post_mix", [1, D])
    g_pre_ffn = din("g_pre_ffn", [1, D])
    g_post_ffn = din("g_post_ffn", [1, D])
    w_in = din("w_in", [D, 7 * D])
    w_qk_sw = din("w_qk_sw", [D, 2 * D])
    b_gate = din("b_gate", [1, 2 * D])
    lam_vecs = din("lam_vecs", [1, 256])
    subln_g = din("subln_g", [1, 128])
    sgu_ln_g = din("sgu_ln_g", [1, D])
    sgu_ln_b = din("sgu_ln_b", [1, D])
    sgu_wT = din("sgu_wT", [8, 128, 128])
    sgu_b = din("sgu_b", [8, 128])
    w_branch_attn = din("w_branch_attn", [D, D])
    w_branch_sgu = din("w_branch_sgu", [D, D])
    w_out = din("w_out", [D, D])
    peer_wq = din("peer_wq", [D, 2 * D])
    peer_keysT = din("peer_keysT", [2, 128, 128])
    peer_u = din("peer_u", [NE, D])
    peer_v = din("peer_v", [NE, D])
    ropeC = din("ropeC", [128, L])
    ropeS = din("ropeS", [128, L])
    ropeCq = din("ropeCq", [128, NQ])
    ropeSq = din("ropeSq", [128, NQ])
    y_out = nc.dram_tensor("y", [NQ, D], F32, kind="ExternalOutput").ap()
    Y = T(y_out, "y_out")

    hT_all = dscr("hT_all", [D, LK], BF16)
    hT_own = dscr("hT_own", [D, NQ], BF16)
    KT = dscr("KT", [D, LK], BF16)
    Vs = dscr("Vs", [LK, D], BF16)
    QT = dscr("QT", [D, NQ], BF16)
    pST = dscr("pST", [D, NQ], BF16)
    gAT = dscr("gAT", [D, NQ], BF16)
    onT = dscr("onT", [D, NQ], BF16)
    XN = dscr("XN", [NQ, D], F32)
    H2T = dscr("H2T", [D, NQ], BF16)
    UT = dscr("UT", [128, 128, D], BF16)
    VB = dscr("VB", [NE, D], BF16)
    SS = dscr("SS", [NQ, 2 * D], F32)
    SM = dscr("SM", [NQ, 16], F32)

    dbg_outs = {}

    es = contextlib.ExitStack()
    with es:
        NF = 25 * 1024
        NB = 54784
        f_t = es.enter_context(nc.sbuf_tensor("arena_f", [128, NF], F32))
        b_t = es.enter_context(nc.sbuf_tensor("arena_b", [128, NB], BF16))
        AFa = Arena(f_t, NF, "f")
        ABa = Arena(b_t, NB, "b")
        pst = [es.enter_context(nc.psum_tensor(f"ps{i}", [128, 1024], F32)) for i in range(4)]
        PS = [T(pst[i // 2][:, (i % 2) * 512:(i % 2 + 1) * 512], f"psbank{i}") for i in range(8)]

        def ps2(i):
            return pst[i][:, :], [PS[2 * i].buf, PS[2 * i + 1].buf]

        def MM(out, lhsT, rhs, start, stop, reads, writes):
            P.op("pe", lambda e: e.matmul(out, lhsT=lhsT, rhs=rhs, start=start, stop=stop), reads, writes)

        def MMG(out, lhsT, rhs, start, stop, reads, writes):
            P.op("pe", lambda e: e.matmul(out, lhsT=lhsT, rhs=rhs, start=start, stop=stop, skip_group_check=True),
                 reads, writes)

        def TR(out, in_, reads, writes):
            MM(out, in_, ident_b.ap, True, True, list(reads) + [ident_b], writes)

        def ACT(out, in_, func, reads, writes, bias=None, scale=None, accum=None):
            kw = {}
            if bias is not None:
                kw["bias"] = bias
            if scale is not None:
                kw["scale"] = scale
            if accum is not None:
                kw["accum_out"] = accum
            P.op("act", lambda e: e.activation(out=out, in_=in_, func=func, **kw), reads, writes)

        def TT(eng, out, in0, in1, op, reads, writes):
            P.op(eng, lambda e: e.tensor_tensor(out=out, in0=in0, in1=in1, op=op), reads, writes)

        def TS(eng, out, in0, s1, s2, op0, op1, reads, writes):
            if s2 is None:
                P.op(eng, lambda e: e.tensor_scalar(out=out, in0=in0, scalar1=s1, scalar2=None, op0=op0), reads, writes)
            else:
                P.op(eng, lambda e: e.tensor_scalar(out=out, in0=in0, scalar1=s1, scalar2=s2, op0=op0, op1=op1), reads, writes)

        def STT(out, in0, scalar, in1, op0, op1, reads, writes):
            P.op("dve", lambda e: e.scalar_tensor_tensor(out=out, in0=in0, scalar=scalar, in1=in1, op0=op0, op1=op1),
                 reads, writes)

        def COPY(eng, out, in_, reads, writes):
            if eng == "act":
                P.op("act", lambda e: e.copy(out=out, in_=in_), reads, writes)
            else:
                P.op(eng, lambda e: e.tensor_copy(out=out, in_=in_), reads, writes)

        def RED(out, in_, op, reads, writes):
            P.op("dve", lambda e: e.tensor_reduce(out=out, in_=in_, axis=AX.X, op=op), reads, writes)

        def MAX8(out, in_, reads, writes):
            P.op("dve", lambda e: e.max(out=out, in_=in_), reads, writes)

        def MREP(out, rep, vals, reads, writes):
            P.op("dve", lambda e: e.match_replace(out=out, in_to_replace=rep, in_values=vals, imm_value=NEG), reads, writes)

        def RECIP(out, in_, reads, writes):
            P.op("dve", lambda e: e.reciprocal(out=out, in_=in_), reads, writes)

        def SQRT(out, in_, reads, writes):
            P.op("act", lambda e: e.sqrt(out=out, in_=in_), reads, writes)

        def MEMSET(eng, out, val, writes):
            P.op(eng, lambda e: e.memset(out, val), (), writes)

        def rstd(t, ss_ap, out_ap, scale, eps):
            TS("dve", out_ap, ss_ap, scale, eps, ALU.mult, ALU.add, [t], [t])
            SQRT(out_ap, out_ap, [t], [t])
            RECIP(out_ap, out_ap, [t], [t])

        iot = AFa.alloc(128, "iot")
        ident_f = AFa.alloc(128, "ident_f")
        ones_f = AFa.alloc(128, "ones_f")
        ident_b = ABa.alloc(128, "ident_b")
        ones_b = ABa.alloc(128, "ones_b")
        P.op("pool", lambda e: e.iota(iot.ap, pattern=[[1, 128]], base=0, channel_multiplier=-1,
                                      allow_small_or_imprecise_dtypes=True), (), [iot.buf])
        P.op("dve", lambda e: e.tensor_single_scalar(out=ident_f.ap, in_=iot.ap, scalar=0.0, op=ALU.is_equal),
             [iot.buf], [ident_f.buf])
        COPY("dve", ident_b.ap, ident_f.ap, [ident_f], [ident_b])
        MEMSET("dve", ones_f.ap, 1.0, [ones_f])
        MEMSET("dve", ones_b.ap, 1.0, [ones_b])

        modT = AFa.alloc(64, "modT")
        a1T = AFa.alloc(8, "a1T")
        a1cT = AFa.alloc(8, "a1cT")
        a2T = AFa.alloc(8, "a2T")
        G1 = AFa.alloc(D, "G1")
        G2 = AFa.alloc(D, "G2")
        neg_lam = AFa.alloc(1, "neg_lam")
        sgT = AFa.alloc(1, "sgT")
        bgT = AFa.alloc(16, "bgT")
        sbT = AFa.alloc(8, "sbT")
        smallf = AFa.alloc(64, "smallf")

        def b1T(dc):
            return modT.ap[:, 0 * 8 + dc:0 * 8 + dc + 1]

        def b1cT(dc):
            return modT.ap[:, 6 * 8 + dc:6 * 8 + dc + 1]

        def b2T(dc):
            return modT.ap[:, 3 * 8 + dc:3 * 8 + dc + 1]

        fmark, bmark = AFa.mark(), ABa.mark()

        def end_phase():
            P.barrier()
            AFa.reset(fmark)
            ABa.reset(bmark)

        def load_w_bf16(dst, src_ap, ncols, q="pool"):
            d3 = dst.ap.rearrange("p (k n) -> p k n", n=ncols)
            s3_ = src_ap.rearrange("(k p) n -> p k n", p=128)
            for dc in range(8):
                for c0 in range(0, ncols, 1024):
                    P.dma(q, d3[:, dc, c0:c0 + 1024], s3_[:, dc, c0:c0 + 1024], writes=[dst])
            return d3

        if "A" in phases:
            cT = AFa.alloc(16, "cT")
            srep = [AFa.alloc(1024, f"srep{r}") for r in range(2)]
            badac = [AFa.alloc(512, f"bada{i}") for i in range(2)]
            mod_bc = AFa.alloc(6 * D, "mod_bc")
            modc_bc = AFa.alloc(2 * D, "modc_bc")
            wa = [AFa.alloc(8 * 512, f"wa{i}") for i in range(2)]
            gtmp = [AFa.alloc(D, f"gtmp{i}") for i in range(2)]
            gpreT = AFa.alloc(8, "gpreT")
            gpfT = AFa.alloc(8, "gpfT")
            lv = AFa.alloc(256, "lv")

            cT3 = cT.ap.rearrange("p (k r) -> p k r", r=2)
            for r in range(2):
                P.dma("sp", cT3[:, :, r], cvec[r, :].rearrange("(k p) -> p k", p=128), writes=[cT],
                      allow_slow_non_contiguous=True)
            ACT(cT.ap, cT.ap, AF.Silu, [cT], [cT])
            for r in range(2):
                COPY("dve", srep[r].ap.rearrange("p (k m) -> p k m", m=128),
                     cT3[:, :, r:r + 1].to_broadcast([128, 8, 128]), [cT], [srep[r]])
            w_ada_v = w_ada.rearrange("(k p) n -> p k n", p=128)
            for cb in range(12):
                w = wa[cb % 2]
                w3 = w.ap.rearrange("p (k n) -> p k n", n=512)
                bd = badac[cb % 2]
                P.dma("sp" if cb % 2 == 0 else "act", w3, w_ada_v[:, :, cb * 512:(cb + 1) * 512], writes=[w])
                P.dma("sp", bd.ap, b_ada[:, cb * 512:(cb + 1) * 512].partition_broadcast(128), writes=[bd])
                for r in range(2):
                    if r == 1 and cb >= 4:
                        continue
                    sr3 = srep[r].ap.rearrange("p (k m) -> p k m", m=128)
                    pb = PS[(cb % 2) * 2 + r]
                    for kc in range(8):
                        MM(pb.ap, sr3[:, kc, :], w3[:, kc, :], kc == 0, kc == 7, [srep[r], w], [pb])
                    dst = mod_bc if r == 0 else modc_bc
                    TT("dve", dst.ap[:, cb * 512:(cb + 1) * 512], pb.ap, bd.ap, ALU.add, [pb, bd], [dst])
            for grp in range(16):
                pb = PS[4 + grp % 4]
                for k4 in range(4):
                    idx = grp * 4 + k4
                    v, fc = idx // 8, idx % 8
                    if v < 6:
                        src, col = mod_bc, v * 1024 + fc * 128
                    else:
                        src, col = modc_bc, (v - 6) * 1024 + fc * 128
                    MM(pb.ap[:, k4 * 128:(k4 + 1) * 128], src.ap[:, col:col + 128], ident_f.ap, True, True,
                       [src, ident_f], [pb])
                COPY("dve", modT.ap[:, grp * 4:(grp + 1) * 4], pb.ap[:, 0:512:128], [pb], [modT])
            P.dma("sp", gpreT.ap, g_pre_mix[0, :].rearrange("(k p) -> p k", p=128), writes=[gpreT],
                  allow_slow_non_contiguous=True)
            P.dma("sp", gpfT.ap, g_pre_ffn[0, :].rearrange("(k p) -> p k", p=128), writes=[gpfT],
                  allow_slow_non_contiguous=True)
            for hh in range(2):
                P.dma("sp", bgT.ap[:, hh * 8:(hh + 1) * 8], b_gate[0, hh * D:(hh + 1) * D].rearrange("(k p) -> p k", p=128),
                      writes=[bgT], allow_slow_non_contiguous=True)
            P.dma("sp", sbT.ap, sgu_b.rearrange("g p -> p g"), writes=[sbT], allow_slow_non_contiguous=True)
            P.dma("sp", sgT.ap, subln_g[0, :].rearrange("(p o) -> p o", o=1), writes=[sgT],
                  allow_slow_non_contiguous=True)
            TS("dve", sgT.ap, sgT.ap, (1.0 - LAM_INIT), None, ALU.mult, None, [sgT], [sgT])
            for (dst, scv, gT) in ((a1T, 1, gpreT), (a1cT, 7, gpreT), (a2T, 4, gpfT)):
                TS("dve", dst.ap, modT.ap[:, scv * 8:(scv + 1) * 8], 1.0, None, ALU.add, None, [modT], [dst])
                TT("dve", dst.ap, dst.ap, gT.ap, ALU.mult, [dst, gT], [dst])
            P.dma("sp", gtmp[0].ap, g_post_mix.partition_broadcast(128), writes=[gtmp[0]])
            TT("dve", G1.ap, mod_bc.ap[:, 2 * D:3 * D], gtmp[0].ap, ALU.mult, [mod_bc, gtmp[0]], [G1])
            P.dma("sp", gtmp[1].ap, g_post_ffn.partition_broadcast(128), writes=[gtmp[1]])
            TT("dve", G2.ap, mod_bc.ap[:, 5 * D:6 * D], gtmp[1].ap, ALU.mult, [mod_bc, gtmp[1]], [G2])
            P.dma("sp", lv.ap, lam_vecs.partition_broadcast(128), writes=[lv])
            for i in range(2):
                TT("dve", lv.ap[:, i * 128:i * 128 + 64], lv.ap[:, i * 128:i * 128 + 64],
                   lv.ap[:, i * 128 + 64:i * 128 + 128], ALU.mult, [lv], [lv])
                RED(smallf.ap[:, i:i + 1], lv.ap[:, i * 128:i * 128 + 64], ALU.add, [lv], [smallf])
            ACT(smallf.ap[:, 0:2], smallf.ap[:, 0:2], AF.Exp, [smallf], [smallf])
            STT(neg_lam.ap, smallf.ap[:, 1:2], -LAM_INIT, smallf.ap[:, 0:1], ALU.add, ALU.subtract, [smallf], [neg_lam])
            if dbg:
                dbg_outs["modT"] = (modT, 64)
                dbg_outs["G1"] = (G1, D)
                dbg_outs["neg_lam"] = (neg_lam, 1)
            end_phase()

        def norm_transpose(src, ntok, aT, bTf, dst, blk_tiles):
            xt = [AFa.alloc(D, f"xt{i}") for i in range(2)]
            junk = ABa.alloc(D, "junk")
            xn = [ABa.alloc(D, f"xn{i}") for i in range(2)]
            hblk = [ABa.alloc(8 * 128 * blk_tiles, f"hblk{i}") for i in range(2)]
            ss = [AFa.alloc(2, f"ss{i}") for i in range(2)]
            ntiles = ntok // 128
            dst_v = dst.ap.rearrange("(k p) t -> p k t", p=128)
            for ti in range(ntiles):
                x_ = xt[ti % 2]
                s_ = ss[ti % 2]
                n_ = xn[ti % 2]
                bi = ti // blk_tiles
                hb = hblk[bi % 2]
                hb3 = hb.ap.rearrange("p (k t) -> p k t", k=8)
                tl = ti % blk_tiles
                P.dma("sp", x_.ap, src[ti * 128:(ti + 1) * 128, :], writes=[x_])
                ACT(junk.ap, x_.ap, AF.Square, [x_], [junk, s_], accum=s_.ap[:, 0:1])
                rstd(s_, s_.ap[:, 0:1], s_.ap[:, 1:2], 1.0 / D, RMS_EPS)
                TS("dve", n_.ap, x_.ap, s_.ap[:, 1:2], None, ALU.mult, None, [x_, s_], [n_])
                pa, pbufs = ps2(ti % 2)
                for dc in range(8):
                    TR(pa[:, dc * 128:(dc + 1) * 128], n_.ap[:, dc * 128:(dc + 1) * 128], [n_], [pbufs[dc // 4]])
                for dc in range(8):
                    o = hb3[:, dc, tl * 128:(tl + 1) * 128]
                    i_ = pa[:, dc * 128:(dc + 1) * 128]
                    if dc // 4 == 0:
                        ACT(o, i_, AF.Identity, [pbufs[dc // 4], aT, modT], [hb], bias=bTf(dc), scale=aT.ap[:, dc:dc + 1])
                    else:
                        TS("dve", o, i_, aT.ap[:, dc:dc + 1], bTf(dc), ALU.mult, ALU.add, [pbufs[dc // 4], aT, modT], [hb])
                if tl == blk_tiles - 1:
                    t0 = bi * blk_tiles * 128
                    P.dma("pool", dst_v[:, :, t0:t0 + blk_tiles * 128], hb3, reads=[hb], writes=[dst])
            end_phase()

        if "B" in phases:
            norm_transpose(ctxb, LC, a1cT, b1cT, T(hT_all.ap[:, 0:LC], hT_all.buf), 2)
            norm_transpose(xb, L, a1T, b1T, T(hT_all.ap[:, LC:LK], hT_all.buf), 4)
            norm_transpose(xown, NQ, a1T, b1T, hT_own, 4)

        if "B" in phases:
            Wq = ABa.alloc(8 * D, "Wq")
            Wqs = ABa.alloc(8 * D, "Wqs")
            Wk = ABa.alloc(8 * D, "Wk")
            Wks = ABa.alloc(8 * D, "Wks")
            Wv = ABa.alloc(8 * D, "Wv")
            Wk3 = load_w_bf16(Wk, w_in[:, D:2 * D], D)
            Wks3 = load_w_bf16(Wks, w_qk_sw[:, D:2 * D], D)
            Wv3 = load_w_bf16(Wv, w_in[:, 2 * D:3 * D], D)
            Wq3 = load_w_bf16(Wq, w_in[:, 0:D], D)
            Wqs3 = load_w_bf16(Wqs, w_qk_sw[:, 0:D], D)
            hblk = [ABa.alloc(8 * 512, f"hb{i}") for i in range(2)]
            Cb = [AFa.alloc(512, f"Cb{i}") for i in range(2)]
            Sb = [AFa.alloc(512, f"Sb{i}") for i in range(2)]
            t1 = [AFa.alloc(512, f"t1_{i}") for i in range(2)]
            t2 = [AFa.alloc(512, f"t2_{i}") for i in range(2)]
            ko = [ABa.alloc(512, f"ko{i}") for i in range(3)]
            vo = [ABa.alloc(D, f"vo{i}") for i in range(2)]

            def qk_block(bi, hsrc, t0, N, W3, Ws3, Wt, Wst, Ctab, Stab, c0, dst, dcol, rope):
                hb = hblk[bi % 2]
                hb3 = hb.ap.rearrange("p (k t) -> p k t", k=8)
                hsrc_v = hsrc.ap.rearrange("(k p) t -> p k t", p=128)
                P.dma("sp", hb3[:, :, 0:N], hsrc_v[:, :, t0:t0 + N], reads=[hsrc], writes=[hb])
                C_ = Cb[bi % 2]
                S_ = Sb[bi % 2]
                if rope:
                    P.dma("act", C_.ap[:, 0:N], Ctab[:, c0:c0 + N], writes=[C_])
                    P.dma("act", S_.ap[:, 0:N], Stab[:, c0:c0 + N], writes=[S_])
                for fc in range(8):
                    pA = PS[(fc % 2) * 2]
                    pB = PS[(fc % 2) * 2 + 1]
                    for dc in range(8):
                        MM(pA.ap[:, 0:N], W3[:, dc, fc * 128:(fc + 1) * 128], hb3[:, dc, 0:N], dc == 0, dc == 7, [Wt, hb], [pA])
                    k_ = ko[fc % 3]
                    if rope:
                        for dc in range(8):
                            MM(pB.ap[:, 0:N], Ws3[:, dc, fc * 128:(fc + 1) * 128], hb3[:, dc, 0:N], dc == 0, dc == 7,
                               [Wst, hb], [pB])
                        a_ = t1[fc % 2]
                        b_ = t2[fc % 2]
                        TT("dve", a_.ap[:, 0:N], pA.ap[:, 0:N], C_.ap[:, 0:N], ALU.mult, [pA, C_], [a_])
                        TT("dve", b_.ap[:, 0:N], pB.ap[:, 0:N], S_.ap[:, 0:N], ALU.mult, [pB, S_], [b_])
                        TT("pool", k_.ap[:, 0:N], a_.ap[:, 0:N], b_.ap[:, 0:N], ALU.add, [a_, b_], [k_])
                    else:
                        COPY("act", k_.ap[:, 0:N], pA.ap[:, 0:N], [pA], [k_])
                    P.dma("pool", dst.ap[fc * 128:(fc + 1) * 128, dcol:dcol + N], k_.ap[:, 0:N], reads=[k_], writes=[dst])
                return hb, hb3

            def v_block(hb, hb3, N, tok0):
                for tl in range(N // 128):
                    v_ = vo[tl % 2]
                    for half in range(2):
                        pV = PS[4 + (tl * 2 + half) % 4]
                        for dc in range(8):
                            MM(pV.ap, hb3[:, dc, tl * 128:(tl + 1) * 128], Wv3[:, dc, half * 512:(half + 1) * 512],
                               dc == 0, dc == 7, [Wv, hb], [pV])
                        COPY("act", v_.ap[:, half * 512:(half + 1) * 512], pV.ap, [pV], [v_])
                    P.dma("pool", Vs.ap[tok0 + tl * 128:tok0 + (tl + 1) * 128, :], v_.ap, reads=[v_], writes=[Vs])

            hb, hb3 = qk_block(0, hT_all, 0, LC, Wk3, Wks3, Wk, Wks, None, None, 0, KT, 0, False)
            v_block(hb, hb3, LC, 0)
            for bi in range(16):
                hb, hb3 = qk_block(bi + 1, hT_all, LC + bi * 512, 512, Wk3, Wks3, Wk, Wks, ropeC, ropeS, bi * 512,
                                   KT, LC + bi * 512, True)
                v_block(hb, hb3, 512, LC + bi * 512)
            for bi in range(8):
                qk_block(bi, hT_own, bi * 512, 512, Wq3, Wqs3, Wq, Wqs, ropeCq, ropeSq, bi * 512, QT, bi * 512, True)
            end_phase()

        if "B" in phases:
            Wz = ABa.alloc(8 * 2 * D, "Wz")
            Wgl = ABa.alloc(8 * 2 * D, "Wgl")
            Wbs = ABa.alloc(8 * D, "Wbs")
            wsT = ABa.alloc(8 * 128, "wsT")
            Wz3 = load_w_bf16(Wz, w_in[:, 3 * D:5 * D], 2 * D)
            Wgl3 = load_w_bf16(Wgl, w_in[:, 5 * D:7 * D], 2 * D)
            Wbs3 = load_w_bf16(Wbs, w_branch_sgu, D)
            wsT3 = wsT.ap.rearrange("q (g p) -> q g p", g=8)
            for g in range(8):
                P.dma("pool", wsT3[:, g, :], sgu_wT[g, :, :], writes=[wsT])
            lng = AFa.alloc(D, "lng")
            lnb = AFa.alloc(D, "lnb")
            P.dma("sp", lng.ap, sgu_ln_g.partition_broadcast(128), writes=[lng])
            P.dma("sp", lnb.ap, sgu_ln_b.partition_broadcast(128), writes=[lnb])
            hb = ABa.alloc(8 * 512, "hb")
            hb3 = hb.ap.rearrange("p (k t) -> p k t", k=8)
            sT_ = ABa.alloc(8 * 512, "sgT")
            sT3 = sT_.ap.rearrange("p (k t) -> p k t", k=8)
            zg = [AFa.alloc(2 * D, f"zg{i}") for i in range(2)]
            vn = AFa.alloc(D, "vn")
            vb_ = ABa.alloc(D, "vnb")
            sgb = [ABa.alloc(D, f"sgb{i}") for i in range(2)]
            junkf = AFa.alloc(D, "junkf")
            st = [AFa.alloc(8, f"st{i}") for i in range(2)]
            gs = [AFa.alloc(512, f"gs{i}") for i in range(2)]
            po = [ABa.alloc(512, f"po{i}") for i in range(2)]
            hsrc_v = hT_own.ap.rearrange("(k p) t -> p k t", p=128)
            npo = 0
            for bi in range(8):
                P.dma("sp", hb3, hsrc_v[:, :, bi * 512:(bi + 1) * 512], reads=[hT_own], writes=[hb])
                for tl in range(4):
                    z_ = zg[tl % 2]
                    s_ = st[tl % 2]
                    for cb in range(4):
                        pz = PS[cb]
                        for dc in range(8):
                            MM(pz.ap, hb3[:, dc, tl * 128:(tl + 1) * 128], Wz3[:, dc, cb * 512:(cb + 1) * 512],
                               dc == 0, dc == 7, [hb, Wz], [pz])
                        ACT(z_.ap[:, cb * 512:(cb + 1) * 512], pz.ap, AF.Gelu, [pz], [z_])
                    v_ap = z_.ap[:, D:2 * D]
                    RED(s_.ap[:, 0:1], v_ap, ALU.add, [z_], [s_])
                    ACT(junkf.ap, v_ap, AF.Square, [z_], [junkf, s_], accum=s_.ap[:, 1:2])
                    TS("dve", s_.ap[:, 2:3], s_.ap[:, 0:1], 1.0 / D, None, ALU.mult, None, [s_], [s_])
                    TT("dve", s_.ap[:, 3:4], s_.ap[:, 2:3], s_.ap[:, 2:3], ALU.mult, [s_], [s_])
                    STT(s_.ap[:, 4:5], s_.ap[:, 1:2], 1.0 / D, s_.ap[:, 3:4], ALU.mult, ALU.subtract, [s_], [s_])
                    rstd(s_, s_.ap[:, 4:5], s_.ap[:, 5:6], 1.0, 1e-5)
                    TS("dve", vn.ap, v_ap, s_.ap[:, 2:3], s_.ap[:, 5:6], ALU.subtract, ALU.mult, [z_, s_], [vn])
                    TT("pool", vn.ap, vn.ap, lng.ap, ALU.mult, [vn, lng], [vn])
                    TT("pool", vb_.ap, vn.ap, lnb.ap, ALU.add, [vn, lnb], [vb_])
                    pm, pmb = ps2(2)
                    for g in range(8):
                        MM(pm[:, g * 128:(g + 1) * 128], wsT3[:, g, :], vb_.ap[:, g * 128:(g + 1) * 128], True, True,
                           [wsT, vb_], [pmb[g // 4]])
                    sg_ = sgb[tl % 2]
                    for g in range(8):
                        STT(sg_.ap[:, g * 128:(g + 1) * 128], pm[:, g * 128:(g + 1) * 128], sbT.ap[:, g:g + 1],
                            z_.ap[:, g * 128:(g + 1) * 128], ALU.add, ALU.mult, [pmb[g // 4], sbT, z_], [sg_])
                    pt, ptb = ps2(3)
                    for fc in range(8):
                        TR(pt[:, fc * 128:(fc + 1) * 128], sg_.ap[:, fc * 128:(fc + 1) * 128], [sg_], [ptb[fc // 4]])
                    for hf in range(2):
                        COPY("act", sT3[:, hf * 4:(hf + 1) * 4, tl * 128:(tl + 1) * 128],
                             pt[:, hf * 512:(hf + 1) * 512].rearrange("p (k t) -> p k t", k=4), [ptb[hf]], [sT_])
                for fc in range(8):
                    pY = PS[0 + fc % 2]
                    pG = PS[2 + fc % 2]
                    pH = PS[4 + fc % 2]
                    for dc in range(8):
                        MM(pY.ap, Wbs3[:, dc, fc * 128:(fc + 1) * 128], sT3[:, dc, :], dc == 0, dc == 7, [Wbs, sT_], [pY])
                    for dc in range(8):
                        MM(pG.ap, Wgl3[:, dc, D + fc * 128:D + (fc + 1) * 128], hb3[:, dc, :], dc == 0, dc == 7, [Wgl, hb], [pG])
                    for dc in range(8):
                        MM(pH.ap, Wgl3[:, dc, fc * 128:(fc + 1) * 128], hb3[:, dc, :], dc == 0, dc == 7, [Wgl, hb], [pH])
                    g_ = gs[fc % 2]
                    ACT(g_.ap, pG.ap, AF.Sigmoid, [pG, bgT], [g_], bias=bgT.ap[:, 8 + fc:9 + fc])
                    o1 = po[npo % 2]
                    npo += 1
                    TT("dve", o1.ap, pY.ap, g_.ap, ALU.mult, [pY, g_], [o1])
                    P.dma("pool", pST.ap[fc * 128:(fc + 1) * 128, bi * 512:(bi + 1) * 512], o1.ap, reads=[o1], writes=[pST])
                    o2 = po[npo % 2]
                    npo += 1
                    ACT(o2.ap, pH.ap, AF.Sigmoid, [pH, bgT], [o2], bias=bgT.ap[:, fc:fc + 1])
                    P.dma("pool", gAT.ap[fc * 128:(fc + 1) * 128, bi * 512:(bi + 1) * 512], o2.ap, reads=[o2], writes=[gAT])
            end_phase()

        if "C" in phases:
            KTab = ABa.alloc(LK, "KTab")
            QTa = ABa.alloc(NQ, "QTa")
            QTb = ABa.alloc(NQ, "QTb")
            Vh = ABa.alloc(66 * 128, "Vh")
            Vh3 = Vh.ap.rearrange("p (k e) -> p k e", e=128)
            PT = [ABa.alloc(512, f"PT{i}") for i in range(6)]
            ono = [ABa.alloc(512, f"ono{i}") for i in range(2)]
            r_ = [AFa.alloc(512, f"r{i}") for i in range(2)]
            tt = [AFa.alloc(512, f"tt{i}") for i in range(2)]
            zacc = [AFa.alloc(512, f"zacc{i}") for i in range(2)]
            zacp = [AFa.alloc(512, f"zacp{i}") for i in range(2)]
            o_ = AFa.alloc(512, "o_")
            sq = AFa.alloc(512, "sq")
            rs = AFa.alloc(512, "rs")
            Vs_v = Vs.ap.rearrange("(kt k) e -> k kt e", k=128)
            NKT = LK // 128
            Ops = (PS[3], PS[4])
            Zps = (PS[5], PS[6])
            MEMSET("dve", QTa.ap[64:128, :], 0.0, [QTa])
            MEMSET("dve", QTb.ap[0:64, :], 0.0, [QTb])
            for h in range(8):
                P.dma("sp", KTab.ap, KT.ap[h * 128:(h + 1) * 128, :], reads=[KT], writes=[KTab])
                P.dma("act", QTa.ap[0:64, :], QT.ap[(2 * h) * 64:(2 * h + 1) * 64, :], reads=[QT], writes=[QTa])
                P.dma("act", QTb.ap[64:128, :], QT.ap[(2 * h + 1) * 64:(2 * h + 2) * 64, :], reads=[QT], writes=[QTb])
                for kq in range(6):
                    P.dma("sp", Vh3[:, kq * 11:(kq + 1) * 11, :], Vs_v[:, kq * 11:(kq + 1) * 11, h * 128:(h + 1) * 128],
                          reads=[Vs], writes=[Vh])
                for qb in range(8):
                    steps = [(j, kt) for j in range(2) for kt in range(NKT)]
                    LOOK = 2

                    def av(n):
                        j, kt = steps[n]
                        p_ = PT[n % 6]
                        MM(Ops[j].ap, Vh3[:, kt, :], p_.ap, kt == 0, kt == NKT - 1, [Vh, p_], [Ops[j]])
                        if kt % 3 == 2:
                            MM(Zps[j].ap, ones_b.ap, p_.ap, kt == 2, False, [ones_b, p_], [Zps[j]])
                        if kt == NKT - 1:
                            MM(Zps[j].ap, ones_f.ap, zacc[j].ap, False, True, [ones_f, zacc[j]], [Zps[j]])

                    for n in range(len(steps) + LOOK):
                        if n < len(steps):
                            j, kt = steps[n]
                            Qt_ = QTa if j == 0 else QTb
                            sp_ = PS[n % 3]
                            p_ = PT[n % 6]
                            MM(sp_.ap, KTab.ap[:, kt * 128:(kt + 1) * 128], Qt_.ap[:, qb * 512:(qb + 1) * 512], True, True,
                               [KTab, Qt_], [sp_])
                            ACT(p_.ap, sp_.ap, AF.Exp, [sp_], [p_], scale=0.125)
                            if kt % 3 == 2:
                                pass
                            elif kt == 0:
                                COPY("dve", zacc[j].ap, p_.ap, [p_], [zacc[j]])
                            else:
                                TT("dve", zacc[j].ap, zacc[j].ap, p_.ap, ALU.add, [zacc[j], p_], [zacc[j]])
                        if n >= LOOK:
                            av(n - LOOK)
                    for j in range(2):
                        RECIP(r_[j].ap, Zps[j].ap, [Zps[j]], [r_[j]])
                        TT("dve", tt[j].ap, Ops[j].ap, r_[j].ap, ALU.mult, [Ops[j], r_[j]], [tt[j]])
                    STT(o_.ap, tt[1].ap, neg_lam.ap[:, 0:1], tt[0].ap, ALU.mult, ALU.add, [tt[0], tt[1], neg_lam], [o_])
                    TT("pool", sq.ap, o_.ap, o_.ap, ALU.mult, [o_], [sq])
                    MM(PS[7].ap, ones_f.ap, sq.ap, True, True, [ones_f, sq], [PS[7]])
                    TS("dve", rs.ap, PS[7].ap, 1.0 / 128, RMS_EPS, ALU.mult, ALU.add, [PS[7]], [rs])
                    SQRT(rs.ap, rs.ap, [rs], [rs])
                    RECIP(rs.ap, rs.ap, [rs], [rs])
                    oo = ono[qb % 2]
                    STT(oo.ap, o_.ap, sgT.ap[:, 0:1], rs.ap, ALU.mult, ALU.mult, [o_, sgT, rs], [oo])
                    P.dma("pool", onT.ap[h * 128:(h + 1) * 128, qb * 512:(qb + 1) * 512], oo.ap, reads=[oo], writes=[onT])
            end_phase()

        if "E" in phases:
            Wba = ABa.alloc(8 * D, "Wba")
            Wo = ABa.alloc(8 * D, "Wo")
            Wba3 = load_w_bf16(Wba, w_branch_attn, D)
            Wo3 = load_w_bf16(Wo, w_out, D)
            onb = [ABa.alloc(8 * 512, f"onb{i}") for i in range(2)]
            gab = [ABa.alloc(8 * 512, f"gab{i}") for i in range(2)]
            psb = [ABa.alloc(8 * 512, f"psb{i}") for i in range(2)]
            mT = ABa.alloc(8 * 512, "mT")
            mT3 = mT.ap.rearrange("p (k t) -> p k t", k=8)
            h2_ = ABa.alloc(8 * 512, "h2b")
            h23 = h2_.ap.rearrange("p (k t) -> p k t", k=8)
            xhb = [ABa.alloc(D, f"xhb{i}") for i in range(2)]
            junk = AFa.alloc(D, "junk")
            mtmp = [AFa.alloc(512, f"mtmp{i}") for i in range(2)]
            xt = [AFa.alloc(D, f"xt{i}") for i in range(2)]
            tq = [AFa.alloc(D, f"tq{i}") for i in range(2)]
            xnw = [AFa.alloc(D, f"xnw{i}") for i in range(2)]
            st = [AFa.alloc(8, f"st{i}") for i in range(2)]
            v_on = onT.ap.rearrange("(k p) t -> p k t", p=128)
            v_ga = gAT.ap.rearrange("(k p) t -> p k t", p=128)
            v_ps = pST.ap.rearrange("(k p) t -> p k t", p=128)
            v_h2 = H2T.ap.rearrange("(k p) t -> p k t", p=128)
            for bi in range(8):
                cs = slice(bi * 512, (bi + 1) * 512)
                on_ = onb[bi % 2]
                ga_ = gab[bi % 2]
                ps_ = psb[bi % 2]
                on3 = on_.ap.rearrange("p (k t) -> p k t", k=8)
                ga3 = ga_.ap.rearrange("p (k t) -> p k t", k=8)
                pS3 = ps_.ap.rearrange("p (k t) -> p k t", k=8)
                P.dma("sp", on3, v_on[:, :, cs], reads=[onT], writes=[on_])
                P.dma("act", ga3, v_ga[:, :, cs], reads=[gAT], writes=[ga_])
                P.dma("sp", pS3, v_ps[:, :, cs], reads=[pST], writes=[ps_])
                for fc in range(8):
                    pY = PS[fc % 2]
                    for hc in range(8):
                        MM(pY.ap, Wba3[:, hc, fc * 128:(fc + 1) * 128], on3[:, hc, :], hc == 0, hc == 7, [Wba, on_], [pY])
                    m_ = mtmp[fc % 2]
                    TT("dve", m_.ap, pY.ap, ga3[:, fc, :], ALU.mult, [pY, ga_], [m_])
                    TT("pool", mT3[:, fc, :], m_.ap, pS3[:, fc, :], ALU.add, [m_, ps_], [mT])
                for tl in range(4):
                    ti = bi * 4 + tl
                    x_ = xt[tl % 2]
                    s_ = st[tl % 2]
                    P.dma("sp", x_.ap, xown[ti * 128:(ti + 1) * 128, :], writes=[x_])
                    py, pyb = ps2(1 + tl % 2)
                    for half in range(2):
                        hs = slice(half * 512, (half + 1) * 512)
                        for mc in range(8):
                            MM(py[:, hs], mT3[:, mc, tl * 128:(tl + 1) * 128], Wo3[:, mc, hs], mc == 0, mc == 7,
                               [mT, Wo], [pyb[half]])
                        ACT(junk.ap[:, hs], py[:, hs], AF.Square, [pyb[half]], [junk, s_], accum=s_.ap[:, 4 + half:5 + half])
                    TT("dve", s_.ap[:, 0:1], s_.ap[:, 4:5], s_.ap[:, 5:6], ALU.add, [s_], [s_])
                    rstd(s_, s_.ap[:, 0:1], s_.ap[:, 1:2], 1.0 / D, RMS_EPS)
                    q_ = tq[tl % 2]
                    for half in range(2):
                        hs = slice(half * 512, (half + 1) * 512)
                        STT(q_.ap[:, hs], py[:, hs], s_.ap[:, 1:2], G1.ap[:, hs], ALU.mult, ALU.mult, [pyb[half], s_, G1], [q_])
                    xn_ = xnw[tl % 2]
                    TT("pool", xn_.ap, q_.ap, x_.ap, ALU.add, [q_, x_], [xn_])
                    P.dma("pool", XN.ap[ti * 128:(ti + 1) * 128, :], xn_.ap, reads=[xn_], writes=[XN])
                    ACT(junk.ap, xn_.ap, AF.Square, [xn_], [junk, s_], accum=s_.ap[:, 2:3])
                    rstd(s_, s_.ap[:, 2:3], s_.ap[:, 3:4], 1.0 / D, RMS_EPS)
                    xh_ = xhb[tl % 2]
                    TS("dve", xh_.ap, xn_.ap, s_.ap[:, 3:4], None, ALU.mult, None, [xn_, s_], [xh_])
                    pt, ptb = ps2(3)
                    for dc in range(8):
                        TR(pt[:, dc * 128:(dc + 1) * 128], xh_.ap[:, dc * 128:(dc + 1) * 128], [xh_], [ptb[dc // 4]])
                    for dc in range(8):
                        o = h23[:, dc, tl * 128:(tl + 1) * 128]
                        i_ = pt[:, dc * 128:(dc + 1) * 128]
                        if dc // 4 == 0:
                            ACT(o, i_, AF.Identity, [ptb[dc // 4], a2T, modT], [h2_], bias=b2T(dc), scale=a2T.ap[:, dc:dc + 1])
                        else:
                            TS("dve", o, i_, a2T.ap[:, dc:dc + 1], b2T(dc), ALU.mult, ALU.add, [ptb[dc // 4], a2T, modT], [h2_])
                P.dma("pool", v_h2[:, :, cs], h23, reads=[h2_], writes=[H2T])
            end_phase()

        if "F" in phases:
            ub = [ABa.alloc(D, f"ub{i}") for i in range(3)]
            vb = [ABa.alloc(D, f"vb{i}") for i in range(3)]
            utb = [ABa.alloc(D, f"utb{i}") for i in range(3)]
            for i in range(128):
                u_ = ub[i % 3]
                v_ = vb[i % 3]
                t_ = utb[i % 3]
                P.dma("pool", u_.ap, peer_u[i * 128:(i + 1) * 128, :], writes=[u_])
                P.dma("pool", v_.ap, peer_v[i * 128:(i + 1) * 128, :], writes=[v_])
                P.dma("sp", VB.ap[i * 128:(i + 1) * 128, :], v_.ap, reads=[v_], writes=[VB])
                pt, ptb = ps2(i % 4)
                for dc in range(8):
                    TR(pt[:, dc * 128:(dc + 1) * 128], u_.ap[:, dc * 128:(dc + 1) * 128], [u_], [ptb[dc // 4]])
                for hf in range(2):
                    COPY("act" if hf == 0 else "dve", t_.ap[:, hf * 512:(hf + 1) * 512], pt[:, hf * 512:(hf + 1) * 512],
                         [ptb[hf]], [t_])
                P.dma("sp", UT.ap[i, :, :], t_.ap, reads=[t_], writes=[UT])
            end_phase()

        if "F" in phases:
            Wpq = ABa.alloc(8 * 2 * D, "Wpq")
            Wpq3 = load_w_bf16(Wpq, peer_wq, 2 * D)
            kT = ABa.alloc(256, "kT")
            kT3 = kT.ap.rearrange("d (p n) -> d p n", p=2)
            for p_i in range(2):
                P.dma("pool", kT3[:, p_i, :], peer_keysT[p_i, :, :], writes=[kT])
            h2t = [ABa.alloc(8 * 128, f"h2t{i}") for i in range(2)]
            qn = ABa.alloc(2 * D, "qn")
            qnT = ABa.alloc(2 * D, "qnT")
            qnT3 = qnT.ap.rearrange("d (g t) -> d g t", g=16)
            sqf = AFa.alloc(2 * D, "sqf")
            ssb = [AFa.alloc(2 * D, f"s_sb{i}") for i in range(2)]
            tv = AFa.alloc(256, "tv")
            tv3 = tv.ap.rearrange("p (g a) -> p g a", g=16)
            tv4 = tv.ap.rearrange("p (h two a) -> p h two a", h=8, two=2)
            workg = [AFa.alloc(128, f"work{g}") for g in range(16)]
            tvg = [Buf(f"tvg{g}") for g in range(16)]
            c24g = [Buf(f"c24g{h}") for h in range(8)]
            cw0 = [AFa.alloc(256, f"cw0_{h}") for h in range(8)]
            cw1 = [AFa.alloc(256, f"cw1_{h}") for h in range(8)]
            cand = AFa.alloc(2 * D, "cand")
            cand3 = cand.ap.rearrange("p (h c) -> p h c", h=8)
            cw = [AFa.alloc(256, f"cw{i}") for i in range(2)]
            c24 = AFa.alloc(8 * 24, "c24")
            c243 = c24.ap.rearrange("p (h c) -> p h c", h=8)
            rg = AFa.alloc(16, "rg")
            smb = [AFa.alloc(16, f"smb{i}") for i in range(2)]
            zz = AFa.alloc(8, "zz")
            e16 = AFa.alloc(128, "e16")
            v_h2 = H2T.ap.rearrange("(k p) t -> p k t", p=128)
            for ti in range(NQ // 128):
                h2_ = h2t[ti % 2]
                h23 = h2_.ap.rearrange("p (k t) -> p k t", k=8)
                s_sb = ssb[ti % 2]
                s3 = s_sb.ap.rearrange("p (g n) -> p g n", g=16)
                sm = smb[ti % 2]
                P.dma("sp", h23, v_h2[:, :, ti * 128:(ti + 1) * 128], reads=[H2T], writes=[h2_])
                for cb in range(4):
                    for dc in range(8):
                        MM(PS[cb].ap, h23[:, dc, :], Wpq3[:, dc, cb * 512:(cb + 1) * 512], dc == 0, dc == 7, [h2_, Wpq], [PS[cb]])
                    ACT(sqf.ap[:, cb * 512:(cb + 1) * 512], PS[cb].ap, AF.Square, [PS[cb]], [sqf])
                RED(rg.ap, sqf.ap.rearrange("p (g n) -> p g n", g=16), ALU.add, [sqf], [rg])
                rstd(rg, rg.ap, rg.ap, 1.0 / 128, RMS_EPS)
                for cb in range(4):
                    TT("dve", qn.ap[:, cb * 512:(cb + 1) * 512].rearrange("p (g n) -> p g n", g=4),
                       PS[cb].ap.rearrange("p (g n) -> p g n", g=4),
                       rg.ap[:, cb * 4:(cb + 1) * 4].unsqueeze(2).to_broadcast([128, 4, 128]), ALU.mult, [PS[cb], rg], [qn])
                for g in range(16):
                    pb = PS[4 + g // 4]
                    TR(pb.ap[:, (g % 4) * 128:(g % 4 + 1) * 128], qn.ap[:, g * 128:(g + 1) * 128], [qn], [pb])
                for c4 in range(4):
                    COPY("act" if c4 % 2 == 0 else "dve", qnT3[:, c4 * 4:(c4 + 1) * 4, :],
                         PS[4 + c4].ap.rearrange("p (g t) -> p g t", g=4), [PS[4 + c4]], [qnT])
                for g in range(16):
                    pb = PS[g // 4]
                    MM(pb.ap[:, (g % 4) * 128:(g % 4 + 1) * 128], qnT3[:, g, :], kT3[:, g % 2, :], True, True, [qnT, kT], [pb])
                for c4 in range(4):
                    COPY("act", s_sb.ap[:, c4 * 512:(c4 + 1) * 512], PS[c4].ap, [PS[c4]], [s_sb])
                P.dma("pool", SS.ap[ti * 128:(ti + 1) * 128, :], s_sb.ap, reads=[s_sb], writes=[SS])
                for g in range(16):
                    MAX8(tv3[:, g, 0:8], s3[:, g, :], [s_sb], [tvg[g]])
                for g in range(16):
                    MREP(workg[g].ap, tv3[:, g, 0:8], s3[:, g, :], [s_sb, tvg[g]], [workg[g]])
                for g in range(16):
                    MAX8(tv3[:, g, 8:16], workg[g].ap, [workg[g]], [tvg[g]])
                TT("dve", cand.ap.rearrange("p (h a b) -> p h a b", h=8, a=16),
                   tv4[:, :, 0, :].unsqueeze(3).to_broadcast([128, 8, 16, 16]),
                   tv4[:, :, 1, :].unsqueeze(2).to_broadcast([128, 8, 16, 16]), ALU.add, tvg, [cand])
                for h in range(8):
                    MAX8(c243[:, h, 0:8], cand3[:, h, :], [cand], [c24g[h]])
                for h in range(8):
                    MREP(cw0[h].ap, c243[:, h, 0:8], cand3[:, h, :], [cand, c24g[h]], [cw0[h]])
                for h in range(8):
                    MAX8(c243[:, h, 8:16], cw0[h].ap, [cw0[h]], [c24g[h]])
                for h in range(8):
                    MREP(cw1[h].ap, c243[:, h, 8:16], cw0[h].ap, [cw0[h], c24g[h]], [cw1[h]])
                for h in range(8):
                    MAX8(c243[:, h, 16:24], cw1[h].ap, [cw1[h]], [c24g[h]])
                TT("dve", sm.ap[:, 0:8], c243[:, :, 15], c243[:, :, 16], ALU.add, c24g, [sm])
                TS("dve", sm.ap[:, 0:8], sm.ap[:, 0:8], 0.5, None, ALU.mult, None, [sm], [sm])
                TT("dve", e16.ap.rearrange("p (h k) -> p h k", h=8), c243[:, :, 0:16],
                   c243[:, :, 0:1].to_broadcast([128, 8, 16]), ALU.subtract, c24g, [e16])
                ACT(e16.ap, e16.ap, AF.Exp, [e16], [e16])
                RED(zz.ap, e16.ap.rearrange("p (h k) -> p h k", h=8), ALU.add, [e16], [zz])
                ACT(zz.ap, zz.ap, AF.Ln, [zz], [zz])
                TT("dve", sm.ap[:, 8:16], zz.ap, c243[:, :, 0], ALU.add, [zz] + c24g, [sm])
                TS("dve", sm.ap[:, 8:16], sm.ap[:, 8:16], -1.0, None, ALU.mult, None, [sm], [sm])
                P.dma("pool", SM.ap[ti * 128:(ti + 1) * 128, :], sm.ap, reads=[sm], writes=[SM])
            end_phase()

        if "F" in phases:
            GTall = ABa.alloc(128 * 256, "GT")
            GT3 = GTall.ap.rearrange("j (i t) -> j i t", t=256)
            GTg = [Buf(f"GTg{g}") for g in range(16)]
            h2blk = [ABa.alloc(8 * 256, f"h2blk{i}") for i in range(2)]
            Mb = [ABa.alloc(1024, f"Mb{i}") for i in range(3)]
            utc = [ABa.alloc(D, f"utc{i}") for i in range(4)]
            vbc = [ABa.alloc(D, f"vbc{i}") for i in range(4)]
            agb = [ABa.alloc(256, f"agb{i}") for i in range(3)]
            ssb = [AFa.alloc(2 * D, f"s_sb{i}") for i in range(2)]
            smb = [AFa.alloc(16, f"smb{i}") for i in range(2)]
            Sf = [AFa.alloc(1024, f"Sf{i}") for i in range(3)]
            Wf = [AFa.alloc(1024, f"Wf{i}") for i in range(2)]
            af = [AFa.alloc(256, f"af{i}") for i in range(2)]
            xnl = [AFa.alloc(D, f"xnl{i}") for i in range(2)]
            yq = [AFa.alloc(D, f"yq{i}") for i in range(2)]
            yo = [AFa.alloc(D, f"yo{i}") for i in range(2)]
            st = [AFa.alloc(8, f"st{i}") for i in range(2)]
            junk = AFa.alloc(D, "junk")
            v_h2 = H2T.ap.rearrange("(k p) t -> p k t", p=128)
            cnt = {"S": 0, "u": 0}
            NST = NQ // 256

            def load_scores(sti):
                for tl in range(2):
                    ti = sti * 2 + tl
                    P.dma("sp", ssb[tl].ap, SS.ap[ti * 128:(ti + 1) * 128, :], reads=[SS], writes=[ssb[tl]])
                    P.dma("sp", smb[tl].ap, SM.ap[ti * 128:(ti + 1) * 128, :], reads=[SM], writes=[smb[tl]])

            def gbuild(tl, ib):
                for _ in gbuild_gen(tl, ib):
                    pass

            def gbuild_gen(tl, ib):
                tcs = slice(tl * 128, (tl + 1) * 128)
                s_sb = ssb[tl]
                sm = smb[tl]
                s3 = s_sb.ap.rearrange("p (g n) -> p g n", g=16)
                gp, gpb = ps2(2)
                for h in range(8):
                    n = cnt["S"]
                    cnt["S"] += 1
                    S_ = Sf[n % 3]
                    W_ = Wf[n % 2]
                    M_ = Mb[n % 3]
                    TT("dve" if h % 4 == 3 else "pool", S_.ap.rearrange("p (i j) -> p i j", i=8),
                       s3[:, 2 * h, ib * 8:(ib + 1) * 8].unsqueeze(2).to_broadcast([128, 8, 128]),
                       s3[:, 2 * h + 1:2 * h + 2, :].to_broadcast([128, 8, 128]), ALU.add, [s_sb], [S_])
                    ACT(W_.ap, S_.ap, AF.Exp, [S_, sm], [W_], bias=sm.ap[:, 8 + h:9 + h])
                    STT(M_.ap, S_.ap, sm.ap[:, h:h + 1], W_.ap, ALU.is_ge, ALU.mult, [S_, W_, sm], [M_])
                    for ii in range(8):
                        MMG(gp[:, ii * 128:(ii + 1) * 128], M_.ap[:, ii * 128:(ii + 1) * 128], ident_b.ap,
                            h == 0 and ii % 4 == 0, h == 7, [M_, ident_b], [gpb[ii // 4]])
                    if h in (1, 3, 5):
                        yield
                for hf in range(2):
                    COPY("act" if hf == 0 else "dve", GT3[:, ib * 8 + hf * 4:ib * 8 + hf * 4 + 4, tcs],
                         gp[:, hf * 512:(hf + 1) * 512].rearrange("p (i t) -> p i t", i=4), [gpb[hf]], [GTg[ib]])
                yield

            def dense(sti, i):
                hb_ = h2blk[sti % 2]
                h23 = hb_.ap.rearrange("p (k t) -> p k t", k=8)
                n = cnt["u"]
                cnt["u"] += 1
                u_ = utc[n % 4]
                v_ = vbc[n % 4]
                u3 = u_.ap.rearrange("p (k j) -> p k j", k=8)
                P.dma("sp", u_.ap, UT.ap[i, :, :], reads=[UT], writes=[u_])
                P.dma("act" if i % 2 == 0 else "sp", v_.ap, VB.ap[i * 128:(i + 1) * 128, :], reads=[VB], writes=[v_])
                pa = PS[6 + n % 2]
                for dc in range(8):
                    MM(pa.ap[:, 0:256], u3[:, dc, :], h23[:, dc, :], dc == 0, dc == 7, [u_, hb_], [pa])
                a_ = af[n % 2]
                ACT(a_.ap, pa.ap[:, 0:256], AF.Gelu, [pa], [a_])
                g_ = agb[n % 3]
                TT("dve", g_.ap, a_.ap, GT3[:, i, :], ALU.mult, [a_, GTg[i // 8]], [g_])
                return (g_, v_, i)

            def dense_out(st_):
                g_, v_, i = st_
                for tl in range(2):
                    for half in range(2):
                        po_ = PS[tl * 2 + half]
                        MM(po_.ap, g_.ap[:, tl * 128:(tl + 1) * 128], v_.ap[:, half * 512:(half + 1) * 512], i == 0, i == 127,
                           [g_, v_], [po_])

            def epilogue(sti):
                for tl in range(2):
                    ti = sti * 2 + tl
                    py, pyb = ps2(tl)
                    s_ = st[tl]
                    x_ = xnl[tl]
                    P.dma("sp", x_.ap, XN.ap[ti * 128:(ti + 1) * 128, :], reads=[XN], writes=[x_])
                    for half in range(2):
                        hs = slice(half * 512, (half + 1) * 512)
                        ACT(junk.ap[:, hs], py[:, hs], AF.Square, [pyb[half]], [junk, s_], accum=s_.ap[:, 4 + half:5 + half])
                    TT("dve", s_.ap[:, 0:1], s_.ap[:, 4:5], s_.ap[:, 5:6], ALU.add, [s_], [s_])
                    rstd(s_, s_.ap[:, 0:1], s_.ap[:, 1:2], 1.0 / D, RMS_EPS)
                    q_ = yq[tl]
                    for half in range(2):
                        hs = slice(half * 512, (half + 1) * 512)
                        STT(q_.ap[:, hs], py[:, hs], s_.ap[:, 1:2], G2.ap[:, hs], ALU.mult, ALU.mult, [pyb[half], s_, G2], [q_])
                    o_ = yo[tl]
                    TT("pool", o_.ap, q_.ap, x_.ap, ALU.add, [q_, x_], [o_])
                    P.dma("pool", Y.ap[ti * 128:(ti + 1) * 128, :], o_.ap, reads=[o_], writes=[Y])

            def load_h2(sti):
                hb_ = h2blk[sti % 2]
                P.dma("sp", hb_.ap.rearrange("p (k t) -> p k t", k=8), v_h2[:, :, sti * 256:(sti + 1) * 256],
                      reads=[H2T], writes=[hb_])

            load_h2(0)
            load_scores(0)
            for ib in range(15):
                for tl in range(2):
                    gbuild(tl, ib)
            for sti in range(NST):
                if sti + 1 < NST:
                    load_h2(sti + 1)
                prev = None
                for ib in range(16):
                    if ib == 0:
                        gens = [gbuild_gen(0, 15), gbuild_gen(1, 15)]
                    elif sti + 1 < NST:
                        gens = [gbuild_gen(0, ib - 1), gbuild_gen(1, ib - 1)]
                    else:
                        gens = []
                    steps = [g for g in gens for _ in range(4)]
                    for pair in range(4):
                        for i in range(ib * 8 + 2 * pair, ib * 8 + 2 * pair + 2):
                            cur = dense(sti, i)
                            if prev is not None:
                                dense_out(prev)
                            prev = cur
                        for _ in range(2):
                            if steps:
                                next(steps.pop(0), None)
                    assert not steps
                    if ib == 0 and sti + 1 < NST:
                        load_scores(sti + 1)
                dense_out(prev)
                epilogue(sti)
            AFa.reset(fmark)
            ABa.reset(bmark)

        if dbg:
            for name, (t, n) in list(dbg_outs.items()):
                o = nc.dram_tensor("dbg_" + name, [128, n], F32, kind="ExternalOutput").ap()
                P.dma("sp", o, t.ap[:, 0:n], reads=[t], writes=[Buf("dbgo_" + name)])
            for name, t in (("hT_all", hT_all), ("hT_own", hT_own), ("KT", KT), ("Vs", Vs), ("QT", QT), ("pST", pST),
                            ("gAT", gAT), ("onT", onT), ("XN", XN), ("H2T", H2T), ("SS", SS), ("SM", SM)):
                if name not in dbg:
                    continue
                shp = list(t.ap.shape)
                o = nc.dram_tensor("dbg_" + name, shp, t.ap.dtype, kind="ExternalOutput").ap()
                P.dma("sp", o, t.ap, reads=[t], writes=[Buf("dbgo_" + name)])

        P.barrier()
        P.emit()
    return nc, P


def _rope_tables():
    f = np.arange(64)
    blk = f // 32
    within = f % 32
    i = within % 16
    first = within < 16
    inv = (10000.0 ** (-(np.arange(16, dtype=np.float32)) / 16.0)).astype(np.float32)
    t = np.arange(L)
    rows = (t // 64).astype(np.float32)
    cols = (t % 64).astype(np.float32)
    pos = np.where(blk[:, None] == 0, rows[None, :], cols[None, :]).astype(np.float32)
    ang = (pos * inv[i][:, None]).astype(np.float32)
    C = np.cos(ang).astype(np.float32)
    S = (np.sin(ang) * np.where(first, -1.0, 1.0)[:, None]).astype(np.float32)
    partner = np.where(first, f + 16, f - 16)
    C128 = np.concatenate([C, C], axis=0)
    S128 = np.concatenate([S, S], axis=0)
    return np.ascontiguousarray(C128), np.ascontiguousarray(S128), partner


_CACHE = {}


def _get_program():
    if "nc" not in _CACHE:
        _CACHE["nc"] = build_program()[0]
    return _CACHE["nc"]


def make_in_maps(inputs):
    f32 = lambda a: np.ascontiguousarray(np.asarray(a, dtype=np.float32))
    x = f32(inputs["x"])
    c = f32(inputs["c"])
    ctx = f32(inputs["ctx"])
    c_ctx = f32(inputs["c_ctx"])
    C128, S128, partner = _rope_tables()
    w_in = f32(inputs["w_in"][0])
    perm = np.concatenate([(np.arange(16)[:, None] * 64 + partner[None, :]).reshape(-1),
                           1024 + (np.arange(16)[:, None] * 64 + partner[None, :]).reshape(-1)])
    w_qk_sw = np.ascontiguousarray(w_in[:, perm])
    lam_vecs = np.concatenate([f32(inputs["lambda_q1"][0]), f32(inputs["lambda_k1"][0]),
                               f32(inputs["lambda_q2"][0]), f32(inputs["lambda_k2"][0])]).reshape(1, 256)
    shared = {
        "w_ada": f32(inputs["w_ada"][0]), "b_ada": f32(inputs["b_ada"][0]).reshape(1, -1),
        "g_pre_mix": f32(inputs["g_pre_mix"][0]).reshape(1, -1), "g_post_mix": f32(inputs["g_post_mix"][0]).reshape(1, -1),
        "g_pre_ffn": f32(inputs["g_pre_ffn"][0]).reshape(1, -1), "g_post_ffn": f32(inputs["g_post_ffn"][0]).reshape(1, -1),
        "w_in": w_in, "w_qk_sw": w_qk_sw, "b_gate": f32(inputs["b_gate"][0]).reshape(1, -1),
        "lam_vecs": np.ascontiguousarray(lam_vecs), "subln_g": f32(inputs["subln_g"][0]).reshape(1, -1),
        "sgu_ln_g": f32(inputs["sgu_ln_g"][0]).reshape(1, -1), "sgu_ln_b": f32(inputs["sgu_ln_b"][0]).reshape(1, -1),
        "sgu_wT": np.ascontiguousarray(np.transpose(f32(inputs["sgu_w"][0]), (0, 2, 1))),
        "sgu_b": f32(inputs["sgu_b"][0]),
        "w_branch_attn": f32(inputs["w_branch_attn"][0]), "w_branch_sgu": f32(inputs["w_branch_sgu"][0]),
        "w_out": f32(inputs["w_out"][0]), "peer_wq": f32(inputs["peer_w_query"][0]),
        "peer_keysT": np.ascontiguousarray(np.transpose(f32(inputs["peer_sub_keys"][0]), (0, 2, 1))),
        "peer_u": f32(inputs["peer_u"][0]), "peer_v": f32(inputs["peer_v"][0]),
        "ropeC": C128, "ropeS": S128,
    }
    in_maps = []
    for core in range(8):
        b, half = core // 2, core % 2
        m = dict(shared)
        m["xb"] = x[b]
        m["xown"] = np.ascontiguousarray(x[b, half * NQ:(half + 1) * NQ])
        m["ctxb"] = ctx[b]
        m["cvec"] = np.ascontiguousarray(np.stack([c[b], c_ctx]))
        m["ropeCq"] = np.ascontiguousarray(C128[:, half * NQ:(half + 1) * NQ])
        m["ropeSq"] = np.ascontiguousarray(S128[:, half * NQ:(half + 1) * NQ])
        in_maps.append(m)
    return in_maps


def kernel(**inputs):
    nc = _get_program()
    in_maps = make_in_maps(inputs)
    res = run_bass_kernel_spmd(nc, in_maps, core_ids=list(range(8)))
    out = np.empty((4, L, D), dtype=np.float32)
    for core in range(8):
        b, half = core // 2, core % 2
        out[b, half * NQ:(half + 1) * NQ] = np.asarray(res.results[core]["y"], dtype=np.float32)
    return out
```

```python
import contextlib
import math
import numpy as np
import concourse.bass as bass
import concourse.mybir as mybir
from concourse.bass_utils import run_bass_kernel_spmd

F32 = mybir.dt.float32
BF16 = mybir.dt.bfloat16
ALU = mybir.AluOpType
AF = mybir.ActivationFunctionType
AX = mybir.AxisListType

D = 1024
L = 8192
LC = 256
LK = L + LC
NQ = 4096
NE = 16384
RMS_EPS = 1e-6
LAM_INIT = 0.8 - 0.6 * math.exp(-0.3 * 0)
NEG = -1.0e30

ENGS = ("pe", "act", "dve", "pool", "sp")
DMA_POOL = {"sp": 28, "act": 8, "pool": 20}
SAME_ENGINE_SYNC = {"pe": False, "act": True, "dve": True, "pool": True, "sp": False}


class Buf:
    __slots__ = ("name", "last_write", "readers")

    def __init__(self, name):
        self.name = name
        self.last_write = None
        self.readers = []


class T:
    __slots__ = ("ap", "buf")

    def __init__(self, ap, buf):
        self.ap = ap
        self.buf = buf if isinstance(buf, Buf) else Buf(buf)

    def __getitem__(self, key):
        return self.ap[key]


def _bufs(xs):
    out = []
    for x in xs:
        if x is None:
            continue
        out.append(x.buf if isinstance(x, T) else x)
    return out


class Prog:
    def __init__(self, nc):
        self.nc = nc
        self.streams = {e: [] for e in ENGS}
        self.cnt = {e: 0 for e in ENGS}
        self.seen = {e: {} for e in ENGS}
        self.dma_i = {q: 0 for q in DMA_POOL}
        self.signal = {e: set() for e in ENGS}

    def _need(self, eng, deps):
        need = {}
        for d in deps:
            if d is None:
                continue
            key, val, src = d
            if src == eng and not SAME_ENGINE_SYNC[eng]:
                continue
            if self.seen[eng].get(key, 0) >= val:
                continue
            if need.get(key, 0) < val:
                need[key] = val
        for key, val in need.items():
            self.seen[eng][key] = val
            self.streams[eng].append(("wait", key, val))
            if key[0] == "eng":
                self.signal[key[1]].add(val)

    @staticmethod
    def _deps(reads, writes):
        deps = []
        for r in reads:
            deps.append(r.last_write)
        for w in writes:
            deps.append(w.last_write)
            deps.extend(w.readers)
        return deps

    @staticmethod
    def _commit(tok, reads, writes):
        for r in reads:
            r.readers.append(tok)
            if len(r.readers) > 64:
                best = {}
                for t in r.readers:
                    if best.get(t[0], (0,))[0] < t[1]:
                        best[t[0]] = (t[1], t[2])
                r.readers = [(k, v[0], v[1]) for k, v in best.items()]
        for w in writes:
            w.last_write = tok
            w.readers = []

    def op(self, eng, fn, reads=(), writes=()):
        reads = _bufs(reads)
        writes = _bufs(writes)
        self._need(eng, self._deps(reads, writes))
        self.cnt[eng] += 1
        tok = (("eng", eng), self.cnt[eng], eng)
        self.streams[eng].append(("op", fn, ("eng", eng), self.cnt[eng]))
        self._commit(tok, reads, writes)
        return tok

    def dma(self, q, out, in_, reads=(), writes=(), **kw):
        reads = _bufs(reads)
        writes = _bufs(writes)
        n = DMA_POOL[q]
        i = self.dma_i[q]
        self.dma_i[q] += 1
        slot, gen = i % n, i // n
        key = ("dma", q, slot)
        deps = self._deps(reads, writes)
        if gen > 0:
            deps.append((key, 16 * gen, None))
        self._need(q, deps)
        tok = (key, 16 * (gen + 1), None)
        self.streams[q].append(("dma", lambda e: e.dma_start(out=out, in_=in_, **kw), key, 16))
        self._commit(tok, reads, writes)
        return tok

    def barrier(self):
        toks = [(("eng", e), self.cnt[e], None) for e in ENGS if self.cnt[e] > 0]
        for q, n in DMA_POOL.items():
            i = self.dma_i[q]
            for slot in range(n):
                g = (i - slot + n - 1) // n if i > slot else 0
                if g > 0:
                    toks.append((("dma", q, slot), 16 * g, None))
        for e in ENGS:
            self._need(e, [t for t in toks if t[0] != ("eng", e)])

    def emit(self):
        nc = self.nc
        with contextlib.ExitStack() as es:
            sems = {}
            for e in ENGS:
                sems[("eng", e)] = es.enter_context(nc.semaphore("s_" + e))
            for q, n in DMA_POOL.items():
                for s in range(n):
                    sems[("dma", q, s)] = es.enter_context(nc.semaphore(f"d_{q}{s}"))
            block = es.enter_context(nc.Block())

            semval = {}
            for en in ENGS:
                c = 0
                m = {}
                for item in self.streams[en]:
                    if item[0] == "op" and item[3] in self.signal[en]:
                        c += 1
                        m[item[3]] = c
                semval[en] = m

            def run(engname):
                def body(e):
                    for item in self.streams[engname]:
                        if item[0] == "wait":
                            key, val = item[1], item[2]
                            if key[0] == "eng":
                                val = semval[key[1]][val]
                            e.wait_ge(sems[key], val)
                        elif item[0] == "dma":
                            _, fn, key, inc = item
                            fn(e).then_inc(sems[key], inc)
                        else:
                            _, fn, key, idx = item
                            ins = fn(e)
                            if idx in self.signal[engname]:
                                ins.then_inc(sems[key], 1)
                return body

            block.sync(run("sp"))
            block.scalar(run("act"))
            block.vector(run("dve"))
            block.gpsimd(run("pool"))
            block.tensor(run("pe"))


class Arena:
    def __init__(self, tensor, size, name):
        self.t = tensor
        self.size = size
        self.off = 0
        self.name = name
        self.n = 0

    def alloc(self, n, name=None):
        assert self.off + n <= self.size, f"arena {self.name} overflow: {self.off}+{n}>{self.size} ({name})"
        ap = self.t[:, self.off:self.off + n]
        self.off += n
        self.n += 1
        return T(ap, Buf(f"{self.name}_{name or self.n}"))

    def mark(self):
        return self.off

    def reset(self, m):
        self.off = m


def build_program(dbg=None, phases="ABCEF"):
    nc = bass.Bass("TRN2", target_bir_lowering=False)
    P = Prog(nc)

    def din(name, shape, dt=F32):
        return nc.dram_tensor(name, list(shape), dt, kind="ExternalInput").ap()

    def dscr(name, shape, dt):
        return T(nc.dram_tensor(name, list(shape), dt, kind="Internal").ap(), name)

    xb = din("xb", [L, D])
    xown = din("xown", [NQ, D])
    ctxb = din("ctxb", [LC, D])
    cvec = din("cvec", [2, D])
    w_ada = din("w_ada", [D, 6 * D])
    b_ada = din("b_ada", [1, 6 * D])
    g_pre_mix = din("g_pre_mix", [1, D])
    g_post_mix = din("g_post_mix", [1, D])
    g_pre_ffn = din("g_pre_ffn", [1, D])
    g_post_ffn = din("g_post_ffn", [1, D])
    w_in = din("w_in", [D, 7 * D])
    w_qk_sw = din("w_qk_sw", [D, 2 * D])
    b_gate = din("b_gate", [1, 2 * D])
    lam_vecs = din("lam_vecs", [1, 256])
    subln_g = din("subln_g", [1, 128])
    sgu_ln_g = din("sgu_ln_g", [1, D])
    sgu_ln_b = din("sgu_ln_b", [1, D])
    sgu_wT = din("sgu_wT", [8, 128, 128])
    sgu_b = din("sgu_b", [8, 128])
    w_branch_attn = din("w_branch_attn", [D, D])
    w_branch_sgu = din("w_branch_sgu", [D, D])
    w_out = din("w_out", [D, D])
    peer_wq = din("peer_wq", [D, 2 * D])
    peer_keysT = din("peer_keysT", [2, 128, 128])
    peer_u = din("peer_u", [NE, D])
    peer_v = din("peer_v", [NE, D])
    ropeC = din("ropeC", [128, L])
    ropeS = din("ropeS", [128, L])
    ropeCq = din("ropeCq", [128, NQ])
    ropeSq = din("ropeSq", [128, NQ])
    y_out = nc.dram_tensor("y", [NQ, D], F32, kind="ExternalOutput").ap()
    Y = T(y_out, "y_out")

    hT_all = dscr("hT_all", [D, LK], BF16)
    hT_own = dscr("hT_own", [D, NQ], BF16)
    KT = dscr("KT", [D, LK], BF16)
    Vs = dscr("Vs", [LK, D], BF16)
    QT = dscr("QT", [D, NQ], BF16)
    pST = dscr("pST", [D, NQ], BF16)
    gAT = dscr("gAT", [D, NQ], BF16)
    onT = dscr("onT", [D, NQ], BF16)
    XN = dscr("XN", [NQ, D], F32)
    H2T = dscr("H2T", [D, NQ], BF16)
    UT = dscr("UT", [128, 128, D], BF16)
    VB = dscr("VB", [NE, D], BF16)
    SS = dscr("SS", [NQ, 2 * D], F32)
    SM = dscr("SM", [NQ, 16], F32)

    dbg_outs = {}

    es = contextlib.ExitStack()
    with es:
        NF = 25 * 1024
        NB = 54784
        f_t = es.enter_context(nc.sbuf_tensor("arena_f", [128, NF], F32))
        b_t = es.enter_context(nc.sbuf_tensor("arena_b", [128, NB], BF16))
        AFa = Arena(f_t, NF, "f")
        ABa = Arena(b_t, NB, "b")
        pst = [es.enter_context(nc.psum_tensor(f"ps{i}", [128, 1024], F32)) for i in range(4)]
        PS = [T(pst[i // 2][:, (i % 2) * 512:(i % 2 + 1) * 512], f"psbank{i}") for i in range(8)]

        def ps2(i):
            return pst[i][:, :], [PS[2 * i].buf, PS[2 * i + 1].buf]

        def MM(out, lhsT, rhs, start, stop, reads, writes):
            P.op("pe", lambda e: e.matmul(out, lhsT=lhsT, rhs=rhs, start=start, stop=stop), reads, writes)

        def MMG(out, lhsT, rhs, start, stop, reads, writes):
            P.op("pe", lambda e: e.matmul(out, lhsT=lhsT, rhs=rhs, start=start, stop=stop, skip_group_check=True),
                 reads, writes)

        def TR(out, in_, reads, writes):
            MM(out, in_, ident_b.ap, True, True, list(reads) + [ident_b], writes)

        def ACT(out, in_, func, reads, writes, bias=None, scale=None, accum=None):
            kw = {}
            if bias is not None:
                kw["bias"] = bias
            if scale is not None:
                kw["scale"] = scale
            if accum is not None:
                kw["accum_out"] = accum
            P.op("act", lambda e: e.activation(out=out, in_=in_, func=func, **kw), reads, writes)

        def TT(eng, out, in0, in1, op, reads, writes):
            P.op(eng, lambda e: e.tensor_tensor(out=out, in0=in0, in1=in1, op=op), reads, writes)

        def TS(eng, out, in0, s1, s2, op0, op1, reads, writes):
            if s2 is None:
                P.op(eng, lambda e: e.tensor_scalar(out=out, in0=in0, scalar1=s1, scalar2=None, op0=op0), reads, writes)
            else:
                P.op(eng, lambda e: e.tensor_scalar(out=out, in0=in0, scalar1=s1, scalar2=s2, op0=op0, op1=op1), reads, writes)

        def STT(out, in0, scalar, in1, op0, op1, reads, writes):
            P.op("dve", lambda e: e.scalar_tensor_tensor(out=out, in0=in0, scalar=scalar, in1=in1, op0=op0, op1=op1),
                 reads, writes)

        def COPY(eng, out, in_, reads, writes):
            if eng == "act":
                P.op("act", lambda e: e.copy(out=out, in_=in_), reads, writes)
            else:
                P.op(eng, lambda e: e.tensor_copy(out=out, in_=in_), reads, writes)

        def RED(out, in_, op, reads, writes):
            P.op("dve", lambda e: e.tensor_reduce(out=out, in_=in_, axis=AX.X, op=op), reads, writes)

        def MAX8(out, in_, reads, writes):
            P.op("dve", lambda e: e.max(out=out, in_=in_), reads, writes)

        def MREP(out, rep, vals, reads, writes):
            P.op("dve", lambda e: e.match_replace(out=out, in_to_replace=rep, in_values=vals, imm_value=NEG), reads, writes)

        def RECIP(out, in_, reads, writes):
            P.op("dve", lambda e: e.reciprocal(out=out, in_=in_), reads, writes)

        def SQRT(out, in_, reads, writes):
            P.op("act", lambda e: e.sqrt(out=out, in_=in_), reads, writes)

        def MEMSET(eng, out, val, writes):
            P.op(eng, lambda e: e.memset(out, val), (), writes)

        def rstd(t, ss_ap, out_ap, scale, eps):
            TS("dve", out_ap, ss_ap, scale, eps, ALU.mult, ALU.add, [t], [t])
            SQRT(out_ap, out_ap, [t], [t])
            RECIP(out_ap, out_ap, [t], [t])

        iot = AFa.alloc(128, "iot")
        ident_f = AFa.alloc(128, "ident_f")
        ones_f = AFa.alloc(128, "ones_f")
        ident_b = ABa.alloc(128, "ident_b")
        ones_b = ABa.alloc(128, "ones_b")
        P.op("pool", lambda e: e.iota(iot.ap, pattern=[[1, 128]], base=0, channel_multiplier=-1,
                                      allow_small_or_imprecise_dtypes=True), (), [iot.buf])
        P.op("dve", lambda e: e.tensor_single_scalar(out=ident_f.ap, in_=iot.ap, scalar=0.0, op=ALU.is_equal),
             [iot.buf], [ident_f.buf])
        COPY("dve", ident_b.ap, ident_f.ap, [ident_f], [ident_b])
        MEMSET("dve", ones_f.ap, 1.0, [ones_f])
        MEMSET("dve", ones_b.ap, 1.0, [ones_b])

        modT = AFa.alloc(64, "modT")
        a1T = AFa.alloc(8, "a1T")
        a1cT = AFa.alloc(8, "a1cT")
        a2T = AFa.alloc(8, "a2T")
        G1 = AFa.alloc(D, "G1")
        G2 = AFa.alloc(D, "G2")
        neg_lam = AFa.alloc(1, "neg_lam")
        sgT = AFa.alloc(1, "sgT")
        bgT = AFa.alloc(16, "bgT")
        sbT = AFa.alloc(8, "sbT")
        smallf = AFa.alloc(64, "smallf")

        def b1T(dc):
            return modT.ap[:, 0 * 8 + dc:0 * 8 + dc + 1]

        def b1cT(dc):
            return modT.ap[:, 6 * 8 + dc:6 * 8 + dc + 1]

        def b2T(dc):
            return modT.ap[:, 3 * 8 + dc:3 * 8 + dc + 1]

        fmark, bmark = AFa.mark(), ABa.mark()

        def end_phase():
            P.barrier()
            AFa.reset(fmark)
            ABa.reset(bmark)

        def load_w_bf16(dst, src_ap, ncols, q="pool"):
            d3 = dst.ap.rearrange("p (k n) -> p k n", n=ncols)
            s3_ = src_ap.rearrange("(k p) n -> p k n", p=128)
            for dc in range(8):
                for c0 in range(0, ncols, 1024):
                    P.dma(q, d3[:, dc, c0:c0 + 1024], s3_[:, dc, c0:c0 + 1024], writes=[dst])
            return d3

        if "A" in phases:
            cT = AFa.alloc(16, "cT")
            srep = [AFa.alloc(1024, f"srep{r}") for r in range(2)]
            badac = [AFa.alloc(512, f"bada{i}") for i in range(2)]
            mod_bc = AFa.alloc(6 * D, "mod_bc")
            modc_bc = AFa.alloc(2 * D, "modc_bc")
            wa = [AFa.alloc(8 * 512, f"wa{i}") for i in range(2)]
            gtmp = [AFa.alloc(D, f"gtmp{i}") for i in range(2)]
            gpreT = AFa.alloc(8, "gpreT")
            gpfT = AFa.alloc(8, "gpfT")
            lv = AFa.alloc(256, "lv")

            cT3 = cT.ap.rearrange("p (k r) -> p k r", r=2)
            for r in range(2):
                P.dma("sp", cT3[:, :, r], cvec[r, :].rearrange("(k p) -> p k", p=128), writes=[cT],
                      allow_slow_non_contiguous=True)
            ACT(cT.ap, cT.ap, AF.Silu, [cT], [cT])
            for r in range(2):
                COPY("dve", srep[r].ap.rearrange("p (k m) -> p k m", m=128),
                     cT3[:, :, r:r + 1].to_broadcast([128, 8, 128]), [cT], [srep[r]])
            w_ada_v = w_ada.rearrange("(k p) n -> p k n", p=128)
            for cb in range(12):
                w = wa[cb % 2]
                w3 = w.ap.rearrange("p (k n) -> p k n", n=512)
                bd = badac[cb % 2]
                P.dma("sp" if cb % 2 == 0 else "act", w3, w_ada_v[:, :, cb * 512:(cb + 1) * 512], writes=[w])
                P.dma("sp", bd.ap, b_ada[:, cb * 512:(cb + 1) * 512].partition_broadcast(128), writes=[bd])
                for r in range(2):
                    if r == 1 and cb >= 4:
                        continue
                    sr3 = srep[r].ap.rearrange("p (k m) -> p k m", m=128)
                    pb = PS[(cb % 2) * 2 + r]
                    for kc in range(8):
                        MM(pb.ap, sr3[:, kc, :], w3[:, kc, :], kc == 0, kc == 7, [srep[r], w], [pb])
                    dst = mod_bc if r == 0 else modc_bc
                    TT("dve", dst.ap[:, cb * 512:(cb + 1) * 512], pb.ap, bd.ap, ALU.add, [pb, bd], [dst])
            for grp in range(16):
                pb = PS[4 + grp % 4]
                for k4 in range(4):
                    idx = grp * 4 + k4
                    v, fc = idx // 8, idx % 8
                    if v < 6:
                        src, col = mod_bc, v * 1024 + fc * 128
                    else:
                        src, col = modc_bc, (v - 6) * 1024 + fc * 128
                    MM(pb.ap[:, k4 * 128:(k4 + 1) * 128], src.ap[:, col:col + 128], ident_f.ap, True, True,
                       [src, ident_f], [pb])
                COPY("dve", modT.ap[:, grp * 4:(grp + 1) * 4], pb.ap[:, 0:512:128], [pb], [modT])
            P.dma("sp", gpreT.ap, g_pre_mix[0, :].rearrange("(k p) -> p k", p=128), writes=[gpreT],
                  allow_slow_non_contiguous=True)
            P.dma("sp", gpfT.ap, g_pre_ffn[0, :].rearrange("(k p) -> p k", p=128), writes=[gpfT],
                  allow_slow_non_contiguous=True)
            for hh in range(2):
                P.dma("sp", bgT.ap[:, hh * 8:(hh + 1) * 8], b_gate[0, hh * D:(hh + 1) * D].rearrange("(k p) -> p k", p=128),
                      writes=[bgT], allow_slow_non_contiguous=True)
            P.dma("sp", sbT.ap, sgu_b.rearrange("g p -> p g"), writes=[sbT], allow_slow_non_contiguous=True)
            P.dma("sp", sgT.ap, subln_g[0, :].rearrange("(p o) -> p o", o=1), writes=[sgT],
                  allow_slow_non_contiguous=True)
            TS("dve", sgT.ap, sgT.ap, (1.0 - LAM_INIT), None, ALU.mult, None, [sgT], [sgT])
            for (dst, scv, gT) in ((a1T, 1, gpreT), (a1cT, 7, gpreT), (a2T, 4, gpfT)):
                TS("dve", dst.ap, modT.ap[:, scv * 8:(scv + 1) * 8], 1.0, None, ALU.add, None, [modT], [dst])
                TT("dve", dst.ap, dst.ap, gT.ap, ALU.mult, [dst, gT], [dst])
            P.dma("sp", gtmp[0].ap, g_post_mix.partition_broadcast(128), writes=[gtmp[0]])
            TT("dve", G1.ap, mod_bc.ap[:, 2 * D:3 * D], gtmp[0].ap, ALU.mult, [mod_bc, gtmp[0]], [G1])
            P.dma("sp", gtmp[1].ap, g_post_ffn.partition_broadcast(128), writes=[gtmp[1]])
            TT("dve", G2.ap, mod_bc.ap[:, 5 * D:6 * D], gtmp[1].ap, ALU.mult, [mod_bc, gtmp[1]], [G2])
            P.dma("sp", lv.ap, lam_vecs.partition_broadcast(128), writes=[lv])
            for i in range(2):
                TT("dve", lv.ap[:, i * 128:i * 128 + 64], lv.ap[:, i * 128:i * 128 + 64],
                   lv.ap[:, i * 128 + 64:i * 128 + 128], ALU.mult, [lv], [lv])
                RED(smallf.ap[:, i:i + 1], lv.ap[:, i * 128:i * 128 + 64], ALU.add, [lv], [smallf])
            ACT(smallf.ap[:, 0:2], smallf.ap[:, 0:2], AF.Exp, [smallf], [smallf])
            STT(neg_lam.ap, smallf.ap[:, 1:2], -LAM_INIT, smallf.ap[:, 0:1], ALU.add, ALU.subtract, [smallf], [neg_lam])
            if dbg:
                dbg_outs["modT"] = (modT, 64)
                dbg_outs["G1"] = (G1, D)
                dbg_outs["neg_lam"] = (neg_lam, 1)
            end_phase()

        def norm_transpose(src, ntok, aT, bTf, dst, blk_tiles):
            xt = [AFa.alloc(D, f"xt{i}") for i in range(4)]
            junk = ABa.alloc(D, "junk")
            xn = [ABa.alloc(D, f"xn{i}") for i in range(4)]
            hblk = [ABa.alloc(8 * 128 * blk_tiles, f"hblk{i}") for i in range(2)]
            ss = [AFa.alloc(2, f"ss{i}") for i in range(4)]
            ntiles = ntok // 128
            dst_v = dst.ap.rearrange("(k p) t -> p k t", p=128)
            for ti in range(ntiles):
                x_ = xt[ti % 4]
                s_ = ss[ti % 4]
                n_ = xn[ti % 4]
                bi = ti // blk_tiles
                hb = hblk[bi % 2]
                hb3 = hb.ap.rearrange("p (k t) -> p k t", k=8)
                tl = ti % blk_tiles
                P.dma("sp", x_.ap, src[ti * 128:(ti + 1) * 128, :], writes=[x_])
                ACT(junk.ap, x_.ap, AF.Square, [x_], [junk, s_], accum=s_.ap[:, 0:1])
                rstd(s_, s_.ap[:, 0:1], s_.ap[:, 1:2], 1.0 / D, RMS_EPS)
                TS("dve", n_.ap, x_.ap, s_.ap[:, 1:2], None, ALU.mult, None, [x_, s_], [n_])
                pa, pbufs = ps2(ti % 4)
                for dc in range(8):
                    TR(pa[:, dc * 128:(dc + 1) * 128], n_.ap[:, dc * 128:(dc + 1) * 128], [n_], [pbufs[dc // 4]])
                for dc in range(8):
                    o = hb3[:, dc, tl * 128:(tl + 1) * 128]
                    i_ = pa[:, dc * 128:(dc + 1) * 128]
                    if dc // 4 == 0:
                        ACT(o, i_, AF.Identity, [pbufs[dc // 4], aT, modT], [hb], bias=bTf(dc), scale=aT.ap[:, dc:dc + 1])
                    else:
                        TS("dve", o, i_, aT.ap[:, dc:dc + 1], bTf(dc), ALU.mult, ALU.add, [pbufs[dc // 4], aT, modT], [hb])
                if tl == blk_tiles - 1:
                    t0 = bi * blk_tiles * 128
                    P.dma("pool", dst_v[:, :, t0:t0 + blk_tiles * 128], hb3, reads=[hb], writes=[dst])
            end_phase()

        if "B" in phases:
            norm_transpose(ctxb, LC, a1cT, b1cT, T(hT_all.ap[:, 0:LC], hT_all.buf), 2)
            norm_transpose(xb, L, a1T, b1T, T(hT_all.ap[:, LC:LK], hT_all.buf), 4)
            norm_transpose(xown, NQ, a1T, b1T, hT_own, 4)

        if "B" in phases:
            Wq = ABa.alloc(8 * D, "Wq")
            Wqs = ABa.alloc(8 * D, "Wqs")
            Wk = ABa.alloc(8 * D, "Wk")
            Wks = ABa.alloc(8 * D, "Wks")
            Wv = ABa.alloc(8 * D, "Wv")
            Wk3 = load_w_bf16(Wk, w_in[:, D:2 * D], D)
            Wks3 = load_w_bf16(Wks, w_qk_sw[:, D:2 * D], D)
            Wv3 = load_w_bf16(Wv, w_in[:, 2 * D:3 * D], D)
            Wq3 = load_w_bf16(Wq, w_in[:, 0:D], D)
            Wqs3 = load_w_bf16(Wqs, w_qk_sw[:, 0:D], D)
            hblk = [ABa.alloc(8 * 512, f"hb{i}") for i in range(2)]
            Cb = [AFa.alloc(512, f"Cb{i}") for i in range(2)]
            Sb = [AFa.alloc(512, f"Sb{i}") for i in range(2)]
            t1 = [AFa.alloc(512, f"t1_{i}") for i in range(2)]
            t2 = [AFa.alloc(512, f"t2_{i}") for i in range(2)]
            ko = [ABa.alloc(512, f"ko{i}") for i in range(3)]
            vo = [ABa.alloc(D, f"vo{i}") for i in range(2)]

            def qk_block(bi, hsrc, t0, N, W3, Ws3, Wt, Wst, Ctab, Stab, c0, dst, dcol, rope):
                hb = hblk[bi % 2]
                hb3 = hb.ap.rearrange("p (k t) -> p k t", k=8)
                hsrc_v = hsrc.ap.rearrange("(k p) t -> p k t", p=128)
                P.dma("sp", hb3[:, :, 0:N], hsrc_v[:, :, t0:t0 + N], reads=[hsrc], writes=[hb])
                C_ = Cb[bi % 2]
                S_ = Sb[bi % 2]
                if rope:
                    P.dma("act", C_.ap[:, 0:N], Ctab[:, c0:c0 + N], writes=[C_])
                    P.dma("act", S_.ap[:, 0:N], Stab[:, c0:c0 + N], writes=[S_])
                for fc in range(8):
                    pA = PS[(fc % 2) * 2]
                    pB = PS[(fc % 2) * 2 + 1]
                    for dc in range(8):
                        MM(pA.ap[:, 0:N], W3[:, dc, fc * 128:(fc + 1) * 128], hb3[:, dc, 0:N], dc == 0, dc == 7, [Wt, hb], [pA])
                    k_ = ko[fc % 3]
                    if rope:
                        for dc in range(8):
                            MM(pB.ap[:, 0:N], Ws3[:, dc, fc * 128:(fc + 1) * 128], hb3[:, dc, 0:N], dc == 0, dc == 7,
                               [Wst, hb], [pB])
                        a_ = t1[fc % 2]
                        b_ = t2[fc % 2]
                        TT("dve", a_.ap[:, 0:N], pA.ap[:, 0:N], C_.ap[:, 0:N], ALU.mult, [pA, C_], [a_])
                        TT("dve", b_.ap[:, 0:N], pB.ap[:, 0:N], S_.ap[:, 0:N], ALU.mult, [pB, S_], [b_])
                        TT("pool", k_.ap[:, 0:N], a_.ap[:, 0:N], b_.ap[:, 0:N], ALU.add, [a_, b_], [k_])
                    else:
                        COPY("act", k_.ap[:, 0:N], pA.ap[:, 0:N], [pA], [k_])
                    P.dma("pool", dst.ap[fc * 128:(fc + 1) * 128, dcol:dcol + N], k_.ap[:, 0:N], reads=[k_], writes=[dst])
                return hb, hb3

            def v_block(hb, hb3, N, tok0):
                for tl in range(N // 128):
                    v_ = vo[tl % 2]
                    for half in range(2):
                        pV = PS[4 + (tl * 2 + half) % 4]
                        for dc in range(8):
                            MM(pV.ap, hb3[:, dc, tl * 128:(tl + 1) * 128], Wv3[:, dc, half * 512:(half + 1) * 512],
                               dc == 0, dc == 7, [Wv, hb], [pV])
                        COPY("act", v_.ap[:, half * 512:(half + 1) * 512], pV.ap, [pV], [v_])
                    P.dma("pool", Vs.ap[tok0 + tl * 128:tok0 + (tl + 1) * 128, :], v_.ap, reads=[v_], writes=[Vs])

            hb, hb3 = qk_block(0, hT_all, 0, LC, Wk3, Wks3, Wk, Wks, None, None, 0, KT, 0, False)
            v_block(hb, hb3, LC, 0)
            for bi in range(16):
                hb, hb3 = qk_block(bi + 1, hT_all, LC + bi * 512, 512, Wk3, Wks3, Wk, Wks, ropeC, ropeS, bi * 512,
                                   KT, LC + bi * 512, True)
                v_block(hb, hb3, 512, LC + bi * 512)
            for bi in range(8):
                qk_block(bi, hT_own, bi * 512, 512, Wq3, Wqs3, Wq, Wqs, ropeCq, ropeSq, bi * 512, QT, bi * 512, True)
            end_phase()

        if "B" in phases:
            Wz = ABa.alloc(8 * 2 * D, "Wz")
            Wgl = ABa.alloc(8 * 2 * D, "Wgl")
            Wbs = ABa.alloc(8 * D, "Wbs")
            wsT = ABa.alloc(8 * 128, "wsT")
            Wz3 = load_w_bf16(Wz, w_in[:, 3 * D:5 * D], 2 * D)
            Wgl3 = load_w_bf16(Wgl, w_in[:, 5 * D:7 * D], 2 * D)
            Wbs3 = load_w_bf16(Wbs, w_branch_sgu, D)
            wsT3 = wsT.ap.rearrange("q (g p) -> q g p", g=8)
            for g in range(8):
                P.dma("pool", wsT3[:, g, :], sgu_wT[g, :, :], writes=[wsT])
            lng = AFa.alloc(D, "lng")
            lnb = AFa.alloc(D, "lnb")
            P.dma("sp", lng.ap, sgu_ln_g.partition_broadcast(128), writes=[lng])
            P.dma("sp", lnb.ap, sgu_ln_b.partition_broadcast(128), writes=[lnb])
            hb = ABa.alloc(8 * 512, "hb")
            hb3 = hb.ap.rearrange("p (k t) -> p k t", k=8)
            sT_ = ABa.alloc(8 * 512, "sgT")
            sT3 = sT_.ap.rearrange("p (k t) -> p k t", k=8)
            zg = [AFa.alloc(2 * D, f"zg{i}") for i in range(2)]
            vn = AFa.alloc(D, "vn")
            vb_ = ABa.alloc(D, "vnb")
            sgb = [ABa.alloc(D, f"sgb{i}") for i in range(2)]
            junkf = AFa.alloc(D, "junkf")
            st = [AFa.alloc(8, f"st{i}") for i in range(2)]
            gs = [AFa.alloc(512, f"gs{i}") for i in range(2)]
            po = [ABa.alloc(512, f"po{i}") for i in range(2)]
            hsrc_v = hT_own.ap.rearrange("(k p) t -> p k t", p=128)
            npo = 0
            for bi in range(8):
                P.dma("sp", hb3, hsrc_v[:, :, bi * 512:(bi + 1) * 512], reads=[hT_own], writes=[hb])
                for tl in range(4):
                    z_ = zg[tl % 2]
                    s_ = st[tl % 2]
                    for cb in range(4):
                        pz = PS[cb]
                        for dc in range(8):
                            MM(pz.ap, hb3[:, dc, tl * 128:(tl + 1) * 128], Wz3[:, dc, cb * 512:(cb + 1) * 512],
                               dc == 0, dc == 7, [hb, Wz], [pz])
                        ACT(z_.ap[:, cb * 512:(cb + 1) * 512], pz.ap, AF.Gelu, [pz], [z_])
                    v_ap = z_.ap[:, D:2 * D]
                    RED(s_.ap[:, 0:1], v_ap, ALU.add, [z_], [s_])
                    ACT(junkf.ap, v_ap, AF.Square, [z_], [junkf, s_], accum=s_.ap[:, 1:2])
                    TS("dve", s_.ap[:, 2:3], s_.ap[:, 0:1], 1.0 / D, None, ALU.mult, None, [s_], [s_])
                    TT("dve", s_.ap[:, 3:4], s_.ap[:, 2:3], s_.ap[:, 2:3], ALU.mult, [s_], [s_])
                    STT(s_.ap[:, 4:5], s_.ap[:, 1:2], 1.0 / D, s_.ap[:, 3:4], ALU.mult, ALU.subtract, [s_], [s_])
                    rstd(s_, s_.ap[:, 4:5], s_.ap[:, 5:6], 1.0, 1e-5)
                    TS("dve", vn.ap, v_ap, s_.ap[:, 2:3], s_.ap[:, 5:6], ALU.subtract, ALU.mult, [z_, s_], [vn])
                    TT("pool", vn.ap, vn.ap, lng.ap, ALU.mult, [vn, lng], [vn])
                    TT("pool", vb_.ap, vn.ap, lnb.ap, ALU.add, [vn, lnb], [vb_])
                    pm, pmb = ps2(2)
                    for g in range(8):
                        MM(pm[:, g * 128:(g + 1) * 128], wsT3[:, g, :], vb_.ap[:, g * 128:(g + 1) * 128], True, True,
                           [wsT, vb_], [pmb[g // 4]])
                    sg_ = sgb[tl % 2]
                    for g in range(8):
                        STT(sg_.ap[:, g * 128:(g + 1) * 128], pm[:, g * 128:(g + 1) * 128], sbT.ap[:, g:g + 1],
                            z_.ap[:, g * 128:(g + 1) * 128], ALU.add, ALU.mult, [pmb[g // 4], sbT, z_], [sg_])
                    pt, ptb = ps2(3)
                    for fc in range(8):
                        TR(pt[:, fc * 128:(fc + 1) * 128], sg_.ap[:, fc * 128:(fc + 1) * 128], [sg_], [ptb[fc // 4]])
                    for hf in range(2):
                        COPY("act", sT3[:, hf * 4:(hf + 1) * 4, tl * 128:(tl + 1) * 128],
                             pt[:, hf * 512:(hf + 1) * 512].rearrange("p (k t) -> p k t", k=4), [ptb[hf]], [sT_])
                for fc in range(8):
                    pY = PS[0 + fc % 2]
                    pG = PS[2 + fc % 2]
                    pH = PS[4 + fc % 2]
                    for dc in range(8):
                        MM(pY.ap, Wbs3[:, dc, fc * 128:(fc + 1) * 128], sT3[:, dc, :], dc == 0, dc == 7, [Wbs, sT_], [pY])
                    for dc in range(8):
                        MM(pG.ap, Wgl3[:, dc, D + fc * 128:D + (fc + 1) * 128], hb3[:, dc, :], dc == 0, dc == 7, [Wgl, hb], [pG])
                    for dc in range(8):
                        MM(pH.ap, Wgl3[:, dc, fc * 128:(fc + 1) * 128], hb3[:, dc, :], dc == 0, dc == 7, [Wgl, hb], [pH])
                    g_ = gs[fc % 2]
                    ACT(g_.ap, pG.ap, AF.Sigmoid, [pG, bgT], [g_], bias=bgT.ap[:, 8 + fc:9 + fc])
                    o1 = po[npo % 2]
                    npo += 1
                    TT("dve", o1.ap, pY.ap, g_.ap, ALU.mult, [pY, g_], [o1])
                    P.dma("pool", pST.ap[fc * 128:(fc + 1) * 128, bi * 512:(bi + 1) * 512], o1.ap, reads=[o1], writes=[pST])
                    o2 = po[npo % 2]
                    npo += 1
                    ACT(o2.ap, pH.ap, AF.Sigmoid, [pH, bgT], [o2], bias=bgT.ap[:, fc:fc + 1])
                    P.dma("pool", gAT.ap[fc * 128:(fc + 1) * 128, bi * 512:(bi + 1) * 512], o2.ap, reads=[o2], writes=[gAT])
            end_phase()

        if "C" in phases:
            KTab = ABa.alloc(LK, "KTab")
            QTa = ABa.alloc(NQ, "QTa")
            QTb = ABa.alloc(NQ, "QTb")
            Vh = ABa.alloc(66 * 128, "Vh")
            Vh3 = Vh.ap.rearrange("p (k e) -> p k e", e=128)
            PT = [ABa.alloc(512, f"PT{i}") for i in range(6)]
            ono = [ABa.alloc(512, f"ono{i}") for i in range(2)]
            r_ = [AFa.alloc(512, f"r{i}") for i in range(2)]
            tt = [AFa.alloc(512, f"tt{i}") for i in range(2)]
            zacc = [AFa.alloc(512, f"zacc{i}") for i in range(2)]
            zacp = [AFa.alloc(512, f"zacp{i}") for i in range(2)]
            o_ = AFa.alloc(512, "o_")
            sq = AFa.alloc(512, "sq")
            rs = AFa.alloc(512, "rs")
            Vs_v = Vs.ap.rearrange("(kt k) e -> k kt e", k=128)
            NKT = LK // 128
            Ops = (PS[3], PS[4])
            Zps = (PS[5], PS[6])
            MEMSET("dve", QTa.ap[64:128, :], 0.0, [QTa])
            MEMSET("dve", QTb.ap[0:64, :], 0.0, [QTb])
            for h in range(8):
                P.dma("sp", KTab.ap, KT.ap[h * 128:(h + 1) * 128, :], reads=[KT], writes=[KTab])
                P.dma("act", QTa.ap[0:64, :], QT.ap[(2 * h) * 64:(2 * h + 1) * 64, :], reads=[QT], writes=[QTa])
                P.dma("act", QTb.ap[64:128, :], QT.ap[(2 * h + 1) * 64:(2 * h + 2) * 64, :], reads=[QT], writes=[QTb])
                for kq in range(6):
                    P.dma("sp", Vh3[:, kq * 11:(kq + 1) * 11, :], Vs_v[:, kq * 11:(kq + 1) * 11, h * 128:(h + 1) * 128],
                          reads=[Vs], writes=[Vh])
                for qb in range(8):
                    steps = [(j, kt) for j in range(2) for kt in range(NKT)]
                    LOOK = 2

                    def av(n):
                        j, kt = steps[n]
                        p_ = PT[n % 6]
                        MM(Ops[j].ap, Vh3[:, kt, :], p_.ap, kt == 0, kt == NKT - 1, [Vh, p_], [Ops[j]])
                        if kt % 3 == 2:
                            MM(Zps[j].ap, ones_b.ap, p_.ap, kt == 2, False, [ones_b, p_], [Zps[j]])
                        if kt == NKT - 1:
                            MM(Zps[j].ap, ones_f.ap, zacc[j].ap, False, True, [ones_f, zacc[j]], [Zps[j]])

                    for n in range(len(steps) + LOOK):
                        if n < len(steps):
                            j, kt = steps[n]
                            Qt_ = QTa if j == 0 else QTb
                            sp_ = PS[n % 3]
                            p_ = PT[n % 6]
                            MM(sp_.ap, KTab.ap[:, kt * 128:(kt + 1) * 128], Qt_.ap[:, qb * 512:(qb + 1) * 512], True, True,
                               [KTab, Qt_], [sp_])
                            ACT(p_.ap, sp_.ap, AF.Exp, [sp_], [p_], scale=0.125)
                            if kt % 3 == 2:
                                pass
                            elif kt == 0:
                                COPY("dve", zacc[j].ap, p_.ap, [p_], [zacc[j]])
                            else:
                                TT("dve", zacc[j].ap, zacc[j].ap, p_.ap, ALU.add, [zacc[j], p_], [zacc[j]])
                        if n >= LOOK:
                            av(n - LOOK)
                    for j in range(2):
                        RECIP(r_[j].ap, Zps[j].ap, [Zps[j]], [r_[j]])
                        TT("dve", tt[j].ap, Ops[j].ap, r_[j].ap, ALU.mult, [Ops[j], r_[j]], [tt[j]])
                    STT(o_.ap, tt[1].ap, neg_lam.ap[:, 0:1], tt[0].ap, ALU.mult, ALU.add, [tt[0], tt[1], neg_lam], [o_])
                    TT("pool", sq.ap, o_.ap, o_.ap, ALU.mult, [o_], [sq])
                    MM(PS[7].ap, ones_f.ap, sq.ap, True, True, [ones_f, sq], [PS[7]])
                    TS("dve", rs.ap, PS[7].ap, 1.0 / 128, RMS_EPS, ALU.mult, ALU.add, [PS[7]], [rs])
                    SQRT(rs.ap, rs.ap, [rs], [rs])
                    RECIP(rs.ap, rs.ap, [rs], [rs])
                    oo = ono[qb % 2]
                    STT(oo.ap, o_.ap, sgT.ap[:, 0:1], rs.ap, ALU.mult, ALU.mult, [o_, sgT, rs], [oo])
                    P.dma("pool", onT.ap[h * 128:(h + 1) * 128, qb * 512:(qb + 1) * 512], oo.ap, reads=[oo], writes=[onT])
            end_phase()

        if "E" in phases:
            Wba = ABa.alloc(8 * D, "Wba")
            Wo = ABa.alloc(8 * D, "Wo")
            Wba3 = load_w_bf16(Wba, w_branch_attn, D)
            Wo3 = load_w_bf16(Wo, w_out, D)
            onb = [ABa.alloc(8 * 512, f"onb{i}") for i in range(2)]
            gab = [ABa.alloc(8 * 512, f"gab{i}") for i in range(2)]
            psb = [ABa.alloc(8 * 512, f"psb{i}") for i in range(2)]
            mT = ABa.alloc(8 * 512, "mT")
            mT3 = mT.ap.rearrange("p (k t) -> p k t", k=8)
            h2_ = ABa.alloc(8 * 512, "h2b")
            h23 = h2_.ap.rearrange("p (k t) -> p k t", k=8)
            xhb = [ABa.alloc(D, f"xhb{i}") for i in range(2)]
            junk = AFa.alloc(D, "junk")
            mtmp = [AFa.alloc(512, f"mtmp{i}") for i in range(2)]
            xt = [AFa.alloc(D, f"xt{i}") for i in range(2)]
            tq = [AFa.alloc(D, f"tq{i}") for i in range(2)]
            xnw = [AFa.alloc(D, f"xnw{i}") for i in range(2)]
            st = [AFa.alloc(8, f"st{i}") for i in range(2)]
            v_on = onT.ap.rearrange("(k p) t -> p k t", p=128)
            v_ga = gAT.ap.rearrange("(k p) t -> p k t", p=128)
            v_ps = pST.ap.rearrange("(k p) t -> p k t", p=128)
            v_h2 = H2T.ap.rearrange("(k p) t -> p k t", p=128)
            for bi in range(8):
                cs = slice(bi * 512, (bi + 1) * 512)
                on_ = onb[bi % 2]
                ga_ = gab[bi % 2]
                ps_ = psb[bi % 2]
                on3 = on_.ap.rearrange("p (k t) -> p k t", k=8)
                ga3 = ga_.ap.rearrange("p (k t) -> p k t", k=8)
                pS3 = ps_.ap.rearrange("p (k t) -> p k t", k=8)
                P.dma("sp", on3, v_on[:, :, cs], reads=[onT], writes=[on_])
                P.dma("act", ga3, v_ga[:, :, cs], reads=[gAT], writes=[ga_])
                P.dma("sp", pS3, v_ps[:, :, cs], reads=[pST], writes=[ps_])
                for fc in range(8):
                    pY = PS[fc % 2]
                    for hc in range(8):
                        MM(pY.ap, Wba3[:, hc, fc * 128:(fc + 1) * 128], on3[:, hc, :], hc == 0, hc == 7, [Wba, on_], [pY])
                    m_ = mtmp[fc % 2]
                    TT("dve", m_.ap, pY.ap, ga3[:, fc, :], ALU.mult, [pY, ga_], [m_])
                    TT("pool", mT3[:, fc, :], m_.ap, pS3[:, fc, :], ALU.add, [m_, ps_], [mT])
                for tl in range(4):
                    ti = bi * 4 + tl
                    x_ = xt[tl % 2]
                    s_ = st[tl % 2]
                    P.dma("sp", x_.ap, xown[ti * 128:(ti + 1) * 128, :], writes=[x_])
                    py, pyb = ps2(1 + tl % 2)
                    for half in range(2):
                        hs = slice(half * 512, (half + 1) * 512)
                        for mc in range(8):
                            MM(py[:, hs], mT3[:, mc, tl * 128:(tl + 1) * 128], Wo3[:, mc, hs], mc == 0, mc == 7,
                               [mT, Wo], [pyb[half]])
                        ACT(junk.ap[:, hs], py[:, hs], AF.Square, [pyb[half]], [junk, s_], accum=s_.ap[:, 4 + half:5 + half])
                    TT("dve", s_.ap[:, 0:1], s_.ap[:, 4:5], s_.ap[:, 5:6], ALU.add, [s_], [s_])
                    rstd(s_, s_.ap[:, 0:1], s_.ap[:, 1:2], 1.0 / D, RMS_EPS)
                    q_ = tq[tl % 2]
                    for half in range(2):
                        hs = slice(half * 512, (half + 1) * 512)
                        STT(q_.ap[:, hs], py[:, hs], s_.ap[:, 1:2], G1.ap[:, hs], ALU.mult, ALU.mult, [pyb[half], s_, G1], [q_])
                    xn_ = xnw[tl % 2]
                    TT("pool", xn_.ap, q_.ap, x_.ap, ALU.add, [q_, x_], [xn_])
                    P.dma("pool", XN.ap[ti * 128:(ti + 1) * 128, :], xn_.ap, reads=[xn_], writes=[XN])
                    ACT(junk.ap, xn_.ap, AF.Square, [xn_], [junk, s_], accum=s_.ap[:, 2:3])
                    rstd(s_, s_.ap[:, 2:3], s_.ap[:, 3:4], 1.0 / D, RMS_EPS)
                    xh_ = xhb[tl % 2]
                    TS("dve", xh_.ap, xn_.ap, s_.ap[:, 3:4], None, ALU.mult, None, [xn_, s_], [xh_])
                    pt, ptb = ps2(3)
                    for dc in range(8):
                        TR(pt[:, dc * 128:(dc + 1) * 128], xh_.ap[:, dc * 128:(dc + 1) * 128], [xh_], [ptb[dc // 4]])
                    for dc in range(8):
                        o = h23[:, dc, tl * 128:(tl + 1) * 128]
                        i_ = pt[:, dc * 128:(dc + 1) * 128]
                        if dc // 4 == 0:
                            ACT(o, i_, AF.Identity, [ptb[dc // 4], a2T, modT], [h2_], bias=b2T(dc), scale=a2T.ap[:, dc:dc + 1])
                        else:
                            TS("dve", o, i_, a2T.ap[:, dc:dc + 1], b2T(dc), ALU.mult, ALU.add, [ptb[dc // 4], a2T, modT], [h2_])
                P.dma("pool", v_h2[:, :, cs], h23, reads=[h2_], writes=[H2T])
            end_phase()

        if "F" in phases:
            ub = [ABa.alloc(D, f"ub{i}") for i in range(3)]
            vb = [ABa.alloc(D, f"vb{i}") for i in range(3)]
            utb = [ABa.alloc(D, f"utb{i}") for i in range(3)]
            for i in range(128):
                u_ = ub[i % 3]
                v_ = vb[i % 3]
                t_ = utb[i % 3]
                P.dma("pool", u_.ap, peer_u[i * 128:(i + 1) * 128, :], writes=[u_])
                P.dma("pool", v_.ap, peer_v[i * 128:(i + 1) * 128, :], writes=[v_])
                P.dma("sp", VB.ap[i * 128:(i + 1) * 128, :], v_.ap, reads=[v_], writes=[VB])
                pt, ptb = ps2(i % 4)
                for dc in range(8):
                    TR(pt[:, dc * 128:(dc + 1) * 128], u_.ap[:, dc * 128:(dc + 1) * 128], [u_], [ptb[dc // 4]])
                for hf in range(2):
                    COPY("act" if hf == 0 else "dve", t_.ap[:, hf * 512:(hf + 1) * 512], pt[:, hf * 512:(hf + 1) * 512],
                         [ptb[hf]], [t_])
                P.dma("sp", UT.ap[i, :, :], t_.ap, reads=[t_], writes=[UT])
            end_phase()

        if "F" in phases:
            Wpq = ABa.alloc(8 * 2 * D, "Wpq")
            Wpq3 = load_w_bf16(Wpq, peer_wq, 2 * D)
            kT = ABa.alloc(256, "kT")
            kT3 = kT.ap.rearrange("d (p n) -> d p n", p=2)
            for p_i in range(2):
                P.dma("pool", kT3[:, p_i, :], peer_keysT[p_i, :, :], writes=[kT])
            h2t = [ABa.alloc(8 * 128, f"h2t{i}") for i in range(2)]
            qn = ABa.alloc(2 * D, "qn")
            qnT = ABa.alloc(2 * D, "qnT")
            qnT3 = qnT.ap.rearrange("d (g t) -> d g t", g=16)
            sqf = AFa.alloc(2 * D, "sqf")
            ssb = [AFa.alloc(2 * D, f"s_sb{i}") for i in range(2)]
            tv = AFa.alloc(256, "tv")
            tv3 = tv.ap.rearrange("p (g a) -> p g a", g=16)
            tv4 = tv.ap.rearrange("p (h two a) -> p h two a", h=8, two=2)
            workg = [AFa.alloc(128, f"work{g}") for g in range(16)]
            tvg = [Buf(f"tvg{g}") for g in range(16)]
            c24g = [Buf(f"c24g{h}") for h in range(8)]
            cw0 = [AFa.alloc(256, f"cw0_{h}") for h in range(8)]
            cw1 = [AFa.alloc(256, f"cw1_{h}") for h in range(8)]
            cand = AFa.alloc(2 * D, "cand")
            cand3 = cand.ap.rearrange("p (h c) -> p h c", h=8)
            cw = [AFa.alloc(256, f"cw{i}") for i in range(2)]
            c24 = AFa.alloc(8 * 24, "c24")
            c243 = c24.ap.rearrange("p (h c) -> p h c", h=8)
            rg = AFa.alloc(16, "rg")
            smb = [AFa.alloc(16, f"smb{i}") for i in range(2)]
            zz = AFa.alloc(8, "zz")
            e16 = AFa.alloc(128, "e16")
            v_h2 = H2T.ap.rearrange("(k p) t -> p k t", p=128)
            for ti in range(NQ // 128):
                h2_ = h2t[ti % 2]
                h23 = h2_.ap.rearrange("p (k t) -> p k t", k=8)
                s_sb = ssb[ti % 2]
                s3 = s_sb.ap.rearrange("p (g n) -> p g n", g=16)
                sm = smb[ti % 2]
                P.dma("sp", h23, v_h2[:, :, ti * 128:(ti + 1) * 128], reads=[H2T], writes=[h2_])
                for cb in range(4):
                    for dc in range(8):
                        MM(PS[cb].ap, h23[:, dc, :], Wpq3[:, dc, cb * 512:(cb + 1) * 512], dc == 0, dc == 7, [h2_, Wpq], [PS[cb]])
                    ACT(sqf.ap[:, cb * 512:(cb + 1) * 512], PS[cb].ap, AF.Square, [PS[cb]], [sqf])
                RED(rg.ap, sqf.ap.rearrange("p (g n) -> p g n", g=16), ALU.add, [sqf], [rg])
                rstd(rg, rg.ap, rg.ap, 1.0 / 128, RMS_EPS)
                for cb in range(4):
                    TT("dve", qn.ap[:, cb * 512:(cb + 1) * 512].rearrange("p (g n) -> p g n", g=4),
                       PS[cb].ap.rearrange("p (g n) -> p g n", g=4),
                       rg.ap[:, cb * 4:(cb + 1) * 4].unsqueeze(2).to_broadcast([128, 4, 128]), ALU.mult, [PS[cb], rg], [qn])
                for g in range(16):
                    pb = PS[4 + g // 4]
                    TR(pb.ap[:, (g % 4) * 128:(g % 4 + 1) * 128], qn.ap[:, g * 128:(g + 1) * 128], [qn], [pb])
                for c4 in range(4):
                    COPY("act" if c4 % 2 == 0 else "dve", qnT3[:, c4 * 4:(c4 + 1) * 4, :],
                         PS[4 + c4].ap.rearrange("p (g t) -> p g t", g=4), [PS[4 + c4]], [qnT])
                for g in range(16):
                    pb = PS[g // 4]
                    MM(pb.ap[:, (g % 4) * 128:(g % 4 + 1) * 128], qnT3[:, g, :], kT3[:, g % 2, :], True, True, [qnT, kT], [pb])
                for c4 in range(4):
                    COPY("act", s_sb.ap[:, c4 * 512:(c4 + 1) * 512], PS[c4].ap, [PS[c4]], [s_sb])
                P.dma("pool", SS.ap[ti * 128:(ti + 1) * 128, :], s_sb.ap, reads=[s_sb], writes=[SS])
                for g in range(16):
                    MAX8(tv3[:, g, 0:8], s3[:, g, :], [s_sb], [tvg[g]])
                for g in range(16):
                    MREP(workg[g].ap, tv3[:, g, 0:8], s3[:, g, :], [s_sb, tvg[g]], [workg[g]])
                for g in range(16):
                    MAX8(tv3[:, g, 8:16], workg[g].ap, [workg[g]], [tvg[g]])
                TT("dve", cand.ap.rearrange("p (h a b) -> p h a b", h=8, a=16),
                   tv4[:, :, 0, :].unsqueeze(3).to_broadcast([128, 8, 16, 16]),
                   tv4[:, :, 1, :].unsqueeze(2).to_broadcast([128, 8, 16, 16]), ALU.add, tvg, [cand])
                for h in range(8):
                    MAX8(c243[:, h, 0:8], cand3[:, h, :], [cand], [c24g[h]])
                for h in range(8):
                    MREP(cw0[h].ap, c243[:, h, 0:8], cand3[:, h, :], [cand, c24g[h]], [cw0[h]])
                for h in range(8):
                    MAX8(c243[:, h, 8:16], cw0[h].ap, [cw0[h]], [c24g[h]])
                for h in range(8):
                    MREP(cw1[h].ap, c243[:, h, 8:16], cw0[h].ap, [cw0[h], c24g[h]], [cw1[h]])
                for h in range(8):
                    MAX8(c243[:, h, 16:24], cw1[h].ap, [cw1[h]], [c24g[h]])
                TT("dve", sm.ap[:, 0:8], c243[:, :, 15], c243[:, :, 16], ALU.add, c24g, [sm])
                TS("dve", sm.ap[:, 0:8], sm.ap[:, 0:8], 0.5, None, ALU.mult, None, [sm], [sm])
                TT("dve", e16.ap.rearrange("p (h k) -> p h k", h=8), c243[:, :, 0:16],
                   c243[:, :, 0:1].to_broadcast([128, 8, 16]), ALU.subtract, c24g, [e16])
                ACT(e16.ap, e16.ap, AF.Exp, [e16], [e16])
                RED(zz.ap, e16.ap.rearrange("p (h k) -> p h k", h=8), ALU.add, [e16], [zz])
                ACT(zz.ap, zz.ap, AF.Ln, [zz], [zz])
                TT("dve", sm.ap[:, 8:16], zz.ap, c243[:, :, 0], ALU.add, [zz] + c24g, [sm])
                TS("dve", sm.ap[:, 8:16], sm.ap[:, 8:16], -1.0, None, ALU.mult, None, [sm], [sm])
                P.dma("pool", SM.ap[ti * 128:(ti + 1) * 128, :], sm.ap, reads=[sm], writes=[SM])
            end_phase()

        if "F" in phases:
            GTall = ABa.alloc(128 * 256, "GT")
            GT3 = GTall.ap.rearrange("j (i t) -> j i t", t=256)
            GTg = [Buf(f"GTg{g}") for g in range(16)]
            h2blk = [ABa.alloc(8 * 256, f"h2blk{i}") for i in range(2)]
            Mb = [ABa.alloc(1024, f"Mb{i}") for i in range(3)]
            utc = [ABa.alloc(D, f"utc{i}") for i in range(4)]
            vbc = [ABa.alloc(D, f"vbc{i}") for i in range(4)]
            agb = [ABa.alloc(256, f"agb{i}") for i in range(3)]
            ssb = [AFa.alloc(2 * D, f"s_sb{i}") for i in range(2)]
            smb = [AFa.alloc(16, f"smb{i}") for i in range(2)]
            Sf = [AFa.alloc(1024, f"Sf{i}") for i in range(3)]
            Wf = [AFa.alloc(1024, f"Wf{i}") for i in range(2)]
            af = [AFa.alloc(256, f"af{i}") for i in range(2)]
            xnl = [AFa.alloc(D, f"xnl{i}") for i in range(2)]
            yq = [AFa.alloc(D, f"yq{i}") for i in range(2)]
            yo = [AFa.alloc(D, f"yo{i}") for i in range(2)]
            st = [AFa.alloc(8, f"st{i}") for i in range(2)]
            junk = AFa.alloc(D, "junk")
            v_h2 = H2T.ap.rearrange("(k p) t -> p k t", p=128)
            cnt = {"S": 0, "u": 0}
            NST = NQ // 256

            def load_scores(sti):
                for tl in range(2):
                    ti = sti * 2 + tl
                    P.dma("sp", ssb[tl].ap, SS.ap[ti * 128:(ti + 1) * 128, :], reads=[SS], writes=[ssb[tl]])
                    P.dma("sp", smb[tl].ap, SM.ap[ti * 128:(ti + 1) * 128, :], reads=[SM], writes=[smb[tl]])

            def gbuild(tl, ib):
                tcs = slice(tl * 128, (tl + 1) * 128)
                s_sb = ssb[tl]
                sm = smb[tl]
                s3 = s_sb.ap.rearrange("p (g n) -> p g n", g=16)
                gp, gpb = ps2(2)
                for h in range(8):
                    n = cnt["S"]
                    cnt["S"] += 1
                    S_ = Sf[n % 3]
                    W_ = Wf[n % 2]
                    M_ = Mb[n % 3]
                    TT("dve" if h % 4 == 3 else "pool", S_.ap.rearrange("p (i j) -> p i j", i=8),
                       s3[:, 2 * h, ib * 8:(ib + 1) * 8].unsqueeze(2).to_broadcast([128, 8, 128]),
                       s3[:, 2 * h + 1:2 * h + 2, :].to_broadcast([128, 8, 128]), ALU.add, [s_sb], [S_])
                    ACT(W_.ap, S_.ap, AF.Exp, [S_, sm], [W_], bias=sm.ap[:, 8 + h:9 + h])
                    STT(M_.ap, S_.ap, sm.ap[:, h:h + 1], W_.ap, ALU.is_ge, ALU.mult, [S_, W_, sm], [M_])
                    for ii in range(8):
                        MMG(gp[:, ii * 128:(ii + 1) * 128], M_.ap[:, ii * 128:(ii + 1) * 128], ident_b.ap,
                            h == 0 and ii % 4 == 0, h == 7, [M_, ident_b], [gpb[ii // 4]])
                for hf in range(2):
                    COPY("act" if hf == 0 else "dve", GT3[:, ib * 8 + hf * 4:ib * 8 + hf * 4 + 4, tcs],
                         gp[:, hf * 512:(hf + 1) * 512].rearrange("p (i t) -> p i t", i=4), [gpb[hf]], [GTg[ib]])

            def dense(sti, i):
                hb_ = h2blk[sti % 2]
                h23 = hb_.ap.rearrange("p (k t) -> p k t", k=8)
                n = cnt["u"]
                cnt["u"] += 1
                u_ = utc[n % 4]
                v_ = vbc[n % 4]
                u3 = u_.ap.rearrange("p (k j) -> p k j", k=8)
                P.dma("sp", u_.ap, UT.ap[i, :, :], reads=[UT], writes=[u_])
                P.dma("act" if i % 2 == 0 else "sp", v_.ap, VB.ap[i * 128:(i + 1) * 128, :], reads=[VB], writes=[v_])
                pa = PS[6 + n % 2]
                for dc in range(8):
                    MM(pa.ap[:, 0:256], u3[:, dc, :], h23[:, dc, :], dc == 0, dc == 7, [u_, hb_], [pa])
                a_ = af[n % 2]
                ACT(a_.ap, pa.ap[:, 0:256], AF.Gelu, [pa], [a_])
                g_ = agb[n % 3]
                TT("dve", g_.ap, a_.ap, GT3[:, i, :], ALU.mult, [a_, GTg[i // 8]], [g_])
                return (g_, v_, i)

            def dense_out(st_):
                g_, v_, i = st_
                for tl in range(2):
                    for half in range(2):
                        po_ = PS[tl * 2 + half]
                        MM(po_.ap, g_.ap[:, tl * 128:(tl + 1) * 128], v_.ap[:, half * 512:(half + 1) * 512], i == 0, i == 127,
                           [g_, v_], [po_])

            def epilogue(sti):
                for tl in range(2):
                    ti = sti * 2 + tl
                    py, pyb = ps2(tl)
                    s_ = st[tl]
                    x_ = xnl[tl]
                    P.dma("sp", x_.ap, XN.ap[ti * 128:(ti + 1) * 128, :], reads=[XN], writes=[x_])
                    for half in range(2):
                        hs = slice(half * 512, (half + 1) * 512)
                        ACT(junk.ap[:, hs], py[:, hs], AF.Square, [pyb[half]], [junk, s_], accum=s_.ap[:, 4 + half:5 + half])
                    TT("dve", s_.ap[:, 0:1], s_.ap[:, 4:5], s_.ap[:, 5:6], ALU.add, [s_], [s_])
                    rstd(s_, s_.ap[:, 0:1], s_.ap[:, 1:2], 1.0 / D, RMS_EPS)
                    q_ = yq[tl]
                    for half in range(2):
                        hs = slice(half * 512, (half + 1) * 512)
                        STT(q_.ap[:, hs], py[:, hs], s_.ap[:, 1:2], G2.ap[:, hs], ALU.mult, ALU.mult, [pyb[half], s_, G2], [q_])
                    o_ = yo[tl]
                    TT("pool", o_.ap, q_.ap, x_.ap, ALU.add, [q_, x_], [o_])
                    P.dma("pool", Y.ap[ti * 128:(ti + 1) * 128, :], o_.ap, reads=[o_], writes=[Y])

            def load_h2(sti):
                hb_ = h2blk[sti % 2]
                P.dma("sp", hb_.ap.rearrange("p (k t) -> p k t", k=8), v_h2[:, :, sti * 256:(sti + 1) * 256],
                      reads=[H2T], writes=[hb_])

            load_h2(0)
            load_scores(0)
            for ib in range(16):
                for tl in range(2):
                    gbuild(tl, ib)
            for sti in range(NST):
                if sti + 1 < NST:
                    load_h2(sti + 1)
                    load_scores(sti + 1)
                prev = None
                for ib in range(16):
                    for i in range(ib * 8, (ib + 1) * 8):
                        cur = dense(sti, i)
                        if prev is not None:
                            dense_out(prev)
                        prev = cur
                    if sti + 1 < NST:
                        for tl in range(2):
                            gbuild(tl, ib)
                dense_out(prev)
                epilogue(sti)
            AFa.reset(fmark)
            ABa.reset(bmark)

        if dbg:
            for name, (t, n) in list(dbg_outs.items()):
                o = nc.dram_tensor("dbg_" + name, [128, n], F32, kind="ExternalOutput").ap()
                P.dma("sp", o, t.ap[:, 0:n], reads=[t], writes=[Buf("dbgo_" + name)])
            for name, t in (("hT_all", hT_all), ("hT_own", hT_own), ("KT", KT), ("Vs", Vs), ("QT", QT), ("pST", pST),
                            ("gAT", gAT), ("onT", onT), ("XN", XN), ("H2T", H2T), ("SS", SS), ("SM", SM)):
                if name not in dbg:
                    continue
                shp = list(t.ap.shape)
                o = nc.dram_tensor("dbg_" + name, shp, t.ap.dtype, kind="ExternalOutput").ap()
                P.dma("sp", o, t.ap, reads=[t], writes=[Buf("dbgo_" + name)])

        P.barrier()
        P.emit()
    return nc, P


def _rope_tables():
    f = np.arange(64)
    blk = f // 32
    within = f % 32
    i = within % 16
    first = within < 16
    inv = (10000.0 ** (-(np.arange(16, dtype=np.float32)) / 16.0)).astype(np.float32)
    t = np.arange(L)
    rows = (t // 64).astype(np.float32)
    cols = (t % 64).astype(np.float32)
    pos = np.where(blk[:, None] == 0, rows[None, :], cols[None, :]).astype(np.float32)
    ang = (pos * inv[i][:, None]).astype(np.float32)
    C = np.cos(ang).astype(np.float32)
    S = (np.sin(ang) * np.where(first, -1.0, 1.0)[:, None]).astype(np.float32)
    partner = np.where(first, f + 16, f - 16)
    C128 = np.concatenate([C, C], axis=0)
    S128 = np.concatenate([S, S], axis=0)
    return np.ascontiguousarray(C128), np.ascontiguousarray(S128), partner


_CACHE = {}


def _get_program():
    if "nc" not in _CACHE:
        _CACHE["nc"] = build_program()[0]
    return _CACHE["nc"]


def make_in_maps(inputs):
    f32 = lambda a: np.ascontiguousarray(np.asarray(a, dtype=np.float32))
    x = f32(inputs["x"])
    c = f32(inputs["c"])
    ctx = f32(inputs["ctx"])
    c_ctx = f32(inputs["c_ctx"])
    C128, S128, partner = _rope_tables()
    w_in = f32(inputs["w_in"][0])
    perm = np.concatenate([(np.arange(16)[:, None] * 64 + partner[None, :]).reshape(-1),
                           1024 + (np.arange(16)[:, None] * 64 + partner[None, :]).reshape(-1)])
    w_qk_sw = np.ascontiguousarray(w_in[:, perm])
    lam_vecs = np.concatenate([f32(inputs["lambda_q1"][0]), f32(inputs["lambda_k1"][0]),
                               f32(inputs["lambda_q2"][0]), f32(inputs["lambda_k2"][0])]).reshape(1, 256)
    shared = {
        "w_ada": f32(inputs["w_ada"][0]), "b_ada": f32(inputs["b_ada"][0]).reshape(1, -1),
        "g_pre_mix": f32(inputs["g_pre_mix"][0]).reshape(1, -1), "g_post_mix": f32(inputs["g_post_mix"][0]).reshape(1, -1),
        "g_pre_ffn": f32(inputs["g_pre_ffn"][0]).reshape(1, -1), "g_post_ffn": f32(inputs["g_post_ffn"][0]).reshape(1, -1),
        "w_in": w_in, "w_qk_sw": w_qk_sw, "b_gate": f32(inputs["b_gate"][0]).reshape(1, -1),
        "lam_vecs": np.ascontiguousarray(lam_vecs), "subln_g": f32(inputs["subln_g"][0]).reshape(1, -1),
        "sgu_ln_g": f32(inputs["sgu_ln_g"][0]).reshape(1, -1), "sgu_ln_b": f32(inputs["sgu_ln_b"][0]).reshape(1, -1),
        "sgu_wT": np.ascontiguousarray(np.transpose(f32(inputs["sgu_w"][0]), (0, 2, 1))),
        "sgu_b": f32(inputs["sgu_b"][0]),
        "w_branch_attn": f32(inputs["w_branch_attn"][0]), "w_branch_sgu": f32(inputs["w_branch_sgu"][0]),
        "w_out": f32(inputs["w_out"][0]), "peer_wq": f32(inputs["peer_w_query"][0]),
        "peer_keysT": np.ascontiguousarray(np.transpose(f32(inputs["peer_sub_keys"][0]), (0, 2, 1))),
        "peer_u": f32(inputs["peer_u"][0]), "peer_v": f32(inputs["peer_v"][0]),
        "ropeC": C128, "ropeS": S128,
    }
    in_maps = []
    for core in range(8):
        b, half = core // 2, core % 2
        m = dict(shared)
        m["xb"] = x[b]
        m["xown"] = np.ascontiguousarray(x[b, half * NQ:(half + 1) * NQ])
        m["ctxb"] = ctx[b]
        m["cvec"] = np.ascontiguousarray(np.stack([c[b], c_ctx]))
        m["ropeCq"] = np.ascontiguousarray(C128[:, half * NQ:(half + 1) * NQ])
        m["ropeSq"] = np.ascontiguousarray(S128[:, half * NQ:(half + 1) * NQ])
        in_maps.append(m)
    return in_maps


def kernel(**inputs):
    nc = _get_program()
    in_maps = make_in_maps(inputs)
    res = run_bass_kernel_spmd(nc, in_maps, core_ids=list(range(8)))
    out = np.empty((4, L, D), dtype=np.float32)
    for core in range(8):
        b, half = core // 2, core % 2
        out[b, half * NQ:(half + 1) * NQ] = np.asarray(res.results[core]["y"], dtype=np.float32)
    return out
```

```python
import contextlib
import math
import numpy as np
import concourse.bass as bass
import concourse.mybir as mybir
from concourse.bass_utils import run_bass_kernel_spmd

F32 = mybir.dt.float32
BF16 = mybir.dt.bfloat16
ALU = mybir.AluOpType
AF = mybir.ActivationFunctionType
AX = mybir.AxisListType

D = 1024
L = 8192
LC = 256
LK = L + LC
NQ = 4096
NE = 16384
RMS_EPS = 1e-6
LAM_INIT = 0.8 - 0.6 * math.exp(-0.3 * 0)
NEG = -1.0e30

ENGS = ("pe", "act", "dve", "pool", "sp")
DMA_POOL = {"sp": 28, "act": 8, "pool": 20}
SAME_ENGINE_SYNC = {"pe": False, "act": True, "dve": True, "pool": True, "sp": False}


class Buf:
    __slots__ = ("name", "last_write", "readers")

    def __init__(self, name):
        self.name = name
        self.last_write = None
        self.readers = []


class T:
    __slots__ = ("ap", "buf")

    def __init__(self, ap, buf):
        self.ap = ap
        self.buf = buf if isinstance(buf, Buf) else Buf(buf)

    def __getitem__(self, key):
        return self.ap[key]


def _bufs(xs):
    out = []
    for x in xs:
        if x is None:
            continue
        out.append(x.buf if isinstance(x, T) else x)
    return out


class Prog:
    def __init__(self, nc):
        self.nc = nc
        self.streams = {e: [] for e in ENGS}
        self.cnt = {e: 0 for e in ENGS}
        self.seen = {e: {} for e in ENGS}
        self.dma_i = {q: 0 for q in DMA_POOL}
        self.signal = {e: set() for e in ENGS}

    def _need(self, eng, deps):
        need = {}
        for d in deps:
            if d is None:
                continue
            key, val, src = d
            if src == eng and not SAME_ENGINE_SYNC[eng]:
                continue
            if self.seen[eng].get(key, 0) >= val:
                continue
            if need.get(key, 0) < val:
                need[key] = val
        for key, val in need.items():
            self.seen[eng][key] = val
            self.streams[eng].append(("wait", key, val))
            if key[0] == "eng":
                self.signal[key[1]].add(val)

    @staticmethod
    def _deps(reads, writes):
        deps = []
        for r in reads:
            deps.append(r.last_write)
        for w in writes:
            deps.append(w.last_write)
            deps.extend(w.readers)
        return deps

    @staticmethod
    def _commit(tok, reads, writes):
        for r in reads:
            r.readers.append(tok)
            if len(r.readers) > 64:
                best = {}
                for t in r.readers:
                    if best.get(t[0], (0,))[0] < t[1]:
                        best[t[0]] = (t[1], t[2])
                r.readers = [(k, v[0], v[1]) for k, v in best.items()]
        for w in writes:
            w.last_write = tok
            w.readers = []

    def op(self, eng, fn, reads=(), writes=()):
        reads = _bufs(reads)
        writes = _bufs(writes)
        self._need(eng, self._deps(reads, writes))
        self.cnt[eng] += 1
        tok = (("eng", eng), self.cnt[eng], eng)
        self.streams[eng].append(("op", fn, ("eng", eng), self.cnt[eng]))
        self._commit(tok, reads, writes)
        return tok

    def dma(self, q, out, in_, reads=(), writes=(), **kw):
        reads = _bufs(reads)
        writes = _bufs(writes)
        n = DMA_POOL[q]
        i = self.dma_i[q]
        self.dma_i[q] += 1
        slot, gen = i % n, i // n
        key = ("dma", q, slot)
        deps = self._deps(reads, writes)
        if gen > 0:
            deps.append((key, 16 * gen, None))
        self._need(q, deps)
        tok = (key, 16 * (gen + 1), None)
        self.streams[q].append(("dma", lambda e: e.dma_start(out=out, in_=in_, **kw), key, 16))
        self._commit(tok, reads, writes)
        return tok

    def barrier(self):
        toks = [(("eng", e), self.cnt[e], None) for e in ENGS if self.cnt[e] > 0]
        for q, n in DMA_POOL.items():
            i = self.dma_i[q]
            for slot in range(n):
                g = (i - slot + n - 1) // n if i > slot else 0
                if g > 0:
                    toks.append((("dma", q, slot), 16 * g, None))
        for e in ENGS:
            self._need(e, [t for t in toks if t[0] != ("eng", e)])

    def emit(self):
        nc = self.nc
        with contextlib.ExitStack() as es:
            sems = {}
            for e in ENGS:
                sems[("eng", e)] = es.enter_context(nc.semaphore("s_" + e))
            for q, n in DMA_POOL.items():
                for s in range(n):
                    sems[("dma", q, s)] = es.enter_context(nc.semaphore(f"d_{q}{s}"))
            block = es.enter_context(nc.Block())

            semval = {}
            for en in ENGS:
                c = 0
                m = {}
                for item in self.streams[en]:
                    if item[0] == "op" and item[3] in self.signal[en]:
                        c += 1
                        m[item[3]] = c
                semval[en] = m

            def run(engname):
                def body(e):
                    for item in self.streams[engname]:
                        if item[0] == "wait":
                            key, val = item[1], item[2]
                            if key[0] == "eng":
                                val = semval[key[1]][val]
                            e.wait_ge(sems[key], val)
                        elif item[0] == "dma":
                            _, fn, key, inc = item
                            fn(e).then_inc(sems[key], inc)
                        else:
                            _, fn, key, idx = item
                            ins = fn(e)
                            if idx in self.signal[engname]:
                                ins.then_inc(sems[key], 1)
                return body

            block.sync(run("sp"))
            block.scalar(run("act"))
            block.vector(run("dve"))
            block.gpsimd(run("pool"))
            block.tensor(run("pe"))


class Arena:
    def __init__(self, tensor, size, name):
        self.t = tensor
        self.size = size
        self.off = 0
        self.name = name
        self.n = 0

    def alloc(self, n, name=None):
        assert self.off + n <= self.size, f"arena {self.name} overflow: {self.off}+{n}>{self.size} ({name})"
        ap = self.t[:, self.off:self.off + n]
        self.off += n
        self.n += 1
        return T(ap, Buf(f"{self.name}_{name or self.n}"))

    def mark(self):
        return self.off

    def reset(self, m):
        self.off = m


def build_program(dbg=None, phases="ABCEF"):
    nc = bass.Bass("TRN2", target_bir_lowering=False)
    P = Prog(nc)

    def din(name, shape, dt=F32):
        return nc.dram_tensor(name, list(shape), dt, kind="ExternalInput").ap()

    def dscr(name, shape, dt):
        return T(nc.dram_tensor(name, list(shape), dt, kind="Internal").ap(), name)

    xb = din("xb", [L, D])
    xown = din("xown", [NQ, D])
    ctxb = din("ctxb", [LC, D])
    cvec = din("cvec", [2, D])
    w_ada = din("w_ada", [D, 6 * D])
    b_ada = din("b_ada", [1, 6 * D])
    g_pre_mix = din("g_pre_mix", [1, D])
    g_post_mix = din("g_post_mix", [1, D])
    g_pre_ffn = din("g_pre_ffn", [1, D])
    g_post_ffn = din("g_post_ffn", [1, D])
    w_in = din("w_in", [D, 7 * D])
    w_qk_sw = din("w_qk_sw", [D, 2 * D])
    b_gate = din("b_gate", [1, 2 * D])
    lam_vecs = din("lam_vecs", [1, 256])
    subln_g = din("subln_g", [1, 128])
    sgu_ln_g = din("sgu_ln_g", [1, D])
    sgu_ln_b = din("sgu_ln_b", [1, D])
    sgu_wT = din("sgu_wT", [8, 128, 128])
    sgu_b = din("sgu_b", [8, 128])
    w_branch_attn = din("w_branch_attn", [D, D])
    w_branch_sgu = din("w_branch_sgu", [D, D])
    w_out = din("w_out", [D, D])
    peer_wq = din("peer_wq", [D, 2 * D])
    peer_keysT = din("peer_keysT", [2, 128, 128])
    peer_u = din("peer_u", [NE, D])
    peer_v = din("peer_v", [NE, D])
    ropeC = din("ropeC", [128, L])
    ropeS = din("ropeS", [128, L])
    ropeCq = din("ropeCq", [128, NQ])
    ropeSq = din("ropeSq", [128, NQ])
    y_out = nc.dram_tensor("y", [NQ, D], F32, kind="ExternalOutput").ap()
    Y = T(y_out, "y_out")

    hT_all = dscr("hT_all", [D, LK], BF16)
    hT_own = dscr("hT_own", [D, NQ], BF16)
    KT = dscr("KT", [D, LK], BF16)
    Vs = dscr("Vs", [LK, D], BF16)
    QT = dscr("QT", [D, NQ], BF16)
    pST = dscr("pST", [D, NQ], BF16)
    gAT = dscr("gAT", [D, NQ], BF16)
    onT = dscr("onT", [D, NQ], BF16)
    XN = dscr("XN", [NQ, D], F32)
    H2T = dscr("H2T", [D, NQ], BF16)
    UT = dscr("UT", [128, 128, D], BF16)
    VB = dscr("VB", [NE, D], BF16)
    SS = dscr("SS", [NQ, 2 * D], F32)
    SM = dscr("SM", [NQ, 16], F32)

    dbg_outs = {}

    es = contextlib.ExitStack()
    with es:
        NF = 25 * 1024
        NB = 54784
        f_t = es.enter_context(nc.sbuf_tensor("arena_f", [128, NF], F32))
        b_t = es.enter_context(nc.sbuf_tensor("arena_b", [128, NB], BF16))
        AFa = Arena(f_t, NF, "f")
        ABa = Arena(b_t, NB, "b")
        pst = [es.enter_context(nc.psum_tensor(f"ps{i}", [128, 1024], F32)) for i in range(4)]
        PS = [T(pst[i // 2][:, (i % 2) * 512:(i % 2 + 1) * 512], f"psbank{i}") for i in range(8)]

        def ps2(i):
            return pst[i][:, :], [PS[2 * i].buf, PS[2 * i + 1].buf]

        def MM(out, lhsT, rhs, start, stop, reads, writes):
            P.op("pe", lambda e: e.matmul(out, lhsT=lhsT, rhs=rhs, start=start, stop=stop), reads, writes)

        def MMG(out, lhsT, rhs, start, stop, reads, writes):
            P.op("pe", lambda e: e.matmul(out, lhsT=lhsT, rhs=rhs, start=start, stop=stop, skip_group_check=True),
                 reads, writes)

        def TR(out, in_, reads, writes):
            MM(out, in_, ident_b.ap, True, True, list(reads) + [ident_b], writes)

        def ACT(out, in_, func, reads, writes, bias=None, scale=None, accum=None):
            kw = {}
            if bias is not None:
                kw["bias"] = bias
            if scale is not None:
                kw["scale"] = scale
            if accum is not None:
                kw["accum_out"] = accum
            P.op("act", lambda e: e.activation(out=out, in_=in_, func=func, **kw), reads, writes)

        def TT(eng, out, in0, in1, op, reads, writes):
            P.op(eng, lambda e: e.tensor_tensor(out=out, in0=in0, in1=in1, op=op), reads, writes)

        def TS(eng, out, in0, s1, s2, op0, op1, reads, writes):
            if s2 is None:
                P.op(eng, lambda e: e.tensor_scalar(out=out, in0=in0, scalar1=s1, scalar2=None, op0=op0), reads, writes)
            else:
                P.op(eng, lambda e: e.tensor_scalar(out=out, in0=in0, scalar1=s1, scalar2=s2, op0=op0, op1=op1), reads, writes)

        def STT(out, in0, scalar, in1, op0, op1, reads, writes):
            P.op("dve", lambda e: e.scalar_tensor_tensor(out=out, in0=in0, scalar=scalar, in1=in1, op0=op0, op1=op1),
                 reads, writes)

        def COPY(eng, out, in_, reads, writes):
            if eng == "act":
                P.op("act", lambda e: e.copy(out=out, in_=in_), reads, writes)
            else:
                P.op(eng, lambda e: e.tensor_copy(out=out, in_=in_), reads, writes)

        def RED(out, in_, op, reads, writes):
            P.op("dve", lambda e: e.tensor_reduce(out=out, in_=in_, axis=AX.X, op=op), reads, writes)

        def MAX8(out, in_, reads, writes):
            P.op("dve", lambda e: e.max(out=out, in_=in_), reads, writes)

        def MREP(out, rep, vals, reads, writes):
            P.op("dve", lambda e: e.match_replace(out=out, in_to_replace=rep, in_values=vals, imm_value=NEG), reads, writes)

        def RECIP(out, in_, reads, writes):
            P.op("dve", lambda e: e.reciprocal(out=out, in_=in_), reads, writes)

        def SQRT(out, in_, reads, writes):
            P.op("act", lambda e: e.sqrt(out=out, in_=in_), reads, writes)

        def MEMSET(eng, out, val, writes):
            P.op(eng, lambda e: e.memset(out, val), (), writes)

        def rstd(t, ss_ap, out_ap, scale, eps):
            TS("dve", out_ap, ss_ap, scale, eps, ALU.mult, ALU.add, [t], [t])
            SQRT(out_ap, out_ap, [t], [t])
            RECIP(out_ap, out_ap, [t], [t])

        iot = AFa.alloc(128, "iot")
        ident_f = AFa.alloc(128, "ident_f")
        ones_f = AFa.alloc(128, "ones_f")
        ident_b = ABa.alloc(128, "ident_b")
        ones_b = ABa.alloc(128, "ones_b")
        P.op("pool", lambda e: e.iota(iot.ap, pattern=[[1, 128]], base=0, channel_multiplier=-1,
                                      allow_small_or_imprecise_dtypes=True), (), [iot.buf])
        P.op("dve", lambda e: e.tensor_single_scalar(out=ident_f.ap, in_=iot.ap, scalar=0.0, op=ALU.is_equal),
             [iot.buf], [ident_f.buf])
        COPY("dve", ident_b.ap, ident_f.ap, [ident_f], [ident_b])
        MEMSET("dve", ones_f.ap, 1.0, [ones_f])
        MEMSET("dve", ones_b.ap, 1.0, [ones_b])

        modT = AFa.alloc(64, "modT")
        a1T = AFa.alloc(8, "a1T")
        a1cT = AFa.alloc(8, "a1cT")
        a2T = AFa.alloc(8, "a2T")
        G1 = AFa.alloc(D, "G1")
        G2 = AFa.alloc(D, "G2")
        neg_lam = AFa.alloc(1, "neg_lam")
        sgT = AFa.alloc(1, "sgT")
        bgT = AFa.alloc(16, "bgT")
        sbT = AFa.alloc(8, "sbT")
        smallf = AFa.alloc(64, "smallf")

        def b1T(dc):
            return modT.ap[:, 0 * 8 + dc:0 * 8 + dc + 1]

        def b1cT(dc):
            return modT.ap[:, 6 * 8 + dc:6 * 8 + dc + 1]

        def b2T(dc):
            return modT.ap[:, 3 * 8 + dc:3 * 8 + dc + 1]

        fmark, bmark = AFa.mark(), ABa.mark()

        def end_phase():
            P.barrier()
            AFa.reset(fmark)
            ABa.reset(bmark)

        def load_w_bf16(dst, src_ap, ncols, q="pool"):
            d3 = dst.ap.rearrange("p (k n) -> p k n", n=ncols)
            s3_ = src_ap.rearrange("(k p) n -> p k n", p=128)
            for dc in range(8):
                for c0 in range(0, ncols, 1024):
                    P.dma(q, d3[:, dc, c0:c0 + 1024], s3_[:, dc, c0:c0 + 1024], writes=[dst])
            return d3

        if "A" in phases:
            cT = AFa.alloc(16, "cT")
            srep = [AFa.alloc(1024, f"srep{r}") for r in range(2)]
            badac = [AFa.alloc(512, f"bada{i}") for i in range(2)]
            mod_bc = AFa.alloc(6 * D, "mod_bc")
            modc_bc = AFa.alloc(2 * D, "modc_bc")
            wa = [AFa.alloc(8 * 512, f"wa{i}") for i in range(2)]
            gtmp = [AFa.alloc(D, f"gtmp{i}") for i in range(2)]
            gpreT = AFa.alloc(8, "gpreT")
            gpfT = AFa.alloc(8, "gpfT")
            lv = AFa.alloc(256, "lv")

            cT3 = cT.ap.rearrange("p (k r) -> p k r", r=2)
            for r in range(2):
                P.dma("sp", cT3[:, :, r], cvec[r, :].rearrange("(k p) -> p k", p=128), writes=[cT],
                      allow_slow_non_contiguous=True)
            ACT(cT.ap, cT.ap, AF.Silu, [cT], [cT])
            for r in range(2):
                COPY("dve", srep[r].ap.rearrange("p (k m) -> p k m", m=128),
                     cT3[:, :, r:r + 1].to_broadcast([128, 8, 128]), [cT], [srep[r]])
            w_ada_v = w_ada.rearrange("(k p) n -> p k n", p=128)
            for cb in range(12):
                w = wa[cb % 2]
                w3 = w.ap.rearrange("p (k n) -> p k n", n=512)
                bd = badac[cb % 2]
                P.dma("sp" if cb % 2 == 0 else "act", w3, w_ada_v[:, :, cb * 512:(cb + 1) * 512], writes=[w])
                P.dma("sp", bd.ap, b_ada[:, cb * 512:(cb + 1) * 512].partition_broadcast(128), writes=[bd])
                for r in range(2):
                    if r == 1 and cb >= 4:
                        continue
                    sr3 = srep[r].ap.rearrange("p (k m) -> p k m", m=128)
                    pb = PS[(cb % 2) * 2 + r]
                    for kc in range(8):
                        MM(pb.ap, sr3[:, kc, :], w3[:, kc, :], kc == 0, kc == 7, [srep[r], w], [pb])
                    dst = mod_bc if r == 0 else modc_bc
                    TT("dve", dst.ap[:, cb * 512:(cb + 1) * 512], pb.ap, bd.ap, ALU.add, [pb, bd], [dst])
            for grp in range(16):
                pb = PS[4 + grp % 4]
                for k4 in range(4):
                    idx = grp * 4 + k4
                    v, fc = idx // 8, idx % 8
                    if v < 6:
                        src, col = mod_bc, v * 1024 + fc * 128
                    else:
                        src, col = modc_bc, (v - 6) * 1024 + fc * 128
                    MM(pb.ap[:, k4 * 128:(k4 + 1) * 128], src.ap[:, col:col + 128], ident_f.ap, True, True,
                       [src, ident_f], [pb])
                COPY("dve", modT.ap[:, grp * 4:(grp + 1) * 4], pb.ap[:, 0:512:128], [pb], [modT])
            P.dma("sp", gpreT.ap, g_pre_mix[0, :].rearrange("(k p) -> p k", p=128), writes=[gpreT],
                  allow_slow_non_contiguous=True)
            P.dma("sp", gpfT.ap, g_pre_ffn[0, :].rearrange("(k p) -> p k", p=128), writes=[gpfT],
                  allow_slow_non_contiguous=True)
            for hh in range(2):
                P.dma("sp", bgT.ap[:, hh * 8:(hh + 1) * 8], b_gate[0, hh * D:(hh + 1) * D].rearrange("(k p) -> p k", p=128),
                      writes=[bgT], allow_slow_non_contiguous=True)
            P.dma("sp", sbT.ap, sgu_b.rearrange("g p -> p g"), writes=[sbT], allow_slow_non_contiguous=True)
            P.dma("sp", sgT.ap, subln_g[0, :].rearrange("(p o) -> p o", o=1), writes=[sgT],
                  allow_slow_non_contiguous=True)
            TS("dve", sgT.ap, sgT.ap, (1.0 - LAM_INIT), None, ALU.mult, None, [sgT], [sgT])
            for (dst, scv, gT) in ((a1T, 1, gpreT), (a1cT, 7, gpreT), (a2T, 4, gpfT)):
                TS("dve", dst.ap, modT.ap[:, scv * 8:(scv + 1) * 8], 1.0, None, ALU.add, None, [modT], [dst])
                TT("dve", dst.ap, dst.ap, gT.ap, ALU.mult, [dst, gT], [dst])
            P.dma("sp", gtmp[0].ap, g_post_mix.partition_broadcast(128), writes=[gtmp[0]])
            TT("dve", G1.ap, mod_bc.ap[:, 2 * D:3 * D], gtmp[0].ap, ALU.mult, [mod_bc, gtmp[0]], [G1])
            P.dma("sp", gtmp[1].ap, g_post_ffn.partition_broadcast(128), writes=[gtmp[1]])
            TT("dve", G2.ap, mod_bc.ap[:, 5 * D:6 * D], gtmp[1].ap, ALU.mult, [mod_bc, gtmp[1]], [G2])
            P.dma("sp", lv.ap, lam_vecs.partition_broadcast(128), writes=[lv])
            for i in range(2):
                TT("dve", lv.ap[:, i * 128:i * 128 + 64], lv.ap[:, i * 128:i * 128 + 64],
                   lv.ap[:, i * 128 + 64:i * 128 + 128], ALU.mult, [lv], [lv])
                RED(smallf.ap[:, i:i + 1], lv.ap[:, i * 128:i * 128 + 64], ALU.add, [lv], [smallf])
            ACT(smallf.ap[:, 0:2], smallf.ap[:, 0:2], AF.Exp, [smallf], [smallf])
            STT(neg_lam.ap, smallf.ap[:, 1:2], -LAM_INIT, smallf.ap[:, 0:1], ALU.add, ALU.subtract, [smallf], [neg_lam])
            if dbg:
                dbg_outs["modT"] = (modT, 64)
                dbg_outs["G1"] = (G1, D)
                dbg_outs["neg_lam"] = (neg_lam, 1)
            end_phase()

        def norm_transpose(src, ntok, aT, bTf, dst, blk_tiles):
            xt = [AFa.alloc(D, f"xt{i}") for i in range(4)]
            junk = ABa.alloc(D, "junk")
            xn = [ABa.alloc(D, f"xn{i}") for i in range(4)]
            hblk = [ABa.alloc(8 * 128 * blk_tiles, f"hblk{i}") for i in range(2)]
            ss = [AFa.alloc(2, f"ss{i}") for i in range(4)]
            ntiles = ntok // 128
            dst_v = dst.ap.rearrange("(k p) t -> p k t", p=128)
            for ti in range(ntiles):
                x_ = xt[ti % 4]
                s_ = ss[ti % 4]
                n_ = xn[ti % 4]
                bi = ti // blk_tiles
                hb = hblk[bi % 2]
                hb3 = hb.ap.rearrange("p (k t) -> p k t", k=8)
                tl = ti % blk_tiles
                P.dma("sp", x_.ap, src[ti * 128:(ti + 1) * 128, :], writes=[x_])
                ACT(junk.ap, x_.ap, AF.Square, [x_], [junk, s_], accum=s_.ap[:, 0:1])
                rstd(s_, s_.ap[:, 0:1], s_.ap[:, 1:2], 1.0 / D, RMS_EPS)
                TS("dve", n_.ap, x_.ap, s_.ap[:, 1:2], None, ALU.mult, None, [x_, s_], [n_])
                pa, pbufs = ps2(ti % 4)
                for dc in range(8):
                    TR(pa[:, dc * 128:(dc + 1) * 128], n_.ap[:, dc * 128:(dc + 1) * 128], [n_], [pbufs[dc // 4]])
                for dc in range(8):
                    o = hb3[:, dc, tl * 128:(tl + 1) * 128]
                    i_ = pa[:, dc * 128:(dc + 1) * 128]
                    if dc // 4 == 0:
                        ACT(o, i_, AF.Identity, [pbufs[dc // 4], aT, modT], [hb], bias=bTf(dc), scale=aT.ap[:, dc:dc + 1])
                    else:
                        TS("dve", o, i_, aT.ap[:, dc:dc + 1], bTf(dc), ALU.mult, ALU.add, [pbufs[dc // 4], aT, modT], [hb])
                if tl == blk_tiles - 1:
                    t0 = bi * blk_tiles * 128
                    P.dma("pool", dst_v[:, :, t0:t0 + blk_tiles * 128], hb3, reads=[hb], writes=[dst])
            end_phase()

        if "B" in phases:
            norm_transpose(ctxb, LC, a1cT, b1cT, T(hT_all.ap[:, 0:LC], hT_all.buf), 2)
            norm_transpose(xb, L, a1T, b1T, T(hT_all.ap[:, LC:LK], hT_all.buf), 4)
            norm_transpose(xown, NQ, a1T, b1T, hT_own, 4)

        if "B" in phases:
            Wq = ABa.alloc(8 * D, "Wq")
            Wqs = ABa.alloc(8 * D, "Wqs")
            Wk = ABa.alloc(8 * D, "Wk")
            Wks = ABa.alloc(8 * D, "Wks")
            Wv = ABa.alloc(8 * D, "Wv")
            Wk3 = load_w_bf16(Wk, w_in[:, D:2 * D], D)
            Wks3 = load_w_bf16(Wks, w_qk_sw[:, D:2 * D], D)
            Wv3 = load_w_bf16(Wv, w_in[:, 2 * D:3 * D], D)
            Wq3 = load_w_bf16(Wq, w_in[:, 0:D], D)
            Wqs3 = load_w_bf16(Wqs, w_qk_sw[:, 0:D], D)
            hblk = [ABa.alloc(8 * 512, f"hb{i}") for i in range(2)]
            Cb = [AFa.alloc(512, f"Cb{i}") for i in range(2)]
            Sb = [AFa.alloc(512, f"Sb{i}") for i in range(2)]
            t1 = [AFa.alloc(512, f"t1_{i}") for i in range(2)]
            t2 = [AFa.alloc(512, f"t2_{i}") for i in range(2)]
            ko = [ABa.alloc(512, f"ko{i}") for i in range(3)]
            vo = [ABa.alloc(D, f"vo{i}") for i in range(2)]

            def qk_block(bi, hsrc, t0, N, W3, Ws3, Wt, Wst, Ctab, Stab, c0, dst, dcol, rope):
                hb = hblk[bi % 2]
                hb3 = hb.ap.rearrange("p (k t) -> p k t", k=8)
                hsrc_v = hsrc.ap.rearrange("(k p) t -> p k t", p=128)
                P.dma("sp", hb3[:, :, 0:N], hsrc_v[:, :, t0:t0 + N], reads=[hsrc], writes=[hb])
                C_ = Cb[bi % 2]
                S_ = Sb[bi % 2]
                if rope:
                    P.dma("act", C_.ap[:, 0:N], Ctab[:, c0:c0 + N], writes=[C_])
                    P.dma("act", S_.ap[:, 0:N], Stab[:, c0:c0 + N], writes=[S_])
                for fc in range(8):
                    pA = PS[(fc % 2) * 2]
                    pB = PS[(fc % 2) * 2 + 1]
                    for dc in range(8):
                        MM(pA.ap[:, 0:N], W3[:, dc, fc * 128:(fc + 1) * 128], hb3[:, dc, 0:N], dc == 0, dc == 7, [Wt, hb], [pA])
                    k_ = ko[fc % 3]
                    if rope:
                        for dc in range(8):
                            MM(pB.ap[:, 0:N], Ws3[:, dc, fc * 128:(fc + 1) * 128], hb3[:, dc, 0:N], dc == 0, dc == 7,
                               [Wst, hb], [pB])
                        a_ = t1[fc % 2]
                        b_ = t2[fc % 2]
                        TT("dve", a_.ap[:, 0:N], pA.ap[:, 0:N], C_.ap[:, 0:N], ALU.mult, [pA, C_], [a_])
                        TT("dve", b_.ap[:, 0:N], pB.ap[:, 0:N], S_.ap[:, 0:N], ALU.mult, [pB, S_], [b_])
                        TT("pool", k_.ap[:, 0:N], a_.ap[:, 0:N], b_.ap[:, 0:N], ALU.add, [a_, b_], [k_])
                    else:
                        COPY("act", k_.ap[:, 0:N], pA.ap[:, 0:N], [pA], [k_])
                    P.dma("pool", dst.ap[fc * 128:(fc + 1) * 128, dcol:dcol + N], k_.ap[:, 0:N], reads=[k_], writes=[dst])
                return hb, hb3

            def v_block(hb, hb3, N, tok0):
                for tl in range(N // 128):
                    v_ = vo[tl % 2]
                    for half in range(2):
                        pV = PS[4 + (tl * 2 + half) % 4]
                        for dc in range(8):
                            MM(pV.ap, hb3[:, dc, tl * 128:(tl + 1) * 128], Wv3[:, dc, half * 512:(half + 1) * 512],
                               dc == 0, dc == 7, [Wv, hb], [pV])
                        COPY("act", v_.ap[:, half * 512:(half + 1) * 512], pV.ap, [pV], [v_])
                    P.dma("pool", Vs.ap[tok0 + tl * 128:tok0 + (tl + 1) * 128, :], v_.ap, reads=[v_], writes=[Vs])

            hb, hb3 = qk_block(0, hT_all, 0, LC, Wk3, Wks3, Wk, Wks, None, None, 0, KT, 0, False)
            v_block(hb, hb3, LC, 0)
            for bi in range(16):
                hb, hb3 = qk_block(bi + 1, hT_all, LC + bi * 512, 512, Wk3, Wks3, Wk, Wks, ropeC, ropeS, bi * 512,
                                   KT, LC + bi * 512, True)
                v_block(hb, hb3, 512, LC + bi * 512)
            for bi in range(8):
                qk_block(bi, hT_own, bi * 512, 512, Wq3, Wqs3, Wq, Wqs, ropeCq, ropeSq, bi * 512, QT, bi * 512, True)
            end_phase()

        if "B" in phases:
            Wz = ABa.alloc(8 * 2 * D, "Wz")
            Wgl = ABa.alloc(8 * 2 * D, "Wgl")
            Wbs = ABa.alloc(8 * D, "Wbs")
            wsT = ABa.alloc(8 * 128, "wsT")
            Wz3 = load_w_bf16(Wz, w_in[:, 3 * D:5 * D], 2 * D)
            Wgl3 = load_w_bf16(Wgl, w_in[:, 5 * D:7 * D], 2 * D)
            Wbs3 = load_w_bf16(Wbs, w_branch_sgu, D)
            wsT3 = wsT.ap.rearrange("q (g p) -> q g p", g=8)
            for g in range(8):
                P.dma("pool", wsT3[:, g, :], sgu_wT[g, :, :], writes=[wsT])
            lng = AFa.alloc(D, "lng")
            lnb = AFa.alloc(D, "lnb")
            P.dma("sp", lng.ap, sgu_ln_g.partition_broadcast(128), writes=[lng])
            P.dma("sp", lnb.ap, sgu_ln_b.partition_broadcast(128), writes=[lnb])
            hb = ABa.alloc(8 * 512, "hb")
            hb3 = hb.ap.rearrange("p (k t) -> p k t", k=8)
            sT_ = ABa.alloc(8 * 512, "sgT")
            sT3 = sT_.ap.rearrange("p (k t) -> p k t", k=8)
            zg = [AFa.alloc(2 * D, f"zg{i}") for i in range(2)]
            vn = AFa.alloc(D, "vn")
            vb_ = ABa.alloc(D, "vnb")
            sgb = [ABa.alloc(D, f"sgb{i}") for i in range(2)]
            junkf = AFa.alloc(D, "junkf")
            st = [AFa.alloc(8, f"st{i}") for i in range(2)]
            gs = [AFa.alloc(512, f"gs{i}") for i in range(2)]
            po = [ABa.alloc(512, f"po{i}") for i in range(2)]
            hsrc_v = hT_own.ap.rearrange("(k p) t -> p k t", p=128)
            npo = 0
            for bi in range(8):
                P.dma("sp", hb3, hsrc_v[:, :, bi * 512:(bi + 1) * 512], reads=[hT_own], writes=[hb])
                for tl in range(4):
                    z_ = zg[tl % 2]
                    s_ = st[tl % 2]
                    for cb in range(4):
                        pz = PS[cb]
                        for dc in range(8):
                            MM(pz.ap, hb3[:, dc, tl * 128:(tl + 1) * 128], Wz3[:, dc, cb * 512:(cb + 1) * 512],
                               dc == 0, dc == 7, [hb, Wz], [pz])
                        ACT(z_.ap[:, cb * 512:(cb + 1) * 512], pz.ap, AF.Gelu, [pz], [z_])
                    v_ap = z_.ap[:, D:2 * D]
                    RED(s_.ap[:, 0:1], v_ap, ALU.add, [z_], [s_])
                    ACT(junkf.ap, v_ap, AF.Square, [z_], [junkf, s_], accum=s_.ap[:, 1:2])
                    TS("dve", s_.ap[:, 2:3], s_.ap[:, 0:1], 1.0 / D, None, ALU.mult, None, [s_], [s_])
                    TT("dve", s_.ap[:, 3:4], s_.ap[:, 2:3], s_.ap[:, 2:3], ALU.mult, [s_], [s_])
                    STT(s_.ap[:, 4:5], s_.ap[:, 1:2], 1.0 / D, s_.ap[:, 3:4], ALU.mult, ALU.subtract, [s_], [s_])
                    rstd(s_, s_.ap[:, 4:5], s_.ap[:, 5:6], 1.0, 1e-5)
                    TS("dve", vn.ap, v_ap, s_.ap[:, 2:3], s_.ap[:, 5:6], ALU.subtract, ALU.mult, [z_, s_], [vn])
                    TT("pool", vn.ap, vn.ap, lng.ap, ALU.mult, [vn, lng], [vn])
                    TT("pool", vb_.ap, vn.ap, lnb.ap, ALU.add, [vn, lnb], [vb_])
                    pm, pmb = ps2(2)
                    for g in range(8):
                        MM(pm[:, g * 128:(g + 1) * 128], wsT3[:, g, :], vb_.ap[:, g * 128:(g + 1) * 128], True, True,
                           [wsT, vb_], [pmb[g // 4]])
                    sg_ = sgb[tl % 2]
                    for g in range(8):
                        STT(sg_.ap[:, g * 128:(g + 1) * 128], pm[:, g * 128:(g + 1) * 128], sbT.ap[:, g:g + 1],
                            z_.ap[:, g * 128:(g + 1) * 128], ALU.add, ALU.mult, [pmb[g // 4], sbT, z_], [sg_])
                    pt, ptb = ps2(3)
                    for fc in range(8):
                        TR(pt[:, fc * 128:(fc + 1) * 128], sg_.ap[:, fc * 128:(fc + 1) * 128], [sg_], [ptb[fc // 4]])
                    for hf in range(2):
                        COPY("act", sT3[:, hf * 4:(hf + 1) * 4, tl * 128:(tl + 1) * 128],
                             pt[:, hf * 512:(hf + 1) * 512].rearrange("p (k t) -> p k t", k=4), [ptb[hf]], [sT_])
                for fc in range(8):
                    pY = PS[0 + fc % 2]
                    pG = PS[2 + fc % 2]
                    pH = PS[4 + fc % 2]
                    for dc in range(8):
                        MM(pY.ap, Wbs3[:, dc, fc * 128:(fc + 1) * 128], sT3[:, dc, :], dc == 0, dc == 7, [Wbs, sT_], [pY])
                    for dc in range(8):
                        MM(pG.ap, Wgl3[:, dc, D + fc * 128:D + (fc + 1) * 128], hb3[:, dc, :], dc == 0, dc == 7, [Wgl, hb], [pG])
                    for dc in range(8):
                        MM(pH.ap, Wgl3[:, dc, fc * 128:(fc + 1) * 128], hb3[:, dc, :], dc == 0, dc == 7, [Wgl, hb], [pH])
                    g_ = gs[fc % 2]
                    ACT(g_.ap, pG.ap, AF.Sigmoid, [pG, bgT], [g_], bias=bgT.ap[:, 8 + fc:9 + fc])
                    o1 = po[npo % 2]
                    npo += 1
                    TT("dve", o1.ap, pY.ap, g_.ap, ALU.mult, [pY, g_], [o1])
                    P.dma("pool", pST.ap[fc * 128:(fc + 1) * 128, bi * 512:(bi + 1) * 512], o1.ap, reads=[o1], writes=[pST])
                    o2 = po[npo % 2]
                    npo += 1
                    ACT(o2.ap, pH.ap, AF.Sigmoid, [pH, bgT], [o2], bias=bgT.ap[:, fc:fc + 1])
                    P.dma("pool", gAT.ap[fc * 128:(fc + 1) * 128, bi * 512:(bi + 1) * 512], o2.ap, reads=[o2], writes=[gAT])
            end_phase()

        if "C" in phases:
            KTab2 = [ABa.alloc(LK, f"KTab{i}") for i in range(2)]
            QTa2 = [ABa.alloc(NQ, f"QTa{i}") for i in range(2)]
            QTb2 = [ABa.alloc(NQ, f"QTb{i}") for i in range(2)]
            Vh2 = [ABa.alloc(66 * 128, f"Vh{i}") for i in range(2)]
            PT = [ABa.alloc(512, f"PT{i}") for i in range(6)]
            ono = [ABa.alloc(512, f"ono{i}") for i in range(2)]
            r_ = [AFa.alloc(512, f"r{i}") for i in range(2)]
            tt = [AFa.alloc(512, f"tt{i}") for i in range(2)]
            zacc = [AFa.alloc(512, f"zacc{i}") for i in range(2)]
            zacp = [AFa.alloc(512, f"zacp{i}") for i in range(2)]
            o_ = AFa.alloc(512, "o_")
            sq = AFa.alloc(512, "sq")
            rs = AFa.alloc(512, "rs")
            Vs_v = Vs.ap.rearrange("(kt k) e -> k kt e", k=128)
            NKT = LK // 128
            Ops = (PS[3], PS[4])
            Zps = (PS[5], PS[6])
            for i2 in range(2):
                MEMSET("dve", QTa2[i2].ap[64:128, :], 0.0, [QTa2[i2]])
                MEMSET("dve", QTb2[i2].ap[0:64, :], 0.0, [QTb2[i2]])
            for h in range(8):
                KTab, QTa, QTb, Vh = KTab2[h % 2], QTa2[h % 2], QTb2[h % 2], Vh2[h % 2]
                Vh3 = Vh.ap.rearrange("p (k e) -> p k e", e=128)
                P.dma("sp", KTab.ap, KT.ap[h * 128:(h + 1) * 128, :], reads=[KT], writes=[KTab])
                P.dma("sp", QTa.ap[0:64, :], QT.ap[(2 * h) * 64:(2 * h + 1) * 64, :], reads=[QT], writes=[QTa])
                P.dma("sp", QTb.ap[64:128, :], QT.ap[(2 * h + 1) * 64:(2 * h + 2) * 64, :], reads=[QT], writes=[QTb])
                for kq in range(6):
                    P.dma("sp", Vh3[:, kq * 11:(kq + 1) * 11, :], Vs_v[:, kq * 11:(kq + 1) * 11, h * 128:(h + 1) * 128],
                          reads=[Vs], writes=[Vh])
                for qb in range(8):
                    steps = [(j, kt) for j in range(2) for kt in range(NKT)]
                    LOOK = 2

                    def av(n):
                        j, kt = steps[n]
                        p_ = PT[n % 6]
                        MM(Ops[j].ap, Vh3[:, kt, :], p_.ap, kt == 0, kt == NKT - 1, [Vh, p_], [Ops[j]])
                        if kt % 3 == 2:
                            MM(Zps[j].ap, ones_b.ap, p_.ap, kt == 2, False, [ones_b, p_], [Zps[j]])
                        if kt == NKT - 1:
                            MM(Zps[j].ap, ones_f.ap, zacc[j].ap, False, True, [ones_f, zacc[j]], [Zps[j]])

                    for n in range(len(steps) + LOOK):
                        if n < len(steps):
                            j, kt = steps[n]
                            Qt_ = QTa if j == 0 else QTb
                            sp_ = PS[n % 3]
                            p_ = PT[n % 6]
                            MM(sp_.ap, KTab.ap[:, kt * 128:(kt + 1) * 128], Qt_.ap[:, qb * 512:(qb + 1) * 512], True, True,
                               [KTab, Qt_], [sp_])
                            ACT(p_.ap, sp_.ap, AF.Exp, [sp_], [p_], scale=0.125)
                            if kt % 3 == 2:
                                pass
                            elif kt == 0:
                                COPY("dve", zacc[j].ap, p_.ap, [p_], [zacc[j]])
                            else:
                                TT("dve", zacc[j].ap, zacc[j].ap, p_.ap, ALU.add, [zacc[j], p_], [zacc[j]])
                        if n >= LOOK:
                            av(n - LOOK)
                    for j in range(2):
                        RECIP(r_[j].ap, Zps[j].ap, [Zps[j]], [r_[j]])
                        TT("dve", tt[j].ap, Ops[j].ap, r_[j].ap, ALU.mult, [Ops[j], r_[j]], [tt[j]])
                    STT(o_.ap, tt[1].ap, neg_lam.ap[:, 0:1], tt[0].ap, ALU.mult, ALU.add, [tt[0], tt[1], neg_lam], [o_])
                    TT("pool", sq.ap, o_.ap, o_.ap, ALU.mult, [o_], [sq])
                    MM(PS[7].ap, ones_f.ap, sq.ap, True, True, [ones_f, sq], [PS[7]])
                    TS("dve", rs.ap, PS[7].ap, 1.0 / 128, RMS_EPS, ALU.mult, ALU.add, [PS[7]], [rs])
                    SQRT(rs.ap, rs.ap, [rs], [rs])
                    RECIP(rs.ap, rs.ap, [rs], [rs])
                    oo = ono[qb % 2]
                    STT(oo.ap, o_.ap, sgT.ap[:, 0:1], rs.ap, ALU.mult, ALU.mult, [o_, sgT, rs], [oo])
                    P.dma("pool", onT.ap[h * 128:(h + 1) * 128, qb * 512:(qb + 1) * 512], oo.ap, reads=[oo], writes=[onT])
            end_phase()

        if "E" in phases:
            Wba = ABa.alloc(8 * D, "Wba")
            Wo = ABa.alloc(8 * D, "Wo")
            Wba3 = load_w_bf16(Wba, w_branch_attn, D)
            Wo3 = load_w_bf16(Wo, w_out, D)
            onb = [ABa.alloc(8 * 512, f"onb{i}") for i in range(2)]
            gab = [ABa.alloc(8 * 512, f"gab{i}") for i in range(2)]
            psb = [ABa.alloc(8 * 512, f"psb{i}") for i in range(2)]
            mT = ABa.alloc(8 * 512, "mT")
            mT3 = mT.ap.rearrange("p (k t) -> p k t", k=8)
            h2_ = ABa.alloc(8 * 512, "h2b")
            h23 = h2_.ap.rearrange("p (k t) -> p k t", k=8)
            xhb = [ABa.alloc(D, f"xhb{i}") for i in range(2)]
            junk = AFa.alloc(D, "junk")
            mtmp = [AFa.alloc(512, f"mtmp{i}") for i in range(2)]
            xt = [AFa.alloc(D, f"xt{i}") for i in range(2)]
            tq = [AFa.alloc(D, f"tq{i}") for i in range(2)]
            xnw = [AFa.alloc(D, f"xnw{i}") for i in range(2)]
            st = [AFa.alloc(8, f"st{i}") for i in range(2)]
            v_on = onT.ap.rearrange("(k p) t -> p k t", p=128)
            v_ga = gAT.ap.rearrange("(k p) t -> p k t", p=128)
            v_ps = pST.ap.rearrange("(k p) t -> p k t", p=128)
            v_h2 = H2T.ap.rearrange("(k p) t -> p k t", p=128)
            for bi in range(8):
                cs = slice(bi * 512, (bi + 1) * 512)
                on_ = onb[bi % 2]
                ga_ = gab[bi % 2]
                ps_ = psb[bi % 2]
                on3 = on_.ap.rearrange("p (k t) -> p k t", k=8)
                ga3 = ga_.ap.rearrange("p (k t) -> p k t", k=8)
                pS3 = ps_.ap.rearrange("p (k t) -> p k t", k=8)
                P.dma("sp", on3, v_on[:, :, cs], reads=[onT], writes=[on_])
                P.dma("act", ga3, v_ga[:, :, cs], reads=[gAT], writes=[ga_])
                P.dma("sp", pS3, v_ps[:, :, cs], reads=[pST], writes=[ps_])
                for fc in range(8):
                    pY = PS[fc % 2]
                    for hc in range(8):
                        MM(pY.ap, Wba3[:, hc, fc * 128:(fc + 1) * 128], on3[:, hc, :], hc == 0, hc == 7, [Wba, on_], [pY])
                    m_ = mtmp[fc % 2]
                    TT("dve", m_.ap, pY.ap, ga3[:, fc, :], ALU.mult, [pY, ga_], [m_])
                    TT("pool", mT3[:, fc, :], m_.ap, pS3[:, fc, :], ALU.add, [m_, ps_], [mT])
                for tl in range(4):
                    ti = bi * 4 + tl
                    x_ = xt[tl % 2]
                    s_ = st[tl % 2]
                    P.dma("sp", x_.ap, xown[ti * 128:(ti + 1) * 128, :], writes=[x_])
                    py, pyb = ps2(1 + tl % 2)
                    for half in range(2):
                        hs = slice(half * 512, (half + 1) * 512)
                        for mc in range(8):
                            MM(py[:, hs], mT3[:, mc, tl * 128:(tl + 1) * 128], Wo3[:, mc, hs], mc == 0, mc == 7,
                               [mT, Wo], [pyb[half]])
                        ACT(junk.ap[:, hs], py[:, hs], AF.Square, [pyb[half]], [junk, s_], accum=s_.ap[:, 4 + half:5 + half])
                    TT("dve", s_.ap[:, 0:1], s_.ap[:, 4:5], s_.ap[:, 5:6], ALU.add, [s_], [s_])
                    rstd(s_, s_.ap[:, 0:1], s_.ap[:, 1:2], 1.0 / D, RMS_EPS)
                    q_ = tq[tl % 2]
                    for half in range(2):
                        hs = slice(half * 512, (half + 1) * 512)
                        STT(q_.ap[:, hs], py[:, hs], s_.ap[:, 1:2], G1.ap[:, hs], ALU.mult, ALU.mult, [pyb[half], s_, G1], [q_])
                    xn_ = xnw[tl % 2]
                    TT("pool", xn_.ap, q_.ap, x_.ap, ALU.add, [q_, x_], [xn_])
                    P.dma("pool", XN.ap[ti * 128:(ti + 1) * 128, :], xn_.ap, reads=[xn_], writes=[XN])
                    ACT(junk.ap, xn_.ap, AF.Square, [xn_], [junk, s_], accum=s_.ap[:, 2:3])
                    rstd(s_, s_.ap[:, 2:3], s_.ap[:, 3:4], 1.0 / D, RMS_EPS)
                    xh_ = xhb[tl % 2]
                    TS("dve", xh_.ap, xn_.ap, s_.ap[:, 3:4], None, ALU.mult, None, [xn_, s_], [xh_])
                    pt, ptb = ps2(3)
                    for dc in range(8):
                        TR(pt[:, dc * 128:(dc + 1) * 128], xh_.ap[:, dc * 128:(dc + 1) * 128], [xh_], [ptb[dc // 4]])
                    for dc in range(8):
                        o = h23[:, dc, tl * 128:(tl + 1) * 128]
                        i_ = pt[:, dc * 128:(dc + 1) * 128]
                        if dc // 4 == 0:
                            ACT(o, i_, AF.Identity, [ptb[dc // 4], a2T, modT], [h2_], bias=b2T(dc), scale=a2T.ap[:, dc:dc + 1])
                        else:
                            TS("dve", o, i_, a2T.ap[:, dc:dc + 1], b2T(dc), ALU.mult, ALU.add, [ptb[dc // 4], a2T, modT], [h2_])
                P.dma("pool", v_h2[:, :, cs], h23, reads=[h2_], writes=[H2T])
            end_phase()

        if "F" in phases:
            ub = [ABa.alloc(D, f"ub{i}") for i in range(3)]
            vb = [ABa.alloc(D, f"vb{i}") for i in range(3)]
            utb = [ABa.alloc(D, f"utb{i}") for i in range(3)]
            for i in range(128):
                u_ = ub[i % 3]
                v_ = vb[i % 3]
                t_ = utb[i % 3]
                P.dma("pool", u_.ap, peer_u[i * 128:(i + 1) * 128, :], writes=[u_])
                P.dma("pool", v_.ap, peer_v[i * 128:(i + 1) * 128, :], writes=[v_])
                P.dma("sp", VB.ap[i * 128:(i + 1) * 128, :], v_.ap, reads=[v_], writes=[VB])
                pt, ptb = ps2(i % 4)
                for dc in range(8):
                    TR(pt[:, dc * 128:(dc + 1) * 128], u_.ap[:, dc * 128:(dc + 1) * 128], [u_], [ptb[dc // 4]])
                for hf in range(2):
                    COPY("act" if hf == 0 else "dve", t_.ap[:, hf * 512:(hf + 1) * 512], pt[:, hf * 512:(hf + 1) * 512],
                         [ptb[hf]], [t_])
                P.dma("sp", UT.ap[i, :, :], t_.ap, reads=[t_], writes=[UT])
            end_phase()

        if "F" in phases:
            Wpq = ABa.alloc(8 * 2 * D, "Wpq")
            Wpq3 = load_w_bf16(Wpq, peer_wq, 2 * D)
            kT = ABa.alloc(256, "kT")
            kT3 = kT.ap.rearrange("d (p n) -> d p n", p=2)
            for p_i in range(2):
                P.dma("pool", kT3[:, p_i, :], peer_keysT[p_i, :, :], writes=[kT])
            h2t = [ABa.alloc(8 * 128, f"h2t{i}") for i in range(2)]
            qn = ABa.alloc(2 * D, "qn")
            qnT = ABa.alloc(2 * D, "qnT")
            qnT3 = qnT.ap.rearrange("d (g t) -> d g t", g=16)
            sqf = AFa.alloc(2 * D, "sqf")
            ssb = [AFa.alloc(2 * D, f"s_sb{i}") for i in range(2)]
            tv = AFa.alloc(256, "tv")
            tv3 = tv.ap.rearrange("p (g a) -> p g a", g=16)
            tv4 = tv.ap.rearrange("p (h two a) -> p h two a", h=8, two=2)
            workg = [AFa.alloc(128, f"work{g}") for g in range(16)]
            tvg = [Buf(f"tvg{g}") for g in range(16)]
            c24g = [Buf(f"c24g{h}") for h in range(8)]
            cw0 = [AFa.alloc(256, f"cw0_{h}") for h in range(8)]
            cw1 = [AFa.alloc(256, f"cw1_{h}") for h in range(8)]
            cand = AFa.alloc(2 * D, "cand")
            cand3 = cand.ap.rearrange("p (h c) -> p h c", h=8)
            cw = [AFa.alloc(256, f"cw{i}") for i in range(2)]
            c24 = AFa.alloc(8 * 24, "c24")
            c243 = c24.ap.rearrange("p (h c) -> p h c", h=8)
            rg = AFa.alloc(16, "rg")
            smb = [AFa.alloc(16, f"smb{i}") for i in range(2)]
            zz = AFa.alloc(8, "zz")
            e16 = AFa.alloc(128, "e16")
            v_h2 = H2T.ap.rearrange("(k p) t -> p k t", p=128)
            for ti in range(NQ // 128):
                h2_ = h2t[ti % 2]
                h23 = h2_.ap.rearrange("p (k t) -> p k t", k=8)
                s_sb = ssb[ti % 2]
                s3 = s_sb.ap.rearrange("p (g n) -> p g n", g=16)
                sm = smb[ti % 2]
                P.dma("sp", h23, v_h2[:, :, ti * 128:(ti + 1) * 128], reads=[H2T], writes=[h2_])
                for cb in range(4):
                    for dc in range(8):
                        MM(PS[cb].ap, h23[:, dc, :], Wpq3[:, dc, cb * 512:(cb + 1) * 512], dc == 0, dc == 7, [h2_, Wpq], [PS[cb]])
                    ACT(sqf.ap[:, cb * 512:(cb + 1) * 512], PS[cb].ap, AF.Square, [PS[cb]], [sqf])
                RED(rg.ap, sqf.ap.rearrange("p (g n) -> p g n", g=16), ALU.add, [sqf], [rg])
                rstd(rg, rg.ap, rg.ap, 1.0 / 128, RMS_EPS)
                for cb in range(4):
                    TT("dve", qn.ap[:, cb * 512:(cb + 1) * 512].rearrange("p (g n) -> p g n", g=4),
                       PS[cb].ap.rearrange("p (g n) -> p g n", g=4),
                       rg.ap[:, cb * 4:(cb + 1) * 4].unsqueeze(2).to_broadcast([128, 4, 128]), ALU.mult, [PS[cb], rg], [qn])
                for g in range(16):
                    pb = PS[4 + g // 4]
                    TR(pb.ap[:, (g % 4) * 128:(g % 4 + 1) * 128], qn.ap[:, g * 128:(g + 1) * 128], [qn], [pb])
                for c4 in range(4):
                    COPY("act" if c4 % 2 == 0 else "dve", qnT3[:, c4 * 4:(c4 + 1) * 4, :],
                         PS[4 + c4].ap.rearrange("p (g t) -> p g t", g=4), [PS[4 + c4]], [qnT])
                for g in range(16):
                    pb = PS[g // 4]
                    MM(pb.ap[:, (g % 4) * 128:(g % 4 + 1) * 128], qnT3[:, g, :], kT3[:, g % 2, :], True, True, [qnT, kT], [pb])
                for c4 in range(4):
                    COPY("act", s_sb.ap[:, c4 * 512:(c4 + 1) * 512], PS[c4].ap, [PS[c4]], [s_sb])
                P.dma("pool", SS.ap[ti * 128:(ti + 1) * 128, :], s_sb.ap, reads=[s_sb], writes=[SS])
                for g in range(16):
                    MAX8(tv3[:, g, 0:8], s3[:, g, :], [s_sb], [tvg[g]])
                for g in range(16):
                    MREP(workg[g].ap, tv3[:, g, 0:8], s3[:, g, :], [s_sb, tvg[g]], [workg[g]])
                for g in range(16):
                    MAX8(tv3[:, g, 8:16], workg[g].ap, [workg[g]], [tvg[g]])
                TT("dve", cand.ap.rearrange("p (h a b) -> p h a b", h=8, a=16),
                   tv4[:, :, 0, :].unsqueeze(3).to_broadcast([128, 8, 16, 16]),
                   tv4[:, :, 1, :].unsqueeze(2).to_broadcast([128, 8, 16, 16]), ALU.add, tvg, [cand])
                for h in range(8):
                    MAX8(c243[:, h, 0:8], cand3[:, h, :], [cand], [c24g[h]])
                for h in range(8):
                    MREP(cw0[h].ap, c243[:, h, 0:8], cand3[:, h, :], [cand, c24g[h]], [cw0[h]])
                for h in range(8):
                    MAX8(c243[:, h, 8:16], cw0[h].ap, [cw0[h]], [c24g[h]])
                for h in range(8):
                    MREP(cw1[h].ap, c243[:, h, 8:16], cw0[h].ap, [cw0[h], c24g[h]], [cw1[h]])
                for h in range(8):
                    MAX8(c243[:, h, 16:24], cw1[h].ap, [cw1[h]], [c24g[h]])
                TT("dve", sm.ap[:, 0:8], c243[:, :, 15], c243[:, :, 16], ALU.add, c24g, [sm])
                TS("dve", sm.ap[:, 0:8], sm.ap[:, 0:8], 0.5, None, ALU.mult, None, [sm], [sm])
                TT("dve", e16.ap.rearrange("p (h k) -> p h k", h=8), c243[:, :, 0:16],
                   c243[:, :, 0:1].to_broadcast([128, 8, 16]), ALU.subtract, c24g, [e16])
                ACT(e16.ap, e16.ap, AF.Exp, [e16], [e16])
                RED(zz.ap, e16.ap.rearrange("p (h k) -> p h k", h=8), ALU.add, [e16], [zz])
                ACT(zz.ap, zz.ap, AF.Ln, [zz], [zz])
                TT("dve", sm.ap[:, 8:16], zz.ap, c243[:, :, 0], ALU.add, [zz] + c24g, [sm])
                TS("dve", sm.ap[:, 8:16], sm.ap[:, 8:16], -1.0, None, ALU.mult, None, [sm], [sm])
                P.dma("pool", SM.ap[ti * 128:(ti + 1) * 128, :], sm.ap, reads=[sm], writes=[SM])
            end_phase()

        if "F" in phases:
            GTall = ABa.alloc(128 * 256, "GT")
            GT3 = GTall.ap.rearrange("j (i t) -> j i t", t=256)
            GTg = [Buf(f"GTg{g}") for g in range(16)]
            h2blk = [ABa.alloc(8 * 256, f"h2blk{i}") for i in range(2)]
            Mb = [ABa.alloc(1024, f"Mb{i}") for i in range(3)]
            utc = [ABa.alloc(D, f"utc{i}") for i in range(4)]
            vbc = [ABa.alloc(D, f"vbc{i}") for i in range(4)]
            agb = [ABa.alloc(256, f"agb{i}") for i in range(3)]
            ssb = [AFa.alloc(2 * D, f"s_sb{i}") for i in range(2)]
            smb = [AFa.alloc(16, f"smb{i}") for i in range(2)]
            Sf = [AFa.alloc(1024, f"Sf{i}") for i in range(3)]
            Wf = [AFa.alloc(1024, f"Wf{i}") for i in range(2)]
            af = [AFa.alloc(256, f"af{i}") for i in range(2)]
            xnl = [AFa.alloc(D, f"xnl{i}") for i in range(2)]
            yq = [AFa.alloc(D, f"yq{i}") for i in range(2)]
            yo = [AFa.alloc(D, f"yo{i}") for i in range(2)]
            st = [AFa.alloc(8, f"st{i}") for i in range(2)]
            junk = AFa.alloc(D, "junk")
            v_h2 = H2T.ap.rearrange("(k p) t -> p k t", p=128)
            cnt = {"S": 0, "u": 0}
            NST = NQ // 256

            def load_scores(sti):
                for tl in range(2):
                    ti = sti * 2 + tl
                    P.dma("sp", ssb[tl].ap, SS.ap[ti * 128:(ti + 1) * 128, :], reads=[SS], writes=[ssb[tl]])
                    P.dma("sp", smb[tl].ap, SM.ap[ti * 128:(ti + 1) * 128, :], reads=[SM], writes=[smb[tl]])

            def gbuild(tl, ib):
                tcs = slice(tl * 128, (tl + 1) * 128)
                s_sb = ssb[tl]
                sm = smb[tl]
                s3 = s_sb.ap.rearrange("p (g n) -> p g n", g=16)
                gp, gpb = ps2(2)
                for h in range(8):
                    n = cnt["S"]
                    cnt["S"] += 1
                    S_ = Sf[n % 3]
                    W_ = Wf[n % 2]
                    M_ = Mb[n % 3]
                    TT("dve" if h % 4 == 3 else "pool", S_.ap.rearrange("p (i j) -> p i j", i=8),
                       s3[:, 2 * h, ib * 8:(ib + 1) * 8].unsqueeze(2).to_broadcast([128, 8, 128]),
                       s3[:, 2 * h + 1:2 * h + 2, :].to_broadcast([128, 8, 128]), ALU.add, [s_sb], [S_])
                    ACT(W_.ap, S_.ap, AF.Exp, [S_, sm], [W_], bias=sm.ap[:, 8 + h:9 + h])
                    STT(M_.ap, S_.ap, sm.ap[:, h:h + 1], W_.ap, ALU.is_ge, ALU.mult, [S_, W_, sm], [M_])
                    for ii in range(8):
                        MMG(gp[:, ii * 128:(ii + 1) * 128], M_.ap[:, ii * 128:(ii + 1) * 128], ident_b.ap,
                            h == 0 and ii % 4 == 0, h == 7, [M_, ident_b], [gpb[ii // 4]])
                for hf in range(2):
                    COPY("act" if hf == 0 else "dve", GT3[:, ib * 8 + hf * 4:ib * 8 + hf * 4 + 4, tcs],
                         gp[:, hf * 512:(hf + 1) * 512].rearrange("p (i t) -> p i t", i=4), [gpb[hf]], [GTg[ib]])

            def dense(sti, i):
                hb_ = h2blk[sti % 2]
                h23 = hb_.ap.rearrange("p (k t) -> p k t", k=8)
                n = cnt["u"]
                cnt["u"] += 1
                u_ = utc[n % 4]
                v_ = vbc[n % 4]
                u3 = u_.ap.rearrange("p (k j) -> p k j", k=8)
                P.dma("sp", u_.ap, UT.ap[i, :, :], reads=[UT], writes=[u_])
                P.dma("act" if i % 2 == 0 else "sp", v_.ap, VB.ap[i * 128:(i + 1) * 128, :], reads=[VB], writes=[v_])
                pa = PS[6 + n % 2]
                for dc in range(8):
                    MM(pa.ap[:, 0:256], u3[:, dc, :], h23[:, dc, :], dc == 0, dc == 7, [u_, hb_], [pa])
                a_ = af[n % 2]
                ACT(a_.ap, pa.ap[:, 0:256], AF.Gelu, [pa], [a_])
                g_ = agb[n % 3]
                TT("dve", g_.ap, a_.ap, GT3[:, i, :], ALU.mult, [a_, GTg[i // 8]], [g_])
                return (g_, v_, i)

            def dense_out(st_):
                g_, v_, i = st_
                for tl in range(2):
                    for half in range(2):
                        po_ = PS[tl * 2 + half]
                        MM(po_.ap, g_.ap[:, tl * 128:(tl + 1) * 128], v_.ap[:, half * 512:(half + 1) * 512], i == 0, i == 127,
                           [g_, v_], [po_])

            def epilogue(sti):
                for tl in range(2):
                    ti = sti * 2 + tl
                    py, pyb = ps2(tl)
                    s_ = st[tl]
                    x_ = xnl[tl]
                    P.dma("sp", x_.ap, XN.ap[ti * 128:(ti + 1) * 128, :], reads=[XN], writes=[x_])
                    for half in range(2):
                        hs = slice(half * 512, (half + 1) * 512)
                        ACT(junk.ap[:, hs], py[:, hs], AF.Square, [pyb[half]], [junk, s_], accum=s_.ap[:, 4 + half:5 + half])
                    TT("dve", s_.ap[:, 0:1], s_.ap[:, 4:5], s_.ap[:, 5:6], ALU.add, [s_], [s_])
                    rstd(s_, s_.ap[:, 0:1], s_.ap[:, 1:2], 1.0 / D, RMS_EPS)
                    q_ = yq[tl]
                    for half in range(2):
                        hs = slice(half * 512, (half + 1) * 512)
                        STT(q_.ap[:, hs], py[:, hs], s_.ap[:, 1:2], G2.ap[:, hs], ALU.mult, ALU.mult, [pyb[half], s_, G2], [q_])
                    o_ = yo[tl]
                    TT("pool", o_.ap, q_.ap, x_.ap, ALU.add, [q_, x_], [o_])
                    P.dma("pool", Y.ap[ti * 128:(ti + 1) * 128, :], o_.ap, reads=[o_], writes=[Y])

            def load_h2(sti):
                hb_ = h2blk[sti % 2]
                P.dma("sp", hb_.ap.rearrange("p (k t) -> p k t", k=8), v_h2[:, :, sti * 256:(sti + 1) * 256],
                      reads=[H2T], writes=[hb_])

            load_h2(0)
            load_scores(0)
            for ib in range(16):
                for tl in range(2):
                    gbuild(tl, ib)
            for sti in range(NST):
                if sti + 1 < NST:
                    load_h2(sti + 1)
                    load_scores(sti + 1)
                prev = None
                for ib in range(16):
                    for i in range(ib * 8, (ib + 1) * 8):
                        cur = dense(sti, i)
                        if prev is not None:
                            dense_out(prev)
                        prev = cur
                    if sti + 1 < NST:
                        for tl in range(2):
                            gbuild(tl, ib)
                dense_out(prev)
                epilogue(sti)
            AFa.reset(fmark)
            ABa.reset(bmark)

        if dbg:
            for name, (t, n) in list(dbg_outs.items()):
                o = nc.dram_tensor("dbg_" + name, [128, n], F32, kind="ExternalOutput").ap()
                P.dma("sp", o, t.ap[:, 0:n], reads=[t], writes=[Buf("dbgo_" + name)])
            for name, t in (("hT_all", hT_all), ("hT_own", hT_own), ("KT", KT), ("Vs", Vs), ("QT", QT), ("pST", pST),
                            ("gAT", gAT), ("onT", onT), ("XN", XN), ("H2T", H2T), ("SS", SS), ("SM", SM)):
                if name not in dbg:
                    continue
                shp = list(t.ap.shape)
                o = nc.dram_tensor("dbg_" + name, shp, t.ap.dtype, kind="ExternalOutput").ap()
                P.dma("sp", o, t.ap, reads=[t], writes=[Buf("dbgo_" + name)])

        P.barrier()
        P.emit()
    return nc, P


def _rope_tables():
    f = np.arange(64)
    blk = f // 32
    within = f % 32
    i = within % 16
    first = within < 16
    inv = (10000.0 ** (-(np.arange(16, dtype=np.float32)) / 16.0)).astype(np.float32)
    t = np.arange(L)
    rows = (t // 64).astype(np.float32)
    cols = (t % 64).astype(np.float32)
    pos = np.where(blk[:, None] == 0, rows[None, :], cols[None, :]).astype(np.float32)
    ang = (pos * inv[i][:, None]).astype(np.float32)
    C = np.cos(ang).astype(np.float32)
    S = (np.sin(ang) * np.where(first, -1.0, 1.0)[:, None]).astype(np.float32)
    partner = np.where(first, f + 16, f - 16)
    C128 = np.concatenate([C, C], axis=0)
    S128 = np.concatenate([S, S], axis=0)
    return np.ascontiguousarray(C128), np.ascontiguousarray(S128), partner


_CACHE = {}


def _get_program():
    if "nc" not in _CACHE:
        _CACHE["nc"] = build_program()[0]
    return _CACHE["nc"]


def make_in_maps(inputs):
    f32 = lambda a: np.ascontiguousarray(np.asarray(a, dtype=np.float32))
    x = f32(inputs["x"])
    c = f32(inputs["c"])
    ctx = f32(inputs["ctx"])
    c_ctx = f32(inputs["c_ctx"])
    C128, S128, partner = _rope_tables()
    w_in = f32(inputs["w_in"][0])
    perm = np.concatenate([(np.arange(16)[:, None] * 64 + partner[None, :]).reshape(-1),
                           1024 + (np.arange(16)[:, None] * 64 + partner[None, :]).reshape(-1)])
    w_qk_sw = np.ascontiguousarray(w_in[:, perm])
    lam_vecs = np.concatenate([f32(inputs["lambda_q1"][0]), f32(inputs["lambda_k1"][0]),
                               f32(inputs["lambda_q2"][0]), f32(inputs["lambda_k2"][0])]).reshape(1, 256)
    shared = {
        "w_ada": f32(inputs["w_ada"][0]), "b_ada": f32(inputs["b_ada"][0]).reshape(1, -1),
        "g_pre_mix": f32(inputs["g_pre_mix"][0]).reshape(1, -1), "g_post_mix": f32(inputs["g_post_mix"][0]).reshape(1, -1),
        "g_pre_ffn": f32(inputs["g_pre_ffn"][0]).reshape(1, -1), "g_post_ffn": f32(inputs["g_post_ffn"][0]).reshape(1, -1),
        "w_in": w_in, "w_qk_sw": w_qk_sw, "b_gate": f32(inputs["b_gate"][0]).reshape(1, -1),
        "lam_vecs": np.ascontiguousarray(lam_vecs), "subln_g": f32(inputs["subln_g"][0]).reshape(1, -1),
        "sgu_ln_g": f32(inputs["sgu_ln_g"][0]).reshape(1, -1), "sgu_ln_b": f32(inputs["sgu_ln_b"][0]).reshape(1, -1),
        "sgu_wT": np.ascontiguousarray(np.transpose(f32(inputs["sgu_w"][0]), (0, 2, 1))),
        "sgu_b": f32(inputs["sgu_b"][0]),
        "w_branch_attn": f32(inputs["w_branch_attn"][0]), "w_branch_sgu": f32(inputs["w_branch_sgu"][0]),
        "w_out": f32(inputs["w_out"][0]), "peer_wq": f32(inputs["peer_w_query"][0]),
        "peer_keysT": np.ascontiguousarray(np.transpose(f32(inputs["peer_sub_keys"][0]), (0, 2, 1))),
        "peer_u": f32(inputs["peer_u"][0]), "peer_v": f32(inputs["peer_v"][0]),
        "ropeC": C128, "ropeS": S128,
    }
    in_maps = []
    for core in range(8):
        b, half = core // 2, core % 2
        m = dict(shared)
        m["xb"] = x[b]
        m["xown"] = np.ascontiguousarray(x[b, half * NQ:(half + 1) * NQ])
        m["ctxb"] = ctx[b]
        m["cvec"] = np.ascontiguousarray(np.stack([c[b], c_ctx]))
        m["ropeCq"] = np.ascontiguousarray(C128[:, half * NQ:(half + 1) * NQ])
        m["ropeSq"] = np.ascontiguousarray(S128[:, half * NQ:(half + 1) * NQ])
        in_maps.append(m)
    return in_maps


def kernel(**inputs):
    nc = _get_program()
    in_maps = make_in_maps(inputs)
    res = run_bass_kernel_spmd(nc, in_maps, core_ids=list(range(8)))
    out = np.empty((4, L, D), dtype=np.float32)
    for core in range(8):
        b, half = core // 2, core % 2
        out[b, half * NQ:(half + 1) * NQ] = np.asarray(res.results[core]["y"], dtype=np.float32)
    return out
```
